# Optimizing a Trainium2 kernel written in Bass

```python
import jax, jax.numpy as jnp
from jax import lax
import numpy as np

D_MODEL = 4096
BATCH = 2
SEQ = 4096
DEPTH = 2

CTX_LEN = 256
GRID_W = 64
HEAD_DIM = 128
ATTN_HEADS = D_MODEL // (2 * HEAD_DIM)
ATTN_KV_HEADS = 4
ATTN_GROUP = ATTN_HEADS // ATTN_KV_HEADS
ATTN_Q = ATTN_HEADS * HEAD_DIM
ATTN_KV = ATTN_KV_HEADS * HEAD_DIM
WINDOW = 128
ATTN_BLOCK = 128
ROPE_BASE = 10000.0
CMLP_GROUPS = D_MODEL // (4 * HEAD_DIM)
CMLP_CH = HEAD_DIM
CMLP_WIDTH = CMLP_GROUPS * CMLP_CH
CMLP_CHUNK = 128
RWKV_HEAD = 64
RWKV_WIDTH = D_MODEL // 4
RWKV_HEADS = RWKV_WIDTH // RWKV_HEAD
RWKV_LORA = 128
RWKV_GN_EPS = 64e-5
CONV_W = 3
N_DIR = 2
D_MIX = ATTN_Q + CMLP_WIDTH + RWKV_WIDTH
D_PROJ = ATTN_Q + 2 * ATTN_KV + 2 * CMLP_WIDTH + 4 * RWKV_WIDTH
N_EXPERTS = 16
N_EXPERT_GROUPS = 4
EXPERTS_PER_GROUP = N_EXPERTS // N_EXPERT_GROUPS
TOP_K = 2
EXPERT_FF = 1024
N_MOD = 6
EPS = 1e-6

kernel_name = 'hybrid_dit_hymba_rwkv7_moe'


def rmsnorm(x, g):
    xf = x.astype(jnp.float32)
    y = xf * lax.rsqrt(jnp.mean(xf * xf, axis=-1, keepdims=True) + EPS)
    return y.astype(x.dtype) * g


def modulate(z, g, shift, scale):
    return rmsnorm(z, g) * (1 + scale) + shift


def rope_1d(x, pos):
    half = x.shape[-1] // 2
    freqs = ROPE_BASE ** (-jnp.arange(half, dtype=jnp.float32) / half)
    ang = pos.astype(jnp.float32)[:, None] * freqs[None, :]
    cos = jnp.cos(ang)[None, :, None, :].astype(x.dtype)
    sin = jnp.sin(ang)[None, :, None, :].astype(x.dtype)
    x1, x2 = jnp.split(x, 2, axis=-1)
    return jnp.concatenate([x1 * cos - x2 * sin, x2 * cos + x1 * sin], axis=-1)


def axial_rope(x, row, col):
    xr, xc = jnp.split(x, 2, axis=-1)
    return jnp.concatenate([rope_1d(xr, row), rope_1d(xc, col)], axis=-1)


def centred_conv(x, w):
    return lax.conv_general_dilated(x, w[:, None, :], window_strides=(1,),
                                    padding=[(CONV_W // 2, CONV_W // 2)],
                                    dimension_numbers=('NWC', 'WIO', 'NWC'),
                                    feature_group_count=x.shape[-1])


def split_proj(zn, w_in_l, conv_l):
    proj = zn @ w_in_l
    offs = [ATTN_Q, ATTN_Q + ATTN_KV, ATTN_Q + 2 * ATTN_KV, ATTN_Q + 2 * ATTN_KV + CMLP_WIDTH,
            ATTN_Q + 2 * ATTN_KV + 2 * CMLP_WIDTH, ATTN_Q + 2 * ATTN_KV + 2 * CMLP_WIDTH + 3 * RWKV_WIDTH]
    q, k, v, u, gv, rkv, g = jnp.split(proj, offs, axis=-1)
    u, gv = jax.nn.gelu(u), jax.nn.gelu(gv)
    r, kr, vr = jnp.split(centred_conv(rkv, conv_l), 3, axis=-1)
    return q, k, v, u, gv, r, kr, vr, g


def latent_attention(q, k, v, kc, vc, sink):
    B, T = q.shape[:2]
    L = kc.shape[1]
    nb = T // ATTN_BLOCK
    nk = 3 * ATTN_BLOCK
    scale = HEAD_DIM ** -0.5
    qb = q.reshape(B, nb, ATTN_BLOCK, ATTN_KV_HEADS, ATTN_GROUP, HEAD_DIM)

    def band(z):
        zp = jnp.pad(z, ((0, 0), (ATTN_BLOCK, ATTN_BLOCK), (0, 0), (0, 0)))
        zp = zp.reshape(B, nb + 2, ATTN_BLOCK, ATTN_KV_HEADS, HEAD_DIM)
        return jnp.concatenate([zp[:, :-2], zp[:, 1:-1], zp[:, 2:]], axis=2)

    kb, vb = band(k), band(v)
    s_loc = jnp.einsum('bnqhgd,bnkhd->bnhgqk', qb, kb).astype(jnp.float32) * scale
    rel = jnp.arange(nk)[None, :] - ATTN_BLOCK - jnp.arange(ATTN_BLOCK)[:, None]
    key_pos = jnp.arange(nb)[:, None] * ATTN_BLOCK - ATTN_BLOCK + jnp.arange(nk)[None, :]
    mask = (jnp.abs(rel) <= WINDOW)[None] & ((key_pos >= 0) & (key_pos < T))[:, None, :]
    s_loc = jnp.where(mask[None, :, None, None], s_loc, -jnp.inf)
    s_ctx = jnp.einsum('bnqhgd,bkhd->bnhgqk', qb, kc).astype(jnp.float32) * scale
    s_sink = jnp.broadcast_to(sink.reshape(1, 1, ATTN_KV_HEADS, ATTN_GROUP, 1, 1).astype(jnp.float32),
                              s_loc.shape[:-1] + (1,))
    p = jax.nn.softmax(jnp.concatenate([s_loc, s_ctx, s_sink], axis=-1), axis=-1).astype(v.dtype)
    o = (jnp.einsum('bnhgqk,bnkhd->bnqhgd', p[..., :nk], vb)
         + jnp.einsum('bnhgqk,bkhd->bnqhgd', p[..., nk:nk + L], vc))
    return o.reshape(B, T, ATTN_Q)


def context_attention(qc, kc, vc, sink):
    B, L = qc.shape[:2]
    q = qc.reshape(B, L, ATTN_KV_HEADS, ATTN_GROUP, HEAD_DIM)
    s = jnp.einsum('bqhgd,bkhd->bhgqk', q, kc).astype(jnp.float32) * (HEAD_DIM ** -0.5)
    s_sink = jnp.broadcast_to(sink.reshape(1, ATTN_KV_HEADS, ATTN_GROUP, 1, 1).astype(jnp.float32),
                              s.shape[:-1] + (1,))
    p = jax.nn.softmax(jnp.concatenate([s, s_sink], axis=-1), axis=-1)[..., :L].astype(vc.dtype)
    return jnp.einsum('bhgqk,bkhd->bqhgd', p, vc).reshape(B, L, ATTN_Q)


def chunk_mlp(u, gv, norm_g, ws, bs):
    B, T = u.shape[:2]
    nc = T // CMLP_CHUNK
    gv = rmsnorm(gv, norm_g).reshape(B, nc, CMLP_CHUNK, CMLP_GROUPS, CMLP_CH)
    mixed = jnp.einsum('gpq,bnqgc->bnpgc', ws, gv) + bs.T[None, None, :, :, None]
    return (u.reshape(B, nc, CMLP_CHUNK, CMLP_GROUPS, CMLP_CH) * mixed).reshape(B, T, CMLP_WIDTH)


def heads(z):
    return z.reshape(z.shape[:-1] + (RWKV_HEADS, RWKV_HEAD))


def wkv_step(S, inp):
    r_t, w_t, kk_t, b_t, k_t, v_t = inp
    sa = jnp.einsum('zbhvk,zbhk->zbhv', S, -kk_t)
    S = S * w_t[..., None, :] + sa[..., :, None] * b_t[..., None, :] + v_t[..., :, None] * k_t[..., None, :]
    return S, jnp.einsum('zbhvk,zbhk->zbhv', S, r_t)


def time_major(z):
    return jnp.moveaxis(jnp.stack([z[0], jnp.flip(z[1], axis=1)]), 2, 0)


def rwkv_scan(zn, r, k, v, w0, w1, w2, a0, a1, a2, kk_p, ka_p, S0):
    f32 = jnp.float32
    w_raw = w0[:, None, None, :] + jnp.einsum('zbtr,zrc->zbtc', jnp.tanh(jnp.einsum('btd,zdr->zbtr', zn, w1)), w2)
    decay = jnp.exp(-jnp.exp(-jax.nn.softplus(-w_raw.astype(f32)) - 0.5))
    a = jax.nn.sigmoid((a0[:, None, None, :] + jnp.einsum('zbtr,zrc->zbtc', jnp.einsum('btd,zdr->zbtr', zn, a1), a2)).astype(f32))
    kf = k.astype(f32)
    kk = heads(kf * kk_p)
    kk = kk * lax.rsqrt(jnp.sum(kk * kk, axis=-1, keepdims=True) + 1e-12)
    a_h = heads(a)
    k_rep = heads(kf)[None] * (1 + (a_h - 1) * heads(ka_p))
    b = kk[None] * a_h
    shp = a_h.shape
    xs = (time_major(jnp.broadcast_to(heads(r.astype(f32))[None], shp)), time_major(heads(decay)),
          time_major(jnp.broadcast_to(kk[None], shp)), time_major(b), time_major(k_rep),
          time_major(jnp.broadcast_to(heads(v.astype(f32))[None], shp)))
    S, ys = lax.scan(wkv_step, S0, xs)
    ys = jnp.moveaxis(ys, 0, 2)
    return ys[0] + jnp.flip(ys[1], axis=1), S


def rwkv_output(y, r, k, v, g, rk, ln_w, ln_b):
    B, T = r.shape[:2]
    mu = jnp.mean(y, axis=-1, keepdims=True)
    var = jnp.mean(jnp.square(y - mu), axis=-1, keepdims=True)
    yn = ((y - mu) * lax.rsqrt(var + RWKV_GN_EPS)).reshape(B, T, RWKV_WIDTH) * ln_w + ln_b
    bonus = (jnp.sum(heads(r) * heads(k) * heads(rk), axis=-1, keepdims=True) * heads(v)).reshape(B, T, RWKV_WIDTH)
    return ((yn + bonus) * jax.nn.sigmoid(g)).astype(r.dtype)


def moe(zn, router_w, router_b, w1, w3, w2):
    shp = zn.shape
    z = zn.reshape(-1, D_MODEL)
    scores = jax.nn.sigmoid((z @ router_w).astype(jnp.float32))
    sel = scores + router_b.astype(jnp.float32)
    grp_score = jnp.sum(lax.top_k(sel.reshape(-1, N_EXPERT_GROUPS, EXPERTS_PER_GROUP), TOP_K)[0], axis=-1)
    best = jnp.argmax(grp_score, axis=-1)
    in_grp = (jnp.arange(N_EXPERTS) // EXPERTS_PER_GROUP)[None, :] == best[:, None]
    _, top_idx = lax.top_k(jnp.where(in_grp, sel, -jnp.inf), TOP_K)
    wts = jnp.take_along_axis(scores, top_idx, axis=-1)
    wts = wts / jnp.sum(wts, axis=-1, keepdims=True)
    gates = jnp.sum(jax.nn.one_hot(top_idx, N_EXPERTS, dtype=jnp.float32) * wts[..., None], axis=1).astype(z.dtype)
    hid = jax.nn.silu(jnp.einsum('nd,edf->nef', z, w1)) * jnp.einsum('nd,edf->nef', z, w3)
    hid = (hid * gates[:, :, None]).reshape(-1, N_EXPERTS * EXPERT_FF)
    return (hid @ w2.reshape(N_EXPERTS * EXPERT_FF, D_MODEL)).reshape(shp)


def setup_inputs(seed: int = 0) -> dict:
    key = jax.random.key(seed)
    ks = jax.random.split(key, 32)

    def nrm(i, shape, s):
        return jax.random.normal(ks[i], shape, jnp.float32) * s

    conv_base = jnp.zeros((CONV_W, 1), jnp.float32).at[CONV_W // 2].set(1.0)
    return {
        'x': nrm(0, (BATCH, SEQ, D_MODEL), 1.0),
        'c': nrm(1, (BATCH, D_MODEL), 1.0),
        'ctx': nrm(2, (BATCH, CTX_LEN, D_MODEL), 1.0),
        'c_ctx': nrm(3, (D_MODEL,), 1.0),
        'ada_w': nrm(4, (DEPTH, D_MODEL, N_MOD * D_MODEL), 0.5 * D_MODEL ** -0.5),
        'ada_b': nrm(5, (DEPTH, N_MOD * D_MODEL), 0.01),
        'norm1_g': 1.0 + nrm(6, (DEPTH, D_MODEL), 0.05),
        'w_in': nrm(7, (DEPTH, D_MODEL, D_PROJ), D_MODEL ** -0.5),
        'rwkv_conv': conv_base + nrm(8, (DEPTH, CONV_W, 3 * RWKV_WIDTH), 0.3),
        'attn_sink': nrm(9, (DEPTH, ATTN_HEADS), 0.5),
        'cmlp_norm_g': 1.0 + nrm(10, (DEPTH, CMLP_WIDTH), 0.05),
        'cmlp_ws': nrm(11, (DEPTH, CMLP_GROUPS, CMLP_CHUNK, CMLP_CHUNK), CMLP_CHUNK ** -0.5),
        'cmlp_b': 1.0 + nrm(12, (DEPTH, CMLP_GROUPS, CMLP_CHUNK), 0.1),
        'rwkv_w0': nrm(13, (DEPTH, N_DIR, RWKV_WIDTH), 1.0),
        'rwkv_w1': nrm(14, (DEPTH, N_DIR, D_MODEL, RWKV_LORA), D_MODEL ** -0.5),
        'rwkv_w2': nrm(15, (DEPTH, N_DIR, RWKV_LORA, RWKV_WIDTH), 0.5 * RWKV_LORA ** -0.5),
        'rwkv_a0': nrm(16, (DEPTH, N_DIR, RWKV_WIDTH), 0.5),
        'rwkv_a1': nrm(17, (DEPTH, N_DIR, D_MODEL, RWKV_LORA), D_MODEL ** -0.5),
        'rwkv_a2': nrm(18, (DEPTH, N_DIR, RWKV_LORA, RWKV_WIDTH), 0.5 * RWKV_LORA ** -0.5),
        'rwkv_kk': 1.0 + nrm(19, (DEPTH, RWKV_WIDTH), 0.1),
        'rwkv_ka': 1.0 + nrm(20, (DEPTH, RWKV_WIDTH), 0.1),
        'rwkv_rk': nrm(21, (DEPTH, RWKV_WIDTH), 0.3),
        'rwkv_ln_w': 1.0 + nrm(22, (DEPTH, RWKV_WIDTH), 0.05),
        'rwkv_ln_b': nrm(23, (DEPTH, RWKV_WIDTH), 0.01),
        'w_out': nrm(24, (DEPTH, D_MIX, D_MODEL), D_MIX ** -0.5),
        'norm2_g': 1.0 + nrm(25, (DEPTH, D_MODEL), 0.05),
        'router_w': nrm(26, (D_MODEL, N_EXPERTS), D_MODEL ** -0.5),
        'router_b': nrm(27, (N_EXPERTS,), 0.01),
        'moe_w1': nrm(28, (DEPTH, N_EXPERTS, D_MODEL, EXPERT_FF), D_MODEL ** -0.5),
        'moe_w3': nrm(29, (DEPTH, N_EXPERTS, D_MODEL, EXPERT_FF), D_MODEL ** -0.5),
        'moe_w2': nrm(30, (DEPTH, N_EXPERTS, EXPERT_FF, D_MODEL), EXPERT_FF ** -0.5),
        'final_g': 1.0 + nrm(31, (D_MODEL,), 0.05),
    }


def reference(x, c, ctx, c_ctx, ada_w, ada_b, norm1_g, w_in, rwkv_conv, attn_sink, cmlp_norm_g, cmlp_ws, cmlp_b,
              rwkv_w0, rwkv_w1, rwkv_w2, rwkv_a0, rwkv_a1, rwkv_a2, rwkv_kk, rwkv_ka, rwkv_rk, rwkv_ln_w, rwkv_ln_b,
              w_out, norm2_g, router_w, router_b, moe_w1, moe_w3, moe_w2, final_g):
    B, T, _ = x.shape
    L = ctx.shape[1]
    rows = T // GRID_W
    row = jnp.repeat(jnp.arange(rows), GRID_W)
    col = jnp.tile(jnp.arange(GRID_W), rows)
    s_zero = jnp.zeros((N_DIR, B, RWKV_HEADS, RWKV_HEAD, RWKV_HEAD), jnp.float32)
    h = ctx
    for l in range(DEPTH):
        mod_x = jax.nn.silu(c) @ ada_w[l] + ada_b[l]
        mod_c = jax.nn.silu(c_ctx) @ ada_w[l] + ada_b[l]
        sh1x, sc1x, g1x, sh2x, sc2x, g2x = [m[:, None, :] for m in jnp.split(mod_x, N_MOD, axis=-1)]
        sh1c, sc1c, g1c, sh2c, sc2c, g2c = jnp.split(mod_c, N_MOD, axis=-1)
        xn = modulate(x, norm1_g[l], sh1x, sc1x)
        hn = modulate(h, norm1_g[l], sh1c, sc1c)
        qx, kx, vx, ux, gvx, rx, krx, vrx, gx = split_proj(xn, w_in[l], rwkv_conv[l])
        qc, kc, vc, uc, gvc, rc, krc, vrc, gc = split_proj(hn, w_in[l], rwkv_conv[l])
        kc = kc.reshape(B, L, ATTN_KV_HEADS, HEAD_DIM)
        vc = vc.reshape(B, L, ATTN_KV_HEADS, HEAD_DIM)
        qx = axial_rope(qx.reshape(B, T, ATTN_HEADS, HEAD_DIM), row, col)
        kx = axial_rope(kx.reshape(B, T, ATTN_KV_HEADS, HEAD_DIM), row, col)
        vx = vx.reshape(B, T, ATTN_KV_HEADS, HEAD_DIM)
        attn_x = latent_attention(qx, kx, vx, kc, vc, attn_sink[l])
        cmlp_x = chunk_mlp(ux, gvx, cmlp_norm_g[l], cmlp_ws[l], cmlp_b[l])
        rw = (rwkv_w0[l], rwkv_w1[l], rwkv_w2[l], rwkv_a0[l], rwkv_a1[l], rwkv_a2[l], rwkv_kk[l], rwkv_ka[l])
        y_c, s_ctx = rwkv_scan(hn, rc, krc, vrc, *rw, s_zero)
        y_x, _ = rwkv_scan(xn, rx, krx, vrx, *rw, s_ctx)
        rwkv_x = rwkv_output(y_x, rx, krx, vrx, gx, rwkv_rk[l], rwkv_ln_w[l], rwkv_ln_b[l])
        x = x + g1x * (jnp.concatenate([attn_x, cmlp_x, rwkv_x], axis=-1) @ w_out[l])
        x = x + g2x * moe(modulate(x, norm2_g[l], sh2x, sc2x), router_w, router_b, moe_w1[l], moe_w3[l], moe_w2[l])
        if l < DEPTH - 1:
            attn_c = context_attention(qc, kc, vc, attn_sink[l])
            cmlp_c = chunk_mlp(uc, gvc, cmlp_norm_g[l], cmlp_ws[l], cmlp_b[l])
            rwkv_c = rwkv_output(y_c, rc, krc, vrc, gc, rwkv_rk[l], rwkv_ln_w[l], rwkv_ln_b[l])
            h = h + g1c * (jnp.concatenate([attn_c, cmlp_c, rwkv_c], axis=-1) @ w_out[l])
            h = h + g2c * moe(modulate(h, norm2_g[l], sh2c, sc2c), router_w, router_b, moe_w1[l], moe_w3[l], moe_w2[l])
    return rmsnorm(x, final_g)
```

```python
import numpy as np
import concourse.bass as bass
import concourse.mybir as mybir
from concourse.bass_utils import run_bass_kernel_spmd

F32 = mybir.dt.float32
AF = mybir.ActivationFunctionType
ALU = mybir.AluOpType
AX = mybir.AxisListType

NCORES = 8
D = 4096
T = 4096
LCTX = 256
DPROJ = 9216
NCH = D // 128


class Buf:
    __slots__ = ("name", "t", "w", "r", "dsem", "dcnt")

    def __init__(self, name, t):
        self.name = name
        self.t = t
        self.w = None
        self.r = {}
        self.dsem = None
        self.dcnt = 0

    def __getitem__(self, idx):
        return self.t[idx]


class KB:
    def __init__(self):
        self.nc = bass.Bass("TRN2", target_bir_lowering=False)
        nc = self.nc
        self.E = {"pe": nc.tensor, "dve": nc.vector, "act": nc.scalar, "pool": nc.gpsimd, "sp": nc.sync}
        self.sem = {e: nc.alloc_semaphore("c_" + e) for e in self.E}
        self.cnt = {e: 0 for e in self.E}
        self.seen = {e: {} for e in self.E}
        self.nb = 0
        self.final = {}

    def sb(self, shape, name=None, dtype=F32):
        self.nb += 1
        name = name or "sb%d" % self.nb
        return Buf(name, self.nc.alloc_sbuf_tensor(name, list(shape), dtype))

    def ps(self, shape, name=None, dtype=F32):
        self.nb += 1
        name = name or "ps%d" % self.nb
        return Buf(name, self.nc.alloc_psum_tensor(name, list(shape), dtype))

    def dram(self, name, shape, kind, dtype=F32):
        return Buf(name, self.nc.dram_tensor(name, list(shape), dtype, kind=kind))

    def _wait(self, eng, deps):
        for k, v in deps.items():
            if self.seen[eng].get(k, 0) < v:
                self.E[eng].wait_ge(self.sem[k], v)
                self.seen[eng][k] = v

    @staticmethod
    def _add(deps, d):
        if d is not None:
            k, v = d
            if deps.get(k, 0) < v:
                deps[k] = v

    def op(self, eng, fn, reads=(), writes=()):
        deps = {}
        for b in reads:
            self._add(deps, b.w)
        for b in writes:
            self._add(deps, b.w)
            for k, v in b.r.items():
                self._add(deps, (k, v))
        if eng == "pe":
            deps.pop("pe", None)
        self._wait(eng, deps)
        inst = fn(self.E[eng])
        self.cnt[eng] += 1
        c = self.cnt[eng]
        inst.then_inc(self.sem[eng], 1)
        for b in reads:
            b.r[eng] = c
        for b in writes:
            b.w = (eng, c)
            b.r = {}
        return inst

    def dma(self, q, out, in_, src=None, dst=None, owner=None, final=False, **kw):
        owner = owner or (dst if dst is not None else src)
        if owner.dsem is None:
            owner.dsem = "d_" + owner.name
            self.sem[owner.dsem] = self.nc.alloc_semaphore(owner.dsem)
        deps = {}
        if src is not None:
            self._add(deps, src.w)
        if dst is not None:
            self._add(deps, dst.w)
            for k, v in dst.r.items():
                self._add(deps, (k, v))
        self._wait(q, deps)
        inst = self.E[q].dma_start(out=out, in_=in_, **kw)
        owner.dcnt += 16
        inst.then_inc(self.sem[owner.dsem], 16)
        if src is not None:
            src.r[owner.dsem] = owner.dcnt
        if dst is not None:
            dst.w = (owner.dsem, owner.dcnt)
            dst.r = {}
        if final:
            self.final[owner.dsem] = owner.dcnt
        return inst

    def finish(self):
        self._wait("sp", dict(self.final))


ADA_COLS = 2 * 6 * D // NCORES


def build_ada():
    k = KB()
    ccT = k.dram("ccT", [128, NCH * 3], "ExternalInput")
    W = k.dram("W", [D, ADA_COLS], "ExternalInput")
    bias = k.dram("bias", [ADA_COLS], "ExternalInput")
    out = k.dram("out", [3, ADA_COLS], "ExternalOutput")
    cc = k.sb([128, NCH * 3], "cc")
    bt = k.sb([3, ADA_COLS], "bt")
    ot = k.sb([3, ADA_COLS], "ot")
    wb = [k.sb([128, NCH, 512], "wb%d" % i) for i in range(2)]
    pb = [k.ps([3, 512], "pb%d" % i) for i in range(2)]
    k.dma("sp", cc[:], ccT.t.ap(), dst=cc)
    k.dma("sp", bt[:], bias.t.ap().partition_broadcast(3), dst=bt)
    k.op("act", lambda e: e.activation(out=cc[:], in_=cc[:], func=AF.Silu), reads=[cc], writes=[cc])
    Wv = W.t.ap().rearrange("(c p) n -> p c n", p=128)
    ncb = ADA_COLS // 512
    for cb in range(ncb):
        w = wb[cb % 2]
        for q4 in range(4):
            k.dma("sp", w[:, q4 * 8:(q4 + 1) * 8, :], Wv[:, q4 * 8:(q4 + 1) * 8, cb * 512:(cb + 1) * 512], dst=w)
        p = pb[cb % 2]
        for c in range(NCH):
            k.op("pe", lambda e, c=c: e.matmul(p[:], lhsT=cc[:, c * 3:(c + 1) * 3], rhs=w[:, c, :],
                                                start=(c == 0), stop=(c == NCH - 1)),
                 reads=[cc, w], writes=[p])
        k.op("dve", lambda e: e.tensor_tensor(out=ot[:, cb * 512:(cb + 1) * 512], in0=p[:],
                                              in1=bt[:, cb * 512:(cb + 1) * 512], op=ALU.add),
             reads=[p, bt], writes=[ot])
    k.dma("sp", out.t.ap(), ot[:], src=ot, final=True)
    k.finish()
    return k.nc


def run_ada(c, c_ctx, ada_w, ada_b):
    cc = np.concatenate([c, c_ctx[None]], 0)
    ccT = np.ascontiguousarray(cc.T.reshape(NCH, 128, 3).transpose(1, 0, 2).reshape(128, NCH * 3))
    per = 6 * D // NCORES
    in_maps = []
    for i in range(NCORES):
        Wc = np.ascontiguousarray(np.concatenate([ada_w[l][:, i * per:(i + 1) * per] for l in range(2)], 1))
        bc = np.ascontiguousarray(np.concatenate([ada_b[l][i * per:(i + 1) * per] for l in range(2)], 0))
        in_maps.append({"ccT": ccT, "W": Wc, "bias": bc})
    res = run_bass_kernel_spmd(build_ada(), in_maps, core_ids=list(range(NCORES)))
    mod = np.zeros((2, 3, 6 * D), np.float32)
    for i in range(NCORES):
        o = res.results[i]["out"]
        for l in range(2):
            mod[l][:, i * per:(i + 1) * per] = o[:, l * per:(l + 1) * per]
    return mod


NT = 9
NTOK = NT * 128


def rms_scale(k, xt, sq, st, width):
    k.op("dve", lambda e: e.tensor_tensor(out=sq[:, :width], in0=xt[:, :width], in1=xt[:, :width], op=ALU.mult),
         reads=[xt], writes=[sq])
    k.op("dve", lambda e: e.tensor_reduce(out=st[:, 0:1], in_=sq[:, :width], axis=AX.X, op=ALU.add),
         reads=[sq], writes=[st])
    k.op("dve", lambda e: e.tensor_scalar(out=st[:, 1:2], in0=st[:, 0:1], scalar1=1.0 / width, scalar2=1e-6,
                                          op0=ALU.mult, op1=ALU.add), reads=[st], writes=[st])
    k.op("act", lambda e: e.activation(out=st[:, 2:3], in_=st[:, 1:2], func=AF.Sqrt), reads=[st], writes=[st])
    k.op("dve", lambda e: e.reciprocal(out=st[:, 3:4], in_=st[:, 2:3]), reads=[st], writes=[st])
    k.op("dve", lambda e: e.tensor_scalar(out=xt[:, :width], in0=xt[:, :width], scalar1=st[:, 3:4], scalar2=None,
                                          op0=ALU.mult), reads=[xt, st], writes=[xt])


def transpose_tile(k, xt, ident, pst, xT_dst, dst_buf, gcol=None, scol=None, mbuf=None, nch=NCH):
    for c4 in range(0, nch, 4):
        p = pst[(c4 // 4) % len(pst)]
        n4 = min(4, nch - c4)
        for j in range(n4):
            c = c4 + j
            k.op("pe", lambda e, c=c, j=j: e.transpose(out=p[:, j * 128:(j + 1) * 128],
                                                       in_=xt[:, c * 128:(c + 1) * 128], identity=ident[:]),
                 reads=[xt, ident], writes=[p])
        for j in range(n4):
            c = c4 + j
            if gcol is not None:
                k.op("dve", lambda e, c=c, j=j: e.tensor_scalar(out=xT_dst(c), in0=p[:, j * 128:(j + 1) * 128],
                                                                scalar1=gcol[:, c:c + 1], scalar2=scol[:, c:c + 1],
                                                                op0=ALU.mult, op1=ALU.add),
                     reads=[p, mbuf], writes=[dst_buf])
            else:
                k.op("act", lambda e, c=c, j=j: e.copy(out=xT_dst(c), in_=p[:, j * 128:(j + 1) * 128]),
                     reads=[p], writes=[dst_buf])


NC1 = DPROJ + 512
GT = 3


def build_l1():
    k = KB()
    xin = k.dram("xin", [NTOK, D], "ExternalInput")
    modcol = k.dram("modcol", [128, 5 * NCH], "ExternalInput")
    identd = k.dram("identd", [128, 128], "ExternalInput")
    W = k.dram("W", [D, NC1], "ExternalInput")
    proj = k.dram("proj", [NTOK, NC1], "ExternalOutput")
    ident = k.sb([128, 128], "ident")
    mc = k.sb([128, 5 * NCH], "mc")
    gs = k.sb([128, 4 * NCH], "gs")
    xt = [k.sb([128, D], "xt%d" % i) for i in range(2)]
    sq = k.sb([128, D], "sq")
    st = k.sb([128, 4], "st")
    xT = k.sb([128, GT * NCH * 128], "xT")
    BW = 256
    wb = [k.sb([128, NCH, BW], "wb%d" % i) for i in range(2)]
    og = [k.sb([128, BW], "og%d" % i) for i in range(3)]
    pst = [k.ps([128, 512], "pst%d" % i) for i in range(2)]
    pso = [k.ps([128, BW], "pso%d" % i) for i in range(3)]
    k.dma("sp", ident[:], identd.t.ap(), dst=ident)
    k.dma("sp", mc[:], modcol.t.ap(), dst=mc)
    g = mc[:, 0:NCH]
    for s in range(2):
        sc = mc[:, (1 + 2 * s) * NCH:(2 + 2 * s) * NCH]
        sh = mc[:, (2 + 2 * s) * NCH:(3 + 2 * s) * NCH]
        k.op("dve", lambda e, s=s, sc=sc: e.tensor_tensor(out=gs[:, 2 * s * NCH:(2 * s + 1) * NCH], in0=g, in1=sc,
                                                          op=ALU.mult), reads=[mc], writes=[gs])
        k.op("dve", lambda e, s=s: e.tensor_tensor(out=gs[:, 2 * s * NCH:(2 * s + 1) * NCH],
                                                   in0=gs[:, 2 * s * NCH:(2 * s + 1) * NCH], in1=g, op=ALU.add),
             reads=[mc, gs], writes=[gs])
        k.op("dve", lambda e, s=s, sh=sh: e.tensor_copy(out=gs[:, (2 * s + 1) * NCH:(2 * s + 2) * NCH], in_=sh),
             reads=[mc], writes=[gs])
    Wv = W.t.ap().rearrange("(c p) n -> p c n", p=128)
    nblk = NC1 // BW
    it = 0
    for grp in range(NT // GT):
        for j in range(GT):
            ti = grp * GT + j
            x = xt[ti % 2]
            k.dma("sp", x[:], xin.t.ap()[ti * 128:(ti + 1) * 128, :], dst=x)
            rms_scale(k, x, sq, st, D)
            s = 0 if ti < 8 else 1
            transpose_tile(k, x, ident, pst,
                           lambda c, j=j: xT[:, (j * NCH + c) * 128:(j * NCH + c + 1) * 128], xT,
                           gcol=gs[:, 2 * s * NCH:(2 * s + 1) * NCH], scol=gs[:, (2 * s + 1) * NCH:(2 * s + 2) * NCH],
                           mbuf=gs)
        for blk in range(nblk):
            w = wb[blk % 2]
            for q4 in range(4):
                k.dma("sp", w[:, q4 * 8:(q4 + 1) * 8, :], Wv[:, q4 * 8:(q4 + 1) * 8, blk * BW:(blk + 1) * BW], dst=w)
            for j in range(GT):
                ti = grp * GT + j
                p = pso[it % 3]
                o = og[it % 3]
                it += 1
                for c in range(NCH):
                    k.op("pe", lambda e, c=c, j=j: e.matmul(p[:], lhsT=xT[:, (j * NCH + c) * 128:(j * NCH + c + 1) * 128],
                                                            rhs=w[:, c, :], start=(c == 0), stop=(c == NCH - 1)),
                         reads=[xT, w], writes=[p])
                k.op("act", lambda e: e.copy(out=o[:], in_=p[:]), reads=[p], writes=[o])
                k.dma("pool", proj.t.ap()[ti * 128:(ti + 1) * 128, blk * BW:(blk + 1) * BW], o[:], src=o, final=True)
    k.finish()
    return k.nc


def tok_rows(i):
    return i // 4, (i % 4) * 1024, (i % 4) % 2


def col_layout(v):
    return np.ascontiguousarray(v.reshape(NCH, 128).T)


def run_l1(x, h, mod_l, norm1_g_l, Wcat):
    ident = np.eye(128, dtype=np.float32)
    in_maps = []
    for i in range(NCORES):
        b, t0, cj = tok_rows(i)
        xin = np.ascontiguousarray(np.concatenate([x[b, t0:t0 + 1024], h[b, cj * 128:(cj + 1) * 128]], 0))
        mc = np.concatenate([col_layout(norm1_g_l), col_layout(mod_l[b, D:2 * D]), col_layout(mod_l[b, 0:D]),
                             col_layout(mod_l[2, D:2 * D]), col_layout(mod_l[2, 0:D])], 1)
        in_maps.append({"xin": xin, "modcol": np.ascontiguousarray(mc), "identd": ident, "W": Wcat})
    res = run_bass_kernel_spmd(build_l1(), in_maps, core_ids=list(range(NCORES)))
    px = np.zeros((2, T, NC1), np.float32)
    ph = np.zeros((2, LCTX, NC1), np.float32)
    for i in range(NCORES):
        b, t0, cj = tok_rows(i)
        o = res.results[i]["proj"]
        px[b, t0:t0 + 1024] = o[:1024]
        if (i % 4) < 2:
            ph[b, cj * 128:(cj + 1) * 128] = o[1024:]
    return px, ph


HD = 128
NH = 16
NKV = 4
CW = 1024
RW = 1024
GELU_C = 1.5957691216057308


def gelu_tanh(k, x, t1, w):
    k.op("dve", lambda e: e.tensor_tensor(out=t1[:, :w], in0=x[:, :w], in1=x[:, :w], op=ALU.mult), reads=[x], writes=[t1])
    k.op("dve", lambda e: e.tensor_scalar(out=t1[:, :w], in0=t1[:, :w], scalar1=0.044715, scalar2=1.0, op0=ALU.mult,
                                          op1=ALU.add), reads=[t1], writes=[t1])
    k.op("dve", lambda e: e.tensor_tensor(out=t1[:, :w], in0=t1[:, :w], in1=x[:, :w], op=ALU.mult), reads=[t1, x], writes=[t1])
    k.op("act", lambda e: e.activation(out=t1[:, :w], in_=t1[:, :w], func=AF.Sigmoid, scale=GELU_C), reads=[t1], writes=[t1])
    k.op("dve", lambda e: e.tensor_tensor(out=x[:, :w], in0=x[:, :w], in1=t1[:, :w], op=ALU.mult), reads=[t1, x], writes=[x])


def rope(k, x, c, s, t1, w):
    def v(b, e):
        return b[:, :w].rearrange("p (a two s) -> p a two s", two=2, s=32)[:, :, e, :]
    for e_ in range(2):
        k.op("dve", lambda e, e_=e_: e.tensor_tensor(out=v(t1, e_), in0=v(x, 1 - e_), in1=v(s, e_), op=ALU.mult),
             reads=[x, s], writes=[t1])
    k.op("dve", lambda e: e.tensor_tensor(out=x[:, :w], in0=x[:, :w], in1=c[:, :w], op=ALU.mult), reads=[x, c], writes=[x])
    k.op("dve", lambda e: e.tensor_tensor(out=x[:, :w], in0=x[:, :w], in1=t1[:, :w], op=ALU.add), reads=[x, t1], writes=[x])


def build_l2(do_attn=True, do_cmlp=True, do_rwkv=True):
    k = KB()
    di = lambda n, s: k.dram(n, s, "ExternalInput")
    qx = di("qx", [NTOK, 2048]); kh = di("kh", [1280, 512]); vh = di("vh", [1280, 512])
    kc = di("kc", [256, 512]); vc = di("vc", [256, 512])
    rqc = di("rqc", [1024, 2048]); rqs = di("rqs", [1024, 2048]); rkc = di("rkc", [1280, 512]); rks = di("rks", [1280, 512])
    masks = di("masks", [4 * 128, 512]); sink = di("sink", [16])
    ug = di("ug", [NTOK, 2048]); cng = di("cng", [CW]); wsT = di("wsT", [128, 1024]); bsT = di("bsT", [128, 8])
    rkvp = di("rkvp", [1026 + 130, 3072]); lora = di("lora", [NTOK, 512]); convw = di("convw", [3, 3072])
    w2a2 = di("w2a2", [128, 4096]); w0a0 = di("w0a0", [1, 4096]); kkp = di("kkp", [RW]); kap = di("kap", [RW])
    identd = di("identd", [128, 128])
    attn = k.dram("attn", [NTOK, 2048], "ExternalOutput")
    cmlp = k.dram("cmlp", [NTOK, CW], "ExternalOutput")
    scin = k.dram("scin", [NTOK, 10 * RW], "ExternalOutput")

    ident = k.sb([128, 128], "ident")
    k.dma("sp", ident[:], identd.t.ap(), dst=ident)
    WK = [k.sb([128, 3072], "wk%d" % i) for i in range(6)]
    pst = [k.ps([128, 512], "pst%d" % i) for i in range(2)]
    psA = [k.ps([128, 512], "psA%d" % i) for i in range(3)]
    psB = [k.ps([128, 512], "psB%d" % i) for i in range(2)]
    scale = float(HD) ** -0.5

    if do_attn:
        kT = k.sb([128, NKV * 1280], "kT")
        va = k.sb([128, 10 * NKV * 129], "va")
        kcT = k.sb([128, NKV * 256], "kcT")
        vca = k.sb([128, 2 * NKV * 129], "vca")
        mk = k.sb([128, 4 * 512], "mk")
        esk = k.sb([128, 16], "esk")
        st = k.sb([128, 8], "stA")
        for m in range(4):
            k.dma("sp", mk[:, m * 512:(m + 1) * 512], masks.t.ap()[m * 128:(m + 1) * 128, :], dst=mk)
        k.dma("sp", esk[:], sink.t.ap().partition_broadcast(128), dst=esk)
        k.op("act", lambda e: e.activation(out=esk[:], in_=esk[:], func=AF.Exp), reads=[esk], writes=[esk])
        k.op("dve", lambda e: e.memset(va[:], 1.0), writes=[va])
        k.op("dve", lambda e: e.memset(vca[:], 1.0), writes=[vca])
        va4 = lambda blk, h: va[:, (blk * NKV + h) * 129:(blk * NKV + h) * 129 + 129]
        vca4 = lambda blk, h: vca[:, (blk * NKV + h) * 129:(blk * NKV + h) * 129 + 129]
        kt_, vt_, c_, s_, t_ = WK[0], WK[1], WK[2], WK[3], WK[4]
        for blk in range(12):
            isctx = blk >= 10
            src_k = kc.t.ap()[(blk - 10) * 128:(blk - 9) * 128, :] if isctx else kh.t.ap()[blk * 128:(blk + 1) * 128, :]
            src_v = vc.t.ap()[(blk - 10) * 128:(blk - 9) * 128, :] if isctx else vh.t.ap()[blk * 128:(blk + 1) * 128, :]
            k.dma("sp", kt_[:, :512], src_k, dst=kt_)
            k.dma("sp", vt_[:, :512], src_v, dst=vt_)
            if not isctx:
                k.dma("sp", c_[:, :512], rkc.t.ap()[blk * 128:(blk + 1) * 128, :], dst=c_)
                k.dma("sp", s_[:, :512], rks.t.ap()[blk * 128:(blk + 1) * 128, :], dst=s_)
                rope(k, kt_, c_, s_, t_, 512)
                transpose_tile(k, kt_, ident, pst, lambda c, blk=blk: kT[:, c * 1280 + blk * 128:c * 1280 + (blk + 1) * 128],
                               kT, nch=NKV)
            else:
                transpose_tile(k, kt_, ident, pst,
                               lambda c, blk=blk: kcT[:, c * 256 + (blk - 10) * 128:c * 256 + (blk - 9) * 128], kcT, nch=NKV)
            for h in range(NKV):
                dst = vca4(blk - 10, h) if isctx else va4(blk, h)
                k.op("dve", lambda e, dst=dst, h=h: e.tensor_copy(out=dst[:, 0:128], in_=vt_[:, h * 128:(h + 1) * 128]),
                     reads=[vt_], writes=[vca if isctx else va])
        qt, qT, PT, ao = WK[0], WK[1], WK[2], WK[3]
        c_, s_, t_ = WK[4], WK[5], WK[2]
        for ti in range(NT):
            isctx = ti == 8
            k.dma("sp", qt[:, :2048], qx.t.ap()[ti * 128:(ti + 1) * 128, :], dst=qt)
            if not isctx:
                k.dma("sp", c_[:, :2048], rqc.t.ap()[ti * 128:(ti + 1) * 128, :], dst=c_)
                k.dma("sp", s_[:, :2048], rqs.t.ap()[ti * 128:(ti + 1) * 128, :], dst=s_)
                rope(k, qt, c_, s_, t_, 2048)
            transpose_tile(k, qt, ident, pst, lambda c: qT[:, c * 128:(c + 1) * 128], qT, nch=NH)
            chunks = []
            if not isctx:
                for d_ in range(3):
                    blk = ti + d_
                    m = None
                    if d_ == 0:
                        m = 0 if ti == 0 else 1
                    if d_ == 2:
                        m = 3 if ti == 7 else 2
                    chunks.append((lambda h, blk=blk: kT[:, h * 1280 + blk * 128:h * 1280 + (blk + 1) * 128],
                                   lambda h, blk=blk: va4(blk, h), va, kT, m))
            for cb in range(2):
                chunks.append((lambda h, cb=cb: kcT[:, h * 256 + cb * 128:h * 256 + (cb + 1) * 128],
                               lambda h, cb=cb: vca4(cb, h), vca, kcT, None))
            for h in range(NKV):
                for ci, (kf, vf, vbuf, kbuf, m) in enumerate(chunks):
                    p = psA[ci % 3]
                    k.op("pe", lambda e, kf=kf, p=p: e.matmul(p[:], lhsT=kf(h), rhs=qT[:, h * 512:(h + 1) * 512],
                                                              start=True, stop=True), reads=[kbuf, qT], writes=[p])
                    k.op("act", lambda e, ci=ci, p=p: e.activation(out=PT[:, ci * 512:(ci + 1) * 512], in_=p[:], func=AF.Exp,
                                                                   scale=scale), reads=[p], writes=[PT])
                    if m is not None:
                        k.op("dve", lambda e, ci=ci, m=m: e.tensor_tensor(out=PT[:, ci * 512:(ci + 1) * 512],
                                                                          in0=PT[:, ci * 512:(ci + 1) * 512],
                                                                          in1=mk[:, m * 512:(m + 1) * 512], op=ALU.mult),
                             reads=[PT, mk], writes=[PT])
                for g in range(4):
                    hq = h * 4 + g
                    p = psB[g % 2]
                    for ci, (kf, vf, vbuf, kbuf, m) in enumerate(chunks):
                        k.op("pe", lambda e, ci=ci, vf=vf, p=p: e.matmul(p[:, 0:129],
                                                                         lhsT=PT[:, ci * 512 + g * 128:ci * 512 + (g + 1) * 128],
                                                                         rhs=vf(h), start=(ci == 0), stop=(ci == len(chunks) - 1)),
                             reads=[PT, vbuf], writes=[p])
                    k.op("dve", lambda e, p=p, hq=hq: e.tensor_tensor(out=st[:, 0:1], in0=p[:, 128:129], in1=esk[:, hq:hq + 1],
                                                                      op=ALU.add), reads=[p, esk], writes=[st])
                    k.op("dve", lambda e: e.reciprocal(out=st[:, 1:2], in_=st[:, 0:1]), reads=[st], writes=[st])
                    k.op("dve", lambda e, p=p, hq=hq: e.tensor_scalar(out=ao[:, hq * 128:(hq + 1) * 128], in0=p[:, 0:128],
                                                                      scalar1=st[:, 1:2], scalar2=None, op0=ALU.mult),
                         reads=[p, st], writes=[ao])
            k.dma("pool", attn.t.ap()[ti * 128:(ti + 1) * 128, :], ao[:, :2048], src=ao, final=True)

    if do_cmlp:
        ng = k.sb([128, CW], "ng"); wst = k.sb([128, 1024], "wst"); bst = k.sb([128, 8], "bst")
        stc = k.sb([128, 4], "stC")
        k.dma("sp", ng[:], cng.t.ap().partition_broadcast(128), dst=ng)
        k.dma("sp", wst[:], wsT.t.ap(), dst=wst)
        k.dma("sp", bst[:], bsT.t.ap(), dst=bst)
        ut, t1, sq, cm = WK[0], WK[1], WK[2], WK[3]
        for ti in range(NT):
            k.dma("sp", ut[:, :2048], ug.t.ap()[ti * 128:(ti + 1) * 128, :], dst=ut)
            gelu_tanh(k, ut, t1, 2048)
            gvv = Buf("gvview", None)
            k.op("dve", lambda e: e.tensor_tensor(out=sq[:, :CW], in0=ut[:, CW:2 * CW], in1=ut[:, CW:2 * CW], op=ALU.mult),
                 reads=[ut], writes=[sq])
            k.op("dve", lambda e: e.tensor_reduce(out=stc[:, 0:1], in_=sq[:, :CW], axis=AX.X, op=ALU.add), reads=[sq], writes=[stc])
            k.op("dve", lambda e: e.tensor_scalar(out=stc[:, 1:2], in0=stc[:, 0:1], scalar1=1.0 / CW, scalar2=1e-6,
                                                  op0=ALU.mult, op1=ALU.add), reads=[stc], writes=[stc])
            k.op("act", lambda e: e.activation(out=stc[:, 2:3], in_=stc[:, 1:2], func=AF.Sqrt), reads=[stc], writes=[stc])
            k.op("dve", lambda e: e.reciprocal(out=stc[:, 3:4], in_=stc[:, 2:3]), reads=[stc], writes=[stc])
            k.op("dve", lambda e: e.scalar_tensor_tensor(out=sq[:, :CW], in0=ut[:, CW:2 * CW], scalar=stc[:, 3:4], in1=ng[:],
                                                         op0=ALU.mult, op1=ALU.mult), reads=[ut, stc, ng], writes=[sq])
            for g in range(8):
                p = psA[(g // 4) % 3]
                k.op("pe", lambda e, g=g, p=p: e.matmul(p[:, (g % 4) * 128:(g % 4 + 1) * 128], lhsT=wst[:, g * 128:(g + 1) * 128],
                                                        rhs=sq[:, g * 128:(g + 1) * 128], start=True, stop=True),
                     reads=[wst, sq], writes=[p])
            for g in range(8):
                p = psA[(g // 4) % 3]
                k.op("dve", lambda e, g=g, p=p: e.scalar_tensor_tensor(out=cm[:, g * 128:(g + 1) * 128],
                                                                       in0=p[:, (g % 4) * 128:(g % 4 + 1) * 128],
                                                                       scalar=bst[:, g:g + 1], in1=ut[:, g * 128:(g + 1) * 128],
                                                                       op0=ALU.add, op1=ALU.mult), reads=[p, bst, ut], writes=[cm])
            k.dma("pool", cmlp.t.ap()[ti * 128:(ti + 1) * 128, :], cm[:, :CW], src=cm, final=True)

    if do_rwkv:
        w2t = k.sb([128, 4096], "w2t"); w0t = k.sb([1, 4096], "w0t"); ones = k.sb([1, 128], "ones1")
        kkpt = k.sb([128, RW], "kkpt"); kapt = k.sb([128, RW], "kapt")
        lt = k.sb([128, 512], "lt"); lT = k.sb([128, 512], "lT")
        ss = k.sb([128, 64], "ss")
        k.dma("sp", w2t[:], w2a2.t.ap(), dst=w2t)
        k.dma("sp", w0t[:], w0a0.t.ap(), dst=w0t)
        k.dma("sp", kkpt[:], kkp.t.ap().partition_broadcast(128), dst=kkpt)
        k.dma("sp", kapt[:], kap.t.ap().partition_broadcast(128), dst=kapt)
        k.op("dve", lambda e: e.memset(ones[:], 1.0), writes=[ones])
        xs, cw, o1, kf, kk, o2 = WK
        for ti in range(NT):
            r0 = ti * 128 if ti < 8 else 1026
            outs = {}
            for part in range(3):
                for j in range(3):
                    k.dma("sp", xs[:, j * 1024:(j + 1) * 1024], rkvp.t.ap()[r0 + j:r0 + j + 128, part * 1024:(part + 1) * 1024], dst=xs)
                    k.dma("sp", cw[:, j * 1024:(j + 1) * 1024],
                          convw.t.ap()[j, part * 1024:(part + 1) * 1024].partition_broadcast(128), dst=cw)
                dstb = kf if part == 1 else o1
                k.op("dve", lambda e: e.tensor_tensor(out=xs[:], in0=xs[:], in1=cw[:], op=ALU.mult), reads=[xs, cw], writes=[xs])
                k.op("dve", lambda e, dstb=dstb: e.tensor_tensor(out=dstb[:, :RW], in0=xs[:, 0:RW], in1=xs[:, RW:2 * RW], op=ALU.add),
                     reads=[xs], writes=[dstb])
                k.op("dve", lambda e, dstb=dstb: e.tensor_tensor(out=dstb[:, :RW], in0=dstb[:, :RW], in1=xs[:, 2 * RW:3 * RW], op=ALU.add),
                     reads=[xs, dstb], writes=[dstb])
                qi = (0, 9, 2)[part]
                k.dma("pool", scin.t.ap()[ti * 128:(ti + 1) * 128, qi * RW:(qi + 1) * RW], dstb[:, :RW], src=dstb, final=True)
            k.op("dve", lambda e: e.tensor_tensor(out=kk[:, :RW], in0=kf[:, :RW], in1=kkpt[:], op=ALU.mult), reads=[kf, kkpt], writes=[kk])
            k.op("dve", lambda e: e.tensor_tensor(out=o2[:, :RW], in0=kk[:, :RW], in1=kk[:, :RW], op=ALU.mult), reads=[kk], writes=[o2])
            k.op("dve", lambda e: e.tensor_reduce(out=ss[:, 0:16], in_=o2[:, :RW].rearrange("p (h n) -> p h n", n=64), axis=AX.X,
                                                  op=ALU.add), reads=[o2], writes=[ss])
            k.op("dve", lambda e: e.tensor_scalar(out=ss[:, 0:16], in0=ss[:, 0:16], scalar1=1e-12, scalar2=None, op0=ALU.add),
                 reads=[ss], writes=[ss])
            k.op("act", lambda e: e.activation(out=ss[:, 16:32], in_=ss[:, 0:16], func=AF.Sqrt), reads=[ss], writes=[ss])
            k.op("dve", lambda e: e.reciprocal(out=ss[:, 32:48], in_=ss[:, 16:32]), reads=[ss], writes=[ss])
            for h in range(16):
                k.op("dve", lambda e, h=h: e.tensor_scalar(out=kk[:, h * 64:(h + 1) * 64], in0=kk[:, h * 64:(h + 1) * 64],
                                                           scalar1=ss[:, 32 + h:33 + h], scalar2=None, op0=ALU.mult),
                     reads=[kk, ss], writes=[kk])
            k.op("dve", lambda e: e.tensor_scalar(out=o2[:, :RW], in0=kk[:, :RW], scalar1=-1.0, scalar2=None, op0=ALU.mult),
                 reads=[kk], writes=[o2])
            k.dma("pool", scin.t.ap()[ti * 128:(ti + 1) * 128, 1 * RW:2 * RW], o2[:, :RW], src=o2, final=True)
            k.dma("sp", lt[:], lora.t.ap()[ti * 128:(ti + 1) * 128, :], dst=lt)
            k.op("act", lambda e: e.activation(out=lt[:, 0:256], in_=lt[:, 0:256], func=AF.Tanh), reads=[lt], writes=[lt])
            transpose_tile(k, lt, ident, pst, lambda c: lT[:, c * 128:(c + 1) * 128], lT, nch=4)
            for z in range(2):
                for half in range(2):
                    p = psA[half]
                    cs = slice(z * 1024 + half * 512, z * 1024 + half * 512 + 512)
                    k.op("pe", lambda e, p=p, cs=cs: e.matmul(p[:], lhsT=lT[:, z * 128:(z + 1) * 128], rhs=w2t[:, cs], start=True,
                                                              stop=False), reads=[lT, w2t], writes=[p])
                    k.op("pe", lambda e, p=p, cs=cs: e.matmul(p[:], lhsT=ones[:], rhs=w0t[:, cs], start=False, stop=True),
                         reads=[ones, w0t], writes=[p])
                    k.op("act", lambda e, p=p, half=half: e.activation(out=o1[:, half * 512:(half + 1) * 512], in_=p[:],
                                                                       func=AF.Sigmoid), reads=[p], writes=[o1])
                k.op("act", lambda e: e.activation(out=o1[:, :RW], in_=o1[:, :RW], func=AF.Exp, scale=-float(np.exp(-0.5))),
                     reads=[o1], writes=[o1])
                k.dma("pool", scin.t.ap()[ti * 128:(ti + 1) * 128, (3 + 3 * z) * RW:(4 + 3 * z) * RW], o1[:, :RW], src=o1, final=True)
                for half in range(2):
                    p = psA[half]
                    cs = slice(2048 + z * 1024 + half * 512, 2048 + z * 1024 + half * 512 + 512)
                    k.op("pe", lambda e, p=p, cs=cs: e.matmul(p[:], lhsT=lT[:, (2 + z) * 128:(3 + z) * 128], rhs=w2t[:, cs], start=True,
                                                              stop=False), reads=[lT, w2t], writes=[p])
                    k.op("pe", lambda e, p=p, cs=cs: e.matmul(p[:], lhsT=ones[:], rhs=w0t[:, cs], start=False, stop=True),
                         reads=[ones, w0t], writes=[p])
                    k.op("act", lambda e, p=p, half=half: e.activation(out=xs[:, half * 512:(half + 1) * 512], in_=p[:],
                                                                       func=AF.Sigmoid), reads=[p], writes=[xs])
                k.op("dve", lambda e: e.tensor_tensor(out=cw[:, :RW], in0=kk[:, :RW], in1=xs[:, :RW], op=ALU.mult),
                     reads=[kk, xs], writes=[cw])
                k.dma("pool", scin.t.ap()[ti * 128:(ti + 1) * 128, (4 + 3 * z) * RW:(5 + 3 * z) * RW], cw[:, :RW], src=cw, final=True)
                k.op("dve", lambda e: e.scalar_tensor_tensor(out=xs[:, RW:2 * RW], in0=xs[:, :RW], scalar=-1.0, in1=kapt[:],
                                                             op0=ALU.add, op1=ALU.mult), reads=[xs, kapt], writes=[xs])
                k.op("dve", lambda e: e.scalar_tensor_tensor(out=xs[:, 2 * RW:3 * RW], in0=xs[:, RW:2 * RW], scalar=1.0, in1=kf[:, :RW],
                                                             op0=ALU.add, op1=ALU.mult), reads=[xs, kf], writes=[xs])
                k.dma("pool", scin.t.ap()[ti * 128:(ti + 1) * 128, (5 + 3 * z) * RW:(6 + 3 * z) * RW], xs[:, 2 * RW:3 * RW], src=xs,
                      final=True)
    k.finish()
    return k.nc


def rope_tables():
    half = 32
    freqs = 10000.0 ** (-np.arange(half, dtype=np.float32) / half)
    t = np.arange(T)
    row = (t // 64).astype(np.float32)[:, None] * freqs[None, :]
    col = (t % 64).astype(np.float32)[:, None] * freqs[None, :]
    cr, sr, cc, sc = np.cos(row), np.sin(row), np.cos(col), np.sin(col)
    C = np.concatenate([cr, cr, cc, cc], 1).astype(np.float32)
    S = np.concatenate([-sr, sr, -sc, sc], 1).astype(np.float32)
    return C, S


def l2_inmaps(px, ph, P):
    C, S = rope_tables()
    ident = np.eye(128, dtype=np.float32)
    jj = np.arange(128)[:, None]
    ii = np.arange(128)[None, :]
    mprev = np.tile((jj >= ii).astype(np.float32), (1, 4))
    mnext = np.tile((jj <= ii).astype(np.float32), (1, 4))
    zero = np.zeros_like(mprev)
    wsT = np.ascontiguousarray(P["cmlp_ws"].transpose(2, 0, 1).reshape(128, 1024))
    bsT = np.ascontiguousarray(P["cmlp_b"].T)
    w2a2 = np.ascontiguousarray(np.concatenate([P["rwkv_w2"][0], P["rwkv_w2"][1], P["rwkv_a2"][0], P["rwkv_a2"][1]], 1))
    w0a0 = np.ascontiguousarray(np.concatenate([P["rwkv_w0"][0], P["rwkv_w0"][1], P["rwkv_a0"][0], P["rwkv_a0"][1]])[None])
    in_maps = []
    for i in range(NCORES):
        b, t0, cj = tok_rows(i)
        cat = lambda a, c: np.ascontiguousarray(np.concatenate([a, c], 0))
        ctile = ph[b, cj * 128:(cj + 1) * 128]
        lat = px[b, t0:t0 + 1024]
        def halo(cols, n, src, s0, s1):
            o = np.zeros((s1 - s0 + 2 * n, cols.stop - cols.start), np.float32)
            lo, hi = max(s0 - n, 0), min(s1 + n, src.shape[0])
            o[lo - (s0 - n):hi - (s0 - n)] = src[lo:hi, cols]
            return o
        Ck = np.zeros((1280, 128), np.float32); Sk = np.zeros((1280, 128), np.float32)
        lo, hi = max(t0 - 128, 0), min(t0 + 1152, T)
        Ck[lo - (t0 - 128):hi - (t0 - 128)] = C[lo:hi]; Sk[lo - (t0 - 128):hi - (t0 - 128)] = S[lo:hi]
        m = np.concatenate([zero if t0 == 0 else mprev, mprev, mnext, zero if t0 + 1024 == T else mnext], 0)
        in_maps.append({
            "qx": cat(lat[:, 0:2048], ctile[:, 0:2048]),
            "kh": halo(slice(2048, 2560), 128, px[b], t0, t0 + 1024), "vh": halo(slice(2560, 3072), 128, px[b], t0, t0 + 1024),
            "kc": np.ascontiguousarray(ph[b][:, 2048:2560]), "vc": np.ascontiguousarray(ph[b][:, 2560:3072]),
            "rqc": np.ascontiguousarray(np.tile(C[t0:t0 + 1024], (1, 16))), "rqs": np.ascontiguousarray(np.tile(S[t0:t0 + 1024], (1, 16))),
            "rkc": np.ascontiguousarray(np.tile(Ck, (1, 4))), "rks": np.ascontiguousarray(np.tile(Sk, (1, 4))),
            "masks": np.ascontiguousarray(m), "sink": np.ascontiguousarray(P["attn_sink"]),
            "ug": cat(lat[:, 3072:5120], ctile[:, 3072:5120]), "cng": np.ascontiguousarray(P["cmlp_norm_g"]),
            "wsT": wsT, "bsT": bsT,
            "rkvp": cat(halo(slice(5120, 8192), 1, px[b], t0, t0 + 1024), halo(slice(5120, 8192), 1, ph[b], cj * 128, cj * 128 + 128)),
            "lora": cat(lat[:, 9216:9728], ctile[:, 9216:9728]), "convw": np.ascontiguousarray(P["rwkv_conv"]),
            "w2a2": w2a2, "w0a0": w0a0, "kkp": np.ascontiguousarray(P["rwkv_kk"]), "kap": np.ascontiguousarray(P["rwkv_ka"]),
            "identd": ident,
        })
    return in_maps


def l2_gather(res, cores=None):
    cores = list(range(NCORES)) if cores is None else cores
    attn_x = np.zeros((2, T, 2048), np.float32); attn_c = np.zeros((2, LCTX, 2048), np.float32)
    cm_x = np.zeros((2, T, CW), np.float32); cm_c = np.zeros((2, LCTX, CW), np.float32)
    sc_x = np.zeros((2, T, 10 * RW), np.float32); sc_c = np.zeros((2, LCTX, 10 * RW), np.float32)
    for r, i in zip(res.results, cores):
        b, t0, cj = tok_rows(i)
        for key, X, Cc in (("attn", attn_x, attn_c), ("cmlp", cm_x, cm_c), ("scin", sc_x, sc_c)):
            X[b, t0:t0 + 1024] = r[key][:1024]
            if (i % 4) < 2:
                Cc[b, cj * 128:(cj + 1) * 128] = r[key][1024:]
    return attn_x, attn_c, cm_x, cm_c, sc_x, sc_c


NS = LCTX + T
TC = 8


def build_l3(ns=NS, tc=TC, stage=9):
    k = KB()
    nchk = ns // tc
    RR = k.dram("RR", [nchk * 2, 4 * tc * 256], "ExternalInput")
    V2 = k.dram("V2", [nchk * 2, 4 * tc * 64], "ExternalInput")
    LL = k.dram("LL", [nchk * 128, 4 * tc * 4], "ExternalInput")
    WC = k.dram("WC", [nchk * 128, 4 * tc], "ExternalInput")
    Y = k.dram("Y", [nchk * 4 * 4, tc * 64], "ExternalOutput")
    rr = [k.sb([2, 4 * tc * 256], "rr%d" % i) for i in range(2)]
    v2 = [k.sb([2, 4 * tc * 64], "v2%d" % i) for i in range(2)]
    ll = [k.sb([128, 4 * tc * 4], "ll%d" % i) for i in range(2)]
    wc = [k.sb([128, 4 * tc], "wc%d" % i) for i in range(2)]
    y4 = [[k.sb([4, tc * 64], "y4_%d_%d" % (i, p)) for p in range(4)] for i in range(2)]
    ST = [k.sb([128, 64], "ST%d" % p) for p in range(4)]
    U = [[k.ps([128, 64], "U%d" % p)] * 2 for p in range(4)]
    P1 = [[k.ps([4, 64], "P%d" % p)] * 2 for p in range(4)]
    for p in range(4):
        k.op("dve", lambda e, p=p: e.memset(ST[p][:], 0.0), writes=[ST[p]])

    def load(c):
        s = c % 2
        k.dma("sp", rr[s][:], RR.t.ap()[c * 2:(c + 1) * 2, :], dst=rr[s])
        k.dma("sp", v2[s][:], V2.t.ap()[c * 2:(c + 1) * 2, :], dst=v2[s])
        k.dma("sp", ll[s][:], LL.t.ap()[c * 128:(c + 1) * 128, :], dst=ll[s])
        k.dma("sp", wc[s][:], WC.t.ap()[c * 128:(c + 1) * 128, :], dst=wc[s])

    load(0)
    for c in range(nchk):
        if c + 1 < nchk:
            load(c + 1)
        s = c % 2
        for tt in range(tc):
            t = c * tc + tt
            for p in range(4):
                u = U[p][t % 2]
                o = (p * tc + tt) * 256
                ov = (p * tc + tt) * 64
                if stage < 1:
                    continue
                k.op("pe", lambda e, u=u, o=o, ov=ov: e.matmul(u[:], lhsT=rr[s][0:2, o:o + 128], rhs=v2[s][0:2, ov:ov + 64],
                                                               start=True, stop=(t == 0)), reads=[rr[s], v2[s]], writes=[u])
                if t > 0 and stage >= 2:
                    yp, tp = (y4[s][p], tt - 1) if tt > 0 else (y4[1 - s][p], tc - 1)
                    k.op("pe", lambda e, u=u, o=o, yp=yp, tp=tp: e.matmul(u[:], lhsT=rr[s][0:2, o + 128:o + 256],
                                                                          rhs=yp[0:2, tp * 64:(tp + 1) * 64], start=False, stop=True),
                         reads=[rr[s], yp], writes=[u])
            for p in range(4):
                u = U[p][t % 2]
                if stage < 3:
                    continue
                k.op("dve", lambda e, p=p, u=u: e.scalar_tensor_tensor(out=ST[p][:], in0=ST[p][:],
                                                                       scalar=wc[s][:, p * tc + tt:p * tc + tt + 1], in1=u[:],
                                                                       op0=ALU.mult, op1=ALU.add), reads=[ST[p], wc[s], u], writes=[ST[p]])
            for p in range(4):
                pp = P1[p][t % 2]
                ol = (p * tc + tt) * 4
                if stage < 4:
                    if tt == 0:
                        k.op("dve", lambda e, p=p: e.memset(y4[s][p][:], 1.0), writes=[y4[s][p]])
                    continue
                k.op("pe", lambda e, p=p, pp=pp, ol=ol: e.matmul(pp[:], lhsT=ll[s][:, ol:ol + 4], rhs=ST[p][:], start=True, stop=True),
                     reads=[ll[s], ST[p]], writes=[pp])
                k.op("act", lambda e, p=p, pp=pp: e.copy(out=y4[s][p][:, tt * 64:(tt + 1) * 64], in_=pp[:]), reads=[pp], writes=[y4[s][p]])
        for p in range(4):
            k.dma("pool", Y.t.ap()[(c * 4 + p) * 4:(c * 4 + p + 1) * 4, :], y4[s][p][:], src=y4[s][p], final=True)
    k.finish()
    return k.nc


def scan_core(i):
    return i // 4, (i // 2) % 2, (i % 2) * 512


def l3_inmaps(sc_x, sc_c, ns=NS, tc=TC):
    nchk = ns // tc
    in_maps = []
    for i in range(NCORES):
        z, b, c0 = scan_core(i)
        seq = np.concatenate([sc_c[b][::-1] if z else sc_c[b], sc_x[b][::-1] if z else sc_x[b]], 0)[:ns]
        q = lambda qi: seq[:, qi * RW + c0:qi * RW + c0 + 512].reshape(ns, 4, 2, 64)
        r, nkk, v, w, bb, kr = q(0), q(1), q(2), q(3 + 3 * z), q(4 + 3 * z), q(5 + 3 * z)
        RRa = np.zeros((nchk, 2, 4, tc, 256), np.float32)
        V2a = np.zeros((nchk, 2, 4, tc, 64), np.float32)
        LLa = np.zeros((nchk, 2, 64, 4, tc, 4), np.float32)
        WCa = np.zeros((nchk, 2, 64, 4, tc), np.float32)
        c5 = lambda a: a.reshape(nchk, tc, 4, 2, 64)
        nkk_next = np.concatenate([nkk[1:], np.zeros_like(nkk[:1])], 0)
        for h in range(2):
            RRa[:, h, :, :, h * 64:(h + 1) * 64] = c5(kr)[:, :, :, h, :].transpose(0, 2, 1, 3)
            RRa[:, h, :, :, 128 + h * 64:128 + (h + 1) * 64] = c5(bb)[:, :, :, h, :].transpose(0, 2, 1, 3)
            V2a[:, h] = c5(v)[:, :, :, h, :].transpose(0, 2, 1, 3)
            LLa[:, h, :, :, :, h] = c5(nkk_next)[:, :, :, h, :].transpose(0, 3, 2, 1)
            LLa[:, h, :, :, :, 2 + h] = c5(r)[:, :, :, h, :].transpose(0, 3, 2, 1)
            WCa[:, h] = c5(w)[:, :, :, h, :].transpose(0, 3, 2, 1)
        in_maps.append({"RR": RRa.reshape(nchk * 2, -1), "V2": V2a.reshape(nchk * 2, -1),
                        "LL": LLa.reshape(nchk * 128, -1), "WC": WCa.reshape(nchk * 128, -1)})
    return in_maps


def l3_gather(res, ns=NS, tc=TC, cores=None):
    cores = list(range(NCORES)) if cores is None else cores
    nchk = ns // tc
    y = np.zeros((2, 2, ns, RW), np.float32)
    for rs, i in zip(res.results, cores):
        z, b, c0 = scan_core(i)
        Ya = rs["Y"].reshape(nchk, 4, 4, tc, 64)
        yy = Ya[:, :, 2:4].transpose(0, 3, 1, 2, 4).reshape(ns, 512)
        if z:
            yy = np.concatenate([yy[:LCTX][::-1], yy[LCTX:][::-1]], 0) if ns == NS else yy[::-1]
        y[z, b, :, c0:c0 + 512] = yy
    return y


GROUPS2 = [(0, 1), (2, 3), (4, 5), (6, 7), (8,)]


def build_l4a():
    k = KB()
    di = lambda n, s: k.dram(n, s, "ExternalInput")
    y01 = di("y01", [NTOK, 2 * RW]); rkv = di("rkv", [NTOK, 3 * RW]); gt = di("gt", [NTOK, RW])
    attn = di("attn", [NTOK, 2048]); cmlp = di("cmlp", [NTOK, CW]); xin = di("xin", [NTOK, D])
    prm = di("prm", [3, RW]); g1row = di("g1row", [2, D]); Wo = di("Wo", [D, D]); identd = di("identd", [128, 128])
    x1 = k.dram("x1", [NTOK, D], "ExternalOutput")
    ident = k.sb([128, 128], "ident")
    k.dma("sp", ident[:], identd.t.ap(), dst=ident)
    pt = k.sb([128, 3 * RW], "pt")
    for j in range(3):
        k.dma("sp", pt[:, j * RW:(j + 1) * RW], prm.t.ap()[j, :].partition_broadcast(128), dst=pt)
    mix = k.sb([128, D], "mix")
    mixT = k.sb([128, 2 * NCH * 128], "mixT")
    BW = 256
    wb = [k.sb([128, NCH, BW], "wb%d" % i) for i in range(2)]
    yt = k.sb([128, 2 * RW], "yt"); rt = k.sb([128, 3 * RW], "rt"); gg = k.sb([128, RW], "gg")
    s1 = k.sb([128, RW], "s1"); s2 = k.sb([128, RW], "s2")
    st = k.sb([128, 128], "st")
    gb = [k.sb([128, BW], "gb%d" % i) for i in range(2)]
    xb = [k.sb([128, BW], "xb%d" % i) for i in range(3)]
    pst = [k.ps([128, 512], "pst%d" % i) for i in range(2)]
    pso = [k.ps([128, BW], "pso%d" % i) for i in range(3)]
    Wv = Wo.t.ap().rearrange("(c p) n -> p c n", p=128)
    it = 0
    for grp in GROUPS2:
        ng = len(grp)
        for j, ti in enumerate(grp):
            rows = slice(ti * 128, (ti + 1) * 128)
            k.dma("sp", mix[:, 0:2048], attn.t.ap()[rows, :], dst=mix)
            k.dma("sp", mix[:, 2048:3072], cmlp.t.ap()[rows, :], dst=mix)
            k.dma("sp", yt[:], y01.t.ap()[rows, :], dst=yt)
            k.dma("sp", rt[:], rkv.t.ap()[rows, :], dst=rt)
            k.dma("sp", gg[:], gt.t.ap()[rows, :], dst=gg)
            k.op("dve", lambda e: e.tensor_tensor(out=s1[:], in0=yt[:, 0:RW], in1=yt[:, RW:2 * RW], op=ALU.add), reads=[yt], writes=[s1])
            k.op("dve", lambda e: e.tensor_reduce(out=st[:, 0:16], in_=s1[:].rearrange("p (h n) -> p h n", n=64), axis=AX.X, op=ALU.add),
                 reads=[s1], writes=[st])
            k.op("dve", lambda e: e.tensor_tensor(out=s2[:], in0=s1[:], in1=s1[:], op=ALU.mult), reads=[s1], writes=[s2])
            k.op("dve", lambda e: e.tensor_reduce(out=st[:, 16:32], in_=s2[:].rearrange("p (h n) -> p h n", n=64), axis=AX.X, op=ALU.add),
                 reads=[s2], writes=[st])
            k.op("dve", lambda e: e.tensor_scalar(out=st[:, 0:32], in0=st[:, 0:32], scalar1=1.0 / 64, scalar2=None, op0=ALU.mult),
                 reads=[st], writes=[st])
            k.op("dve", lambda e: e.tensor_tensor(out=st[:, 32:48], in0=st[:, 0:16], in1=st[:, 0:16], op=ALU.mult), reads=[st], writes=[st])
            k.op("dve", lambda e: e.tensor_tensor(out=st[:, 48:64], in0=st[:, 16:32], in1=st[:, 32:48], op=ALU.subtract), reads=[st], writes=[st])
            k.op("dve", lambda e: e.tensor_scalar(out=st[:, 48:64], in0=st[:, 48:64], scalar1=64e-5, scalar2=None, op0=ALU.add),
                 reads=[st], writes=[st])
            k.op("act", lambda e: e.activation(out=st[:, 64:80], in_=st[:, 48:64], func=AF.Sqrt), reads=[st], writes=[st])
            k.op("dve", lambda e: e.reciprocal(out=st[:, 80:96], in_=st[:, 64:80]), reads=[st], writes=[st])
            for h in range(16):
                k.op("dve", lambda e, h=h: e.tensor_scalar(out=s1[:, h * 64:(h + 1) * 64], in0=s1[:, h * 64:(h + 1) * 64],
                                                           scalar1=st[:, h:h + 1], scalar2=st[:, 80 + h:81 + h], op0=ALU.subtract,
                                                           op1=ALU.mult), reads=[s1, st], writes=[s1])
            k.op("dve", lambda e: e.tensor_tensor(out=s1[:], in0=s1[:], in1=pt[:, RW:2 * RW], op=ALU.mult), reads=[s1, pt], writes=[s1])
            k.op("dve", lambda e: e.tensor_tensor(out=s1[:], in0=s1[:], in1=pt[:, 2 * RW:3 * RW], op=ALU.add), reads=[s1, pt], writes=[s1])
            k.op("dve", lambda e: e.tensor_tensor(out=s2[:], in0=rt[:, 0:RW], in1=rt[:, RW:2 * RW], op=ALU.mult), reads=[rt], writes=[s2])
            k.op("dve", lambda e: e.tensor_tensor(out=s2[:], in0=s2[:], in1=pt[:, 0:RW], op=ALU.mult), reads=[s2, pt], writes=[s2])
            k.op("dve", lambda e: e.tensor_reduce(out=st[:, 96:112], in_=s2[:].rearrange("p (h n) -> p h n", n=64), axis=AX.X, op=ALU.add),
                 reads=[s2], writes=[st])
            for h in range(16):
                k.op("dve", lambda e, h=h: e.scalar_tensor_tensor(out=s1[:, h * 64:(h + 1) * 64], in0=rt[:, 2 * RW + h * 64:2 * RW + (h + 1) * 64],
                                                                  scalar=st[:, 96 + h:97 + h], in1=s1[:, h * 64:(h + 1) * 64],
                                                                  op0=ALU.mult, op1=ALU.add), reads=[rt, st, s1], writes=[s1])
            k.op("act", lambda e: e.activation(out=gg[:], in_=gg[:], func=AF.Sigmoid), reads=[gg], writes=[gg])
            k.op("dve", lambda e: e.tensor_tensor(out=mix[:, 3072:4096], in0=s1[:], in1=gg[:], op=ALU.mult), reads=[s1, gg], writes=[mix])
            transpose_tile(k, mix, ident, pst, lambda c, j=j: mixT[:, (c * 2 + j) * 128:(c * 2 + j + 1) * 128], mixT)
        for blk in range(D // BW):
            w = wb[blk % 2]
            cs = slice(blk * BW, (blk + 1) * BW)
            for q4 in range(4):
                k.dma("sp", w[:, q4 * 8:(q4 + 1) * 8, :], Wv[:, q4 * 8:(q4 + 1) * 8, cs], dst=w)
            sets = sorted(set(0 if ti < 8 else 1 for ti in grp))
            for s_ in sets:
                k.dma("sp", gb[s_][:], g1row.t.ap()[s_, cs].partition_broadcast(128), dst=gb[s_])
            for j, ti in enumerate(grp):
                p = pso[it % 3]
                xx = xb[it % 3]
                it += 1
                k.dma("sp", xx[:], xin.t.ap()[ti * 128:(ti + 1) * 128, cs], dst=xx)
                for c in range(NCH):
                    k.op("pe", lambda e, c=c, j=j, p=p: e.matmul(p[:], lhsT=mixT[:, (c * 2 + j) * 128:(c * 2 + j + 1) * 128], rhs=w[:, c, :],
                                                                 start=(c == 0), stop=(c == NCH - 1)), reads=[mixT, w], writes=[p])
                g_ = gb[0 if ti < 8 else 1]
                k.op("dve", lambda e, p=p, g_=g_: e.tensor_tensor(out=p[:], in0=p[:], in1=g_[:], op=ALU.mult), reads=[p, g_], writes=[p])
                k.op("dve", lambda e, p=p, xx=xx: e.tensor_tensor(out=xx[:], in0=xx[:], in1=p[:], op=ALU.add), reads=[p, xx], writes=[xx])
                k.dma("pool", x1.t.ap()[ti * 128:(ti + 1) * 128, cs], xx[:], src=xx, final=True)
    k.finish()
    return k.nc


def l4a_inmaps(y, sc_x, sc_c, px, ph, attn_x, attn_c, cm_x, cm_c, x, h, mod_l, P):
    ident = np.eye(128, dtype=np.float32)
    prm = np.ascontiguousarray(np.stack([P["rwkv_rk"], P["rwkv_ln_w"], P["rwkv_ln_b"]]))
    in_maps = []
    for i in range(NCORES):
        b, t0, cj = tok_rows(i)
        cat = lambda a, c: np.ascontiguousarray(np.concatenate([a, c], 0))
        lat = slice(t0, t0 + 1024); ct = slice(cj * 128, (cj + 1) * 128)
        yl = np.concatenate([y[0, b, LCTX:][lat], y[1, b, LCTX:][lat]], 1)
        yc = np.concatenate([y[0, b, :LCTX][ct], y[1, b, :LCTX][ct]], 1)
        sel = lambda a: np.concatenate([a[:, 0:RW], a[:, 9 * RW:10 * RW], a[:, 2 * RW:3 * RW]], 1)
        in_maps.append({
            "y01": cat(yl, yc), "rkv": cat(sel(sc_x[b][lat]), sel(sc_c[b][ct])),
            "gt": cat(px[b][lat, 8192:9216], ph[b][ct, 8192:9216]),
            "attn": cat(attn_x[b][lat], attn_c[b][ct]), "cmlp": cat(cm_x[b][lat], cm_c[b][ct]),
            "xin": cat(x[b][lat], h[b][ct]), "prm": prm,
            "g1row": np.ascontiguousarray(np.stack([mod_l[b, 2 * D:3 * D], mod_l[2, 2 * D:3 * D]])),
            "Wo": P["w_out"], "identd": ident})
    return in_maps


def tok_gather(res, key, width, cores=None):
    cores = list(range(NCORES)) if cores is None else cores
    X = np.zeros((2, T, width), np.float32); Hc = np.zeros((2, LCTX, width), np.float32)
    for r, i in zip(res.results, cores):
        b, t0, cj = tok_rows(i)
        X[b, t0:t0 + 1024] = r[key][:1024]
        if (i % 4) < 2:
            Hc[b, cj * 128:(cj + 1) * 128] = r[key][1024:]
    return X, Hc


NE = 16
FF = 1024


def build_l4b(final=False, n_exp=NE):
    k = KB()
    di = lambda n, s: k.dram(n, s, "ExternalInput")
    x1 = di("x1", [NTOK, D]); modcol = di("modcol", [128, 5 * NCH]); g2row = di("g2row", [2, D])
    rwc = di("rwc", [128, NCH * NE]); rb = di("rb", [NE]); fg = di("fg", [D]); identd = di("identd", [128, 128])
    W1 = di("W1", [NE * D, FF]); W3 = di("W3", [NE * D, FF]); W2 = di("W2", [NE * FF, D])
    x2 = k.dram("x2", [NTOK, D], "ExternalOutput")
    xf = k.dram("xf", [NTOK, D], "ExternalOutput") if final else None
    ident = k.sb([128, 128], "ident")
    k.dma("sp", ident[:], identd.t.ap(), dst=ident)
    mc = k.sb([128, 5 * NCH], "mc"); gs = k.sb([128, 4 * NCH], "gs")
    k.dma("sp", mc[:], modcol.t.ap(), dst=mc)
    g = mc[:, 0:NCH]
    for s in range(2):
        sc = mc[:, (1 + 2 * s) * NCH:(2 + 2 * s) * NCH]
        sh = mc[:, (2 + 2 * s) * NCH:(3 + 2 * s) * NCH]
        k.op("dve", lambda e, s=s, sc=sc: e.tensor_tensor(out=gs[:, 2 * s * NCH:(2 * s + 1) * NCH], in0=g, in1=sc, op=ALU.mult),
             reads=[mc], writes=[gs])
        k.op("dve", lambda e, s=s: e.tensor_tensor(out=gs[:, 2 * s * NCH:(2 * s + 1) * NCH], in0=gs[:, 2 * s * NCH:(2 * s + 1) * NCH],
                                                   in1=g, op=ALU.add), reads=[mc, gs], writes=[gs])
        k.op("dve", lambda e, s=s, sh=sh: e.tensor_copy(out=gs[:, (2 * s + 1) * NCH:(2 * s + 2) * NCH], in_=sh), reads=[mc], writes=[gs])
    rw = k.sb([128, NCH * NE], "rw"); rbt = k.sb([128, NE], "rbt")
    k.dma("sp", rw[:], rwc.t.ap(), dst=rw)
    k.dma("sp", rbt[:], rb.t.ap().partition_broadcast(128), dst=rbt)
    xt = k.sb([128, D], "xt"); sq = k.sb([128, D], "sq"); st = k.sb([128, 4], "st")
    znT = k.sb([128, NCH * 256], "znT")
    acc = [k.sb([128, D], "acc%d" % j) for j in range(2)]
    wh = [k.sb([128, 16, 128], "wh%d" % i) for i in range(4)]
    w2s = [k.sb([128, 8, 256], "w2s%d" % i) for i in range(2)]
    hidT = k.sb([128, 8 * 256], "hidT"); sl = k.sb([128, 256], "sl")
    R = [k.sb([128, 160], "R%d" % j) for j in range(2)]
    pst = [k.ps([128, 512], "pst%d" % i) for i in range(2)]
    pH = [k.ps([128, 256], "pH%d" % i) for i in range(2)]
    po = [k.ps([128, 256], "po%d" % i) for i in range(2)]
    pr = k.ps([128, NE], "pr")

    def route(r, j):
        o = lambda fn, rd=(), wr=(): k.op("dve", fn, reads=[r] + list(rd), writes=[r] + list(wr))
        k.op("act", lambda e: e.activation(out=r[:, 0:16], in_=pr[:], func=AF.Sigmoid), reads=[pr], writes=[r])
        o(lambda e: e.tensor_tensor(out=r[:, 16:32], in0=r[:, 0:16], in1=rbt[:], op=ALU.add), rd=[rbt])
        sel3 = r[:, 16:32].rearrange("p (g e) -> p g e", e=4)
        ps6 = r[:, 32:56].rearrange("p (g s) -> p g s", s=6)
        idx = 0
        for a in range(4):
            for b in range(a + 1, 4):
                o(lambda e, a=a, b=b, idx=idx: e.tensor_tensor(out=ps6[:, :, idx], in0=sel3[:, :, a], in1=sel3[:, :, b], op=ALU.add))
                idx += 1
        o(lambda e: e.tensor_reduce(out=r[:, 56:60], in_=ps6, axis=AX.X, op=ALU.max))
        o(lambda e: e.tensor_reduce(out=r[:, 60:61], in_=r[:, 56:60], axis=AX.X, op=ALU.max))
        o(lambda e: e.tensor_scalar(out=r[:, 61:65], in0=r[:, 56:60], scalar1=r[:, 60:61], scalar2=None, op0=ALU.is_equal))
        o(lambda e: e.tensor_reduce(out=r[:, 65:69], in_=sel3, axis=AX.X, op=ALU.max))
        for gi in range(4):
            o(lambda e, gi=gi: e.tensor_scalar(out=r[:, 69 + gi * 4:73 + gi * 4], in0=r[:, 16 + gi * 4:20 + gi * 4],
                                               scalar1=r[:, 65 + gi:66 + gi], scalar2=None, op0=ALU.is_equal))
        o(lambda e: e.scalar_tensor_tensor(out=r[:, 85:101], in0=r[:, 69:85], scalar=-1e30, in1=r[:, 16:32], op0=ALU.mult, op1=ALU.add))
        o(lambda e: e.tensor_reduce(out=r[:, 101:105], in_=r[:, 85:101].rearrange("p (g e) -> p g e", e=4), axis=AX.X, op=ALU.max))
        for gi in range(4):
            o(lambda e, gi=gi: e.tensor_scalar(out=r[:, 105 + gi * 4:109 + gi * 4], in0=r[:, 85 + gi * 4:89 + gi * 4],
                                               scalar1=r[:, 101 + gi:102 + gi], scalar2=None, op0=ALU.is_equal))
        o(lambda e: e.tensor_tensor(out=r[:, 105:121], in0=r[:, 105:121], in1=r[:, 69:85], op=ALU.add))
        for gi in range(4):
            o(lambda e, gi=gi: e.tensor_scalar(out=r[:, 105 + gi * 4:109 + gi * 4], in0=r[:, 105 + gi * 4:109 + gi * 4],
                                               scalar1=r[:, 61 + gi:62 + gi], scalar2=None, op0=ALU.mult))
        o(lambda e: e.tensor_tensor(out=r[:, 121:137], in0=r[:, 105:121], in1=r[:, 0:16], op=ALU.mult))
        o(lambda e: e.tensor_reduce(out=r[:, 137:138], in_=r[:, 121:137], axis=AX.X, op=ALU.add))
        o(lambda e: e.reciprocal(out=r[:, 138:139], in_=r[:, 137:138]))
        o(lambda e: e.tensor_scalar(out=r[:, 139:155], in0=r[:, 121:137], scalar1=r[:, 138:139], scalar2=None, op0=ALU.mult))

    for grp in GROUPS2:
        ng = len(grp)
        ntok = ng * 128
        for j, ti in enumerate(grp):
            k.dma("sp", xt[:], x1.t.ap()[ti * 128:(ti + 1) * 128, :], dst=xt)
            rms_scale(k, xt, sq, st, D)
            s = 0 if ti < 8 else 1
            transpose_tile(k, xt, ident, pst, lambda c, j=j: znT[:, (c * 2 + j) * 128:(c * 2 + j + 1) * 128], znT,
                           gcol=gs[:, 2 * s * NCH:(2 * s + 1) * NCH], scol=gs[:, (2 * s + 1) * NCH:(2 * s + 2) * NCH], mbuf=gs)
            for c in range(NCH):
                k.op("pe", lambda e, c=c, j=j: e.matmul(pr[:], lhsT=znT[:, (c * 2 + j) * 128:(c * 2 + j + 1) * 128],
                                                        rhs=rw[:, c * NE:(c + 1) * NE], start=(c == 0), stop=(c == NCH - 1)),
                     reads=[znT, rw], writes=[pr])
            route(R[j], j)
            k.op("dve", lambda e, j=j: e.memset(acc[j][:], 0.0), writes=[acc[j]])
        for ex in range(n_exp):
            for fc in range(8):
                for wi, Wd in ((0, W1), (2, W3)):
                    for hf in range(2):
                        wbuf = wh[wi + hf]
                        src = Wd.t.ap()[ex * D + hf * 2048:ex * D + (hf + 1) * 2048, :].rearrange("(c p) n -> p c n", p=128)
                        for q2 in range(2):
                            k.dma("sp", wbuf[:, q2 * 8:(q2 + 1) * 8, :], src[:, q2 * 8:(q2 + 1) * 8, fc * 128:(fc + 1) * 128], dst=wbuf)
                    p = pH[wi // 2]
                    for c in range(NCH):
                        k.op("pe", lambda e, c=c, p=p, wi=wi: e.matmul(p[:, 0:ntok], lhsT=wh[wi + c // 16][:, c % 16, :],
                                                                       rhs=znT[:, c * 256:c * 256 + ntok], start=(c == 0),
                                                                       stop=(c == NCH - 1)), reads=[wh[wi + c // 16], znT], writes=[p])
                k.op("act", lambda e: e.activation(out=sl[:, 0:ntok], in_=pH[0][:, 0:ntok], func=AF.Silu), reads=[pH[0]], writes=[sl])
                k.op("dve", lambda e, fc=fc: e.tensor_tensor(out=hidT[:, fc * 256:fc * 256 + ntok], in0=sl[:, 0:ntok], in1=pH[1][:, 0:ntok],
                                                             op=ALU.mult), reads=[sl, pH[1]], writes=[hidT])
            for dblk in range(D // 256):
                w2 = w2s[dblk % 2]
                src = W2.t.ap()[ex * FF:(ex + 1) * FF, :].rearrange("(f p) n -> p f n", p=128)
                k.dma("sp", w2[:], src[:, :, dblk * 256:(dblk + 1) * 256], dst=w2)
                for j in range(ng):
                    p = po[j]
                    for fc in range(8):
                        k.op("pe", lambda e, fc=fc, p=p, j=j: e.matmul(p[:], lhsT=hidT[:, fc * 256 + j * 128:fc * 256 + (j + 1) * 128],
                                                                       rhs=w2[:, fc, :], start=(fc == 0), stop=(fc == 7)),
                             reads=[hidT, w2], writes=[p])
                    k.op("dve", lambda e, p=p, j=j, dblk=dblk: e.scalar_tensor_tensor(
                        out=acc[j][:, dblk * 256:(dblk + 1) * 256], in0=p[:], scalar=R[j][:, 139 + ex:140 + ex],
                        in1=acc[j][:, dblk * 256:(dblk + 1) * 256], op0=ALU.mult, op1=ALU.add), reads=[p, R[j], acc[j]], writes=[acc[j]])
        for j, ti in enumerate(grp):
            rows = slice(ti * 128, (ti + 1) * 128)
            k.dma("sp", xt[:], x1.t.ap()[rows, :], dst=xt)
            k.dma("sp", sq[:], g2row.t.ap()[0 if ti < 8 else 1, :].partition_broadcast(128), dst=sq)
            k.op("dve", lambda e, j=j: e.tensor_tensor(out=acc[j][:], in0=acc[j][:], in1=sq[:], op=ALU.mult), reads=[acc[j], sq], writes=[acc[j]])
            k.op("dve", lambda e, j=j: e.tensor_tensor(out=acc[j][:], in0=acc[j][:], in1=xt[:], op=ALU.add), reads=[acc[j], xt], writes=[acc[j]])
            k.dma("pool", x2.t.ap()[rows, :], acc[j][:], src=acc[j], final=True)
            if final:
                k.dma("sp", xt[:], fg.t.ap().partition_broadcast(128), dst=xt)
                rms_scale(k, acc[j], sq, st, D)
                k.op("dve", lambda e, j=j: e.tensor_tensor(out=acc[j][:], in0=acc[j][:], in1=xt[:], op=ALU.mult), reads=[acc[j], xt],
                     writes=[acc[j]])
                k.dma("pool", xf.t.ap()[rows, :], acc[j][:], src=acc[j], final=True)
    k.finish()
    return k.nc


def l4b_inmaps(x1x, x1c, mod_l, norm2_g_l, router_w, router_b, w1, w3, w2, final_g):
    ident = np.eye(128, dtype=np.float32)
    rwc = np.ascontiguousarray(router_w.reshape(NCH, 128, NE).transpose(1, 0, 2).reshape(128, NCH * NE))
    W1 = w1.reshape(NE * D, FF); W3 = w3.reshape(NE * D, FF); W2 = w2.reshape(NE * FF, D)
    in_maps = []
    for i in range(NCORES):
        b, t0, cj = tok_rows(i)
        mc = np.concatenate([col_layout(norm2_g_l), col_layout(mod_l[b, 4 * D:5 * D]), col_layout(mod_l[b, 3 * D:4 * D]),
                             col_layout(mod_l[2, 4 * D:5 * D]), col_layout(mod_l[2, 3 * D:4 * D])], 1)
        in_maps.append({"x1": np.ascontiguousarray(np.concatenate([x1x[b, t0:t0 + 1024], x1c[b, cj * 128:(cj + 1) * 128]], 0)),
                        "modcol": np.ascontiguousarray(mc),
                        "g2row": np.ascontiguousarray(np.stack([mod_l[b, 5 * D:6 * D], mod_l[2, 5 * D:6 * D]])),
                        "rwc": rwc, "rb": np.ascontiguousarray(router_b), "fg": np.ascontiguousarray(final_g), "identd": ident,
                        "W1": W1, "W3": W3, "W2": W2})
    return in_maps


_ALL = list(range(NCORES))


def _run(nc, in_maps):
    return run_bass_kernel_spmd(nc, in_maps, core_ids=_ALL)


def kernel(x, c, ctx, c_ctx, ada_w, ada_b, norm1_g, w_in, rwkv_conv, attn_sink, cmlp_norm_g, cmlp_ws, cmlp_b,
           rwkv_w0, rwkv_w1, rwkv_w2, rwkv_a0, rwkv_a1, rwkv_a2, rwkv_kk, rwkv_ka, rwkv_rk, rwkv_ln_w, rwkv_ln_b,
           w_out, norm2_g, router_w, router_b, moe_w1, moe_w3, moe_w2, final_g):
    f = lambda a: np.ascontiguousarray(np.asarray(a, dtype=np.float32))
    x, h = f(x), f(ctx)
    mod = run_ada(f(c), f(c_ctx), np.asarray(ada_w), np.asarray(ada_b))
    xf = None
    for l in range(2):
        P = dict(attn_sink=f(attn_sink[l]), cmlp_norm_g=f(cmlp_norm_g[l]), cmlp_ws=f(cmlp_ws[l]), cmlp_b=f(cmlp_b[l]),
                 rwkv_conv=f(rwkv_conv[l]), rwkv_w0=f(rwkv_w0[l]), rwkv_w2=f(rwkv_w2[l]), rwkv_a0=f(rwkv_a0[l]),
                 rwkv_a2=f(rwkv_a2[l]), rwkv_kk=f(rwkv_kk[l]), rwkv_ka=f(rwkv_ka[l]), rwkv_rk=f(rwkv_rk[l]),
                 rwkv_ln_w=f(rwkv_ln_w[l]), rwkv_ln_b=f(rwkv_ln_b[l]), w_out=f(w_out[l]))
        Wcat = np.ascontiguousarray(np.concatenate([w_in[l], rwkv_w1[l][0], rwkv_w1[l][1], rwkv_a1[l][0], rwkv_a1[l][1]], 1))
        px, ph = run_l1(x, h, mod[l], f(norm1_g[l]), Wcat)
        del Wcat
        attn_x, attn_c, cm_x, cm_c, sc_x, sc_c = l2_gather(_run(build_l2(), l2_inmaps(px, ph, P)))
        y = l3_gather(_run(build_l3(), l3_inmaps(sc_x, sc_c)))
        x1x, x1c = tok_gather(_run(build_l4a(), l4a_inmaps(y, sc_x, sc_c, px, ph, attn_x, attn_c, cm_x, cm_c, x, h, mod[l], P)),
                              "x1", D)
        del px, ph, attn_x, attn_c, cm_x, cm_c, sc_x, sc_c, y
        last = l == 1
        res = _run(build_l4b(final=last), l4b_inmaps(x1x, x1c, mod[l], f(norm2_g[l]), f(router_w), f(router_b),
                                                     f(moe_w1[l]), f(moe_w3[l]), f(moe_w2[l]), f(final_g)))
        x, h = tok_gather(res, "x2", D)
        if last:
            xf, _ = tok_gather(res, "xf", D)
    return xf
```

```python
import numpy as np
import concourse.bass as bass
import concourse.mybir as mybir
from concourse.bass_utils import run_bass_kernel_spmd

F32 = mybir.dt.float32
AF = mybir.ActivationFunctionType
ALU = mybir.AluOpType
AX = mybir.AxisListType

NCORES = 8
F32R = mybir.dt.float32r
MM_R = [1]


def mmr(e, out, lhsT, rhs, start, stop):
    return e.matmul(out, lhsT=lhsT, rhs=rhs, start=start, stop=stop)


BF16 = mybir.dt.bfloat16


def mdt():
    return {False: F32, True: F32R, 1: F32R, 2: BF16}[MM_R[0]]
D = 4096
T = 4096
LCTX = 256
DPROJ = 9216
NCH = D // 128


class Buf:
    __slots__ = ("name", "t", "w", "r", "dsem", "dcnt")

    def __init__(self, name, t):
        self.name = name
        self.t = t
        self.w = None
        self.r = {}
        self.dsem = None
        self.dcnt = 0

    def __getitem__(self, idx):
        return self.t[idx]


class KB:
    def __init__(self):
        self.nc = bass.Bass("TRN2", target_bir_lowering=False)
        nc = self.nc
        self.E = {"pe": nc.tensor, "dve": nc.vector, "act": nc.scalar, "pool": nc.gpsimd, "sp": nc.sync}
        self.sem = {e: nc.alloc_semaphore("c_" + e) for e in self.E}
        self.cnt = {e: 0 for e in self.E}
        self.seen = {e: {} for e in self.E}
        self.nb = 0
        self.final = {}

    def sb(self, shape, name=None, dtype=F32):
        self.nb += 1
        name = name or "sb%d" % self.nb
        return Buf(name, self.nc.alloc_sbuf_tensor(name, list(shape), dtype))

    def ps(self, shape, name=None, dtype=F32):
        self.nb += 1
        name = name or "ps%d" % self.nb
        return Buf(name, self.nc.alloc_psum_tensor(name, list(shape), dtype))

    def dram(self, name, shape, kind, dtype=F32):
        return Buf(name, self.nc.dram_tensor(name, list(shape), dtype, kind=kind))

    def _wait(self, eng, deps):
        for k, v in deps.items():
            if self.seen[eng].get(k, 0) < v:
                self.E[eng].wait_ge(self.sem[k], v)
                self.seen[eng][k] = v

    @staticmethod
    def _add(deps, d):
        if d is not None:
            k, v = d
            if deps.get(k, 0) < v:
                deps[k] = v

    def op(self, eng, fn, reads=(), writes=()):
        deps = {}
        for b in reads:
            self._add(deps, b.w)
        for b in writes:
            self._add(deps, b.w)
            for k, v in b.r.items():
                self._add(deps, (k, v))
        if eng == "pe":
            deps.pop("pe", None)
        self._wait(eng, deps)
        inst = fn(self.E[eng])
        self.cnt[eng] += 1
        c = self.cnt[eng]
        inst.then_inc(self.sem[eng], 1)
        for b in reads:
            b.r[eng] = c
        for b in writes:
            b.w = (eng, c)
            b.r = {}
        return inst

    def dma(self, q, out, in_, src=None, dst=None, owner=None, final=False, **kw):
        owner = owner or (dst if dst is not None else src)
        if owner.dsem is None:
            owner.dsem = "d_" + owner.name
            self.sem[owner.dsem] = self.nc.alloc_semaphore(owner.dsem)
        deps = {}
        if src is not None:
            self._add(deps, src.w)
        if dst is not None:
            self._add(deps, dst.w)
            for k, v in dst.r.items():
                self._add(deps, (k, v))
        self._wait(q, deps)
        inst = self.E[q].dma_start(out=out, in_=in_, **kw)
        owner.dcnt += 16
        inst.then_inc(self.sem[owner.dsem], 16)
        if src is not None:
            src.r[owner.dsem] = owner.dcnt
        if dst is not None:
            dst.w = (owner.dsem, owner.dcnt)
            dst.r = {}
        if final:
            self.final[owner.dsem] = owner.dcnt
        return inst

    def finish(self):
        self._wait("sp", dict(self.final))


ADA_COLS = 2 * 6 * D // NCORES


def build_ada():
    k = KB()
    ccT = k.dram("ccT", [128, NCH * 3], "ExternalInput")
    W = k.dram("W", [D, ADA_COLS], "ExternalInput")
    bias = k.dram("bias", [ADA_COLS], "ExternalInput")
    out = k.dram("out", [3, ADA_COLS], "ExternalOutput")
    cc = k.sb([128, NCH * 3], "cc")
    bt = k.sb([3, ADA_COLS], "bt")
    ot = k.sb([3, ADA_COLS], "ot")
    wb = [k.sb([128, NCH, 512], "wb%d" % i) for i in range(2)]
    pb = [k.ps([3, 512], "pb%d" % i) for i in range(2)]
    k.dma("sp", cc[:], ccT.t.ap(), dst=cc)
    k.dma("sp", bt[:], bias.t.ap().partition_broadcast(3), dst=bt)
    k.op("act", lambda e: e.activation(out=cc[:], in_=cc[:], func=AF.Silu), reads=[cc], writes=[cc])
    Wv = W.t.ap().rearrange("(c p) n -> p c n", p=128)
    ncb = ADA_COLS // 512
    for cb in range(ncb):
        w = wb[cb % 2]
        for q4 in range(4):
            k.dma("sp", w[:, q4 * 8:(q4 + 1) * 8, :], Wv[:, q4 * 8:(q4 + 1) * 8, cb * 512:(cb + 1) * 512], dst=w)
        p = pb[cb % 2]
        for c in range(NCH):
            k.op("pe", lambda e, c=c: e.matmul(p[:], lhsT=cc[:, c * 3:(c + 1) * 3], rhs=w[:, c, :],
                                                start=(c == 0), stop=(c == NCH - 1)),
                 reads=[cc, w], writes=[p])
        k.op("dve", lambda e: e.tensor_tensor(out=ot[:, cb * 512:(cb + 1) * 512], in0=p[:],
                                              in1=bt[:, cb * 512:(cb + 1) * 512], op=ALU.add),
             reads=[p, bt], writes=[ot])
    k.dma("sp", out.t.ap(), ot[:], src=ot, final=True)
    k.finish()
    return k.nc


def run_ada(c, c_ctx, ada_w, ada_b):
    cc = np.concatenate([c, c_ctx[None]], 0)
    ccT = np.ascontiguousarray(cc.T.reshape(NCH, 128, 3).transpose(1, 0, 2).reshape(128, NCH * 3))
    per = 6 * D // NCORES
    in_maps = []
    for i in range(NCORES):
        Wc = np.ascontiguousarray(np.concatenate([ada_w[l][:, i * per:(i + 1) * per] for l in range(2)], 1))
        bc = np.ascontiguousarray(np.concatenate([ada_b[l][i * per:(i + 1) * per] for l in range(2)], 0))
        in_maps.append({"ccT": ccT, "W": Wc, "bias": bc})
    res = run_bass_kernel_spmd(build_ada(), in_maps, core_ids=list(range(NCORES)))
    mod = np.zeros((2, 3, 6 * D), np.float32)
    for i in range(NCORES):
        o = res.results[i]["out"]
        for l in range(2):
            mod[l][:, i * per:(i + 1) * per] = o[:, l * per:(l + 1) * per]
    return mod


NT = 9
NTOK = NT * 128


def rms_scale(k, xt, sq, st, width):
    k.op("dve", lambda e: e.tensor_tensor(out=sq[:, :width], in0=xt[:, :width], in1=xt[:, :width], op=ALU.mult),
         reads=[xt], writes=[sq])
    k.op("dve", lambda e: e.tensor_reduce(out=st[:, 0:1], in_=sq[:, :width], axis=AX.X, op=ALU.add),
         reads=[sq], writes=[st])
    k.op("dve", lambda e: e.tensor_scalar(out=st[:, 1:2], in0=st[:, 0:1], scalar1=1.0 / width, scalar2=1e-6,
                                          op0=ALU.mult, op1=ALU.add), reads=[st], writes=[st])
    k.op("act", lambda e: e.activation(out=st[:, 2:3], in_=st[:, 1:2], func=AF.Sqrt), reads=[st], writes=[st])
    k.op("dve", lambda e: e.reciprocal(out=st[:, 3:4], in_=st[:, 2:3]), reads=[st], writes=[st])
    k.op("dve", lambda e: e.tensor_scalar(out=xt[:, :width], in0=xt[:, :width], scalar1=st[:, 3:4], scalar2=None,
                                          op0=ALU.mult), reads=[xt, st], writes=[xt])


def transpose_tile(k, xt, ident, pst, xT_dst, dst_buf, gcol=None, scol=None, mbuf=None, nch=NCH):
    for c4 in range(0, nch, 4):
        p = pst[(c4 // 4) % len(pst)]
        n4 = min(4, nch - c4)
        for j in range(n4):
            c = c4 + j
            k.op("pe", lambda e, c=c, j=j: e.transpose(out=p[:, j * 128:(j + 1) * 128],
                                                       in_=xt[:, c * 128:(c + 1) * 128], identity=ident[:]),
                 reads=[xt, ident], writes=[p])
        for j in range(n4):
            c = c4 + j
            if gcol is not None:
                k.op("dve", lambda e, c=c, j=j: e.tensor_scalar(out=xT_dst(c), in0=p[:, j * 128:(j + 1) * 128],
                                                                scalar1=gcol[:, c:c + 1], scalar2=scol[:, c:c + 1],
                                                                op0=ALU.mult, op1=ALU.add),
                     reads=[p, mbuf], writes=[dst_buf])
            else:
                k.op("act", lambda e, c=c, j=j: e.copy(out=xT_dst(c), in_=p[:, j * 128:(j + 1) * 128]),
                     reads=[p], writes=[dst_buf])


NC1 = DPROJ + 512
GT = 3


def build_l1():
    k = KB()
    xin = k.dram("xin", [NTOK, D], "ExternalInput")
    modcol = k.dram("modcol", [128, 5 * NCH], "ExternalInput")
    identd = k.dram("identd", [128, 128], "ExternalInput")
    W = k.dram("W", [D, NC1], "ExternalInput")
    proj = k.dram("proj", [NTOK, NC1], "ExternalOutput")
    ident = k.sb([128, 128], "ident")
    mc = k.sb([128, 5 * NCH], "mc")
    gs = k.sb([128, 4 * NCH], "gs")
    xt = [k.sb([128, D], "xt%d" % i) for i in range(2)]
    sq = k.sb([128, D], "sq")
    st = k.sb([128, 4], "st")
    xT = k.sb([128, GT * NCH * 128], "xT", mdt())
    BW = 256
    wb = [k.sb([128, NCH, BW], "wb%d" % i, mdt()) for i in range(2)]
    og = [k.sb([128, BW], "og%d" % i) for i in range(3)]
    pst = [k.ps([128, 512], "pst%d" % i) for i in range(2)]
    pso = [k.ps([128, BW], "pso%d" % i) for i in range(3)]
    k.dma("sp", ident[:], identd.t.ap(), dst=ident)
    k.dma("sp", mc[:], modcol.t.ap(), dst=mc)
    g = mc[:, 0:NCH]
    for s in range(2):
        sc = mc[:, (1 + 2 * s) * NCH:(2 + 2 * s) * NCH]
        sh = mc[:, (2 + 2 * s) * NCH:(3 + 2 * s) * NCH]
        k.op("dve", lambda e, s=s, sc=sc: e.tensor_tensor(out=gs[:, 2 * s * NCH:(2 * s + 1) * NCH], in0=g, in1=sc,
                                                          op=ALU.mult), reads=[mc], writes=[gs])
        k.op("dve", lambda e, s=s: e.tensor_tensor(out=gs[:, 2 * s * NCH:(2 * s + 1) * NCH],
                                                   in0=gs[:, 2 * s * NCH:(2 * s + 1) * NCH], in1=g, op=ALU.add),
             reads=[mc, gs], writes=[gs])
        k.op("dve", lambda e, s=s, sh=sh: e.tensor_copy(out=gs[:, (2 * s + 1) * NCH:(2 * s + 2) * NCH], in_=sh),
             reads=[mc], writes=[gs])
    Wv = W.t.ap().rearrange("(c p) n -> p c n", p=128)
    nblk = NC1 // BW
    it = 0
    for grp in range(NT // GT):
        for j in range(GT):
            ti = grp * GT + j
            x = xt[ti % 2]
            k.dma("sp", x[:], xin.t.ap()[ti * 128:(ti + 1) * 128, :], dst=x)
            rms_scale(k, x, sq, st, D)
            s = 0 if ti < 8 else 1
            transpose_tile(k, x, ident, pst,
                           lambda c, j=j: xT[:, (j * NCH + c) * 128:(j * NCH + c + 1) * 128], xT,
                           gcol=gs[:, 2 * s * NCH:(2 * s + 1) * NCH], scol=gs[:, (2 * s + 1) * NCH:(2 * s + 2) * NCH],
                           mbuf=gs)
        for blk in range(nblk):
            w = wb[blk % 2]
            for q4 in range(4):
                k.dma("pool" if MM_R[0] else "sp", w[:, q4 * 8:(q4 + 1) * 8, :], Wv[:, q4 * 8:(q4 + 1) * 8, blk * BW:(blk + 1) * BW], dst=w)
            for j in range(GT):
                ti = grp * GT + j
                p = pso[it % 3]
                o = og[it % 3]
                it += 1
                for c in range(NCH):
                    k.op("pe", lambda e, c=c, j=j: mmr(e, p[:], xT[:, (j * NCH + c) * 128:(j * NCH + c + 1) * 128],
                                                       w[:, c, :], (c == 0), (c == NCH - 1)),
                         reads=[xT, w], writes=[p])
                k.op("act", lambda e: e.copy(out=o[:], in_=p[:]), reads=[p], writes=[o])
                k.dma("sp" if MM_R[0] else "pool", proj.t.ap()[ti * 128:(ti + 1) * 128, blk * BW:(blk + 1) * BW], o[:], src=o, final=True)
    k.finish()
    return k.nc


def tok_rows(i):
    return i // 4, (i % 4) * 1024, (i % 4) % 2


def col_layout(v):
    return np.ascontiguousarray(v.reshape(NCH, 128).T)


def run_l1(x, h, mod_l, norm1_g_l, Wcat):
    ident = np.eye(128, dtype=np.float32)
    in_maps = []
    for i in range(NCORES):
        b, t0, cj = tok_rows(i)
        xin = np.ascontiguousarray(np.concatenate([x[b, t0:t0 + 1024], h[b, cj * 128:(cj + 1) * 128]], 0))
        mc = np.concatenate([col_layout(norm1_g_l), col_layout(mod_l[b, D:2 * D]), col_layout(mod_l[b, 0:D]),
                             col_layout(mod_l[2, D:2 * D]), col_layout(mod_l[2, 0:D])], 1)
        in_maps.append({"xin": xin, "modcol": np.ascontiguousarray(mc), "identd": ident, "W": Wcat})
    res = run_bass_kernel_spmd(build_l1(), in_maps, core_ids=list(range(NCORES)))
    px = np.zeros((2, T, NC1), np.float32)
    ph = np.zeros((2, LCTX, NC1), np.float32)
    for i in range(NCORES):
        b, t0, cj = tok_rows(i)
        o = res.results[i]["proj"]
        px[b, t0:t0 + 1024] = o[:1024]
        if (i % 4) < 2:
            ph[b, cj * 128:(cj + 1) * 128] = o[1024:]
    return px, ph


HD = 128
NH = 16
NKV = 4
CW = 1024
RW = 1024
GELU_C = 1.5957691216057308


def gelu_tanh(k, x, t1, w):
    k.op("dve", lambda e: e.tensor_tensor(out=t1[:, :w], in0=x[:, :w], in1=x[:, :w], op=ALU.mult), reads=[x], writes=[t1])
    k.op("dve", lambda e: e.tensor_scalar(out=t1[:, :w], in0=t1[:, :w], scalar1=0.044715, scalar2=1.0, op0=ALU.mult,
                                          op1=ALU.add), reads=[t1], writes=[t1])
    k.op("dve", lambda e: e.tensor_tensor(out=t1[:, :w], in0=t1[:, :w], in1=x[:, :w], op=ALU.mult), reads=[t1, x], writes=[t1])
    k.op("act", lambda e: e.activation(out=t1[:, :w], in_=t1[:, :w], func=AF.Sigmoid, scale=GELU_C), reads=[t1], writes=[t1])
    k.op("dve", lambda e: e.tensor_tensor(out=x[:, :w], in0=x[:, :w], in1=t1[:, :w], op=ALU.mult), reads=[t1, x], writes=[x])


def rope(k, x, c, s, t1, w):
    def v(b, e):
        return b[:, :w].rearrange("p (a two s) -> p a two s", two=2, s=32)[:, :, e, :]
    for e_ in range(2):
        k.op("dve", lambda e, e_=e_: e.tensor_tensor(out=v(t1, e_), in0=v(x, 1 - e_), in1=v(s, e_), op=ALU.mult),
             reads=[x, s], writes=[t1])
    k.op("dve", lambda e: e.tensor_tensor(out=x[:, :w], in0=x[:, :w], in1=c[:, :w], op=ALU.mult), reads=[x, c], writes=[x])
    k.op("dve", lambda e: e.tensor_tensor(out=x[:, :w], in0=x[:, :w], in1=t1[:, :w], op=ALU.add), reads=[x, t1], writes=[x])


def build_l2(do_attn=True, do_cmlp=True, do_rwkv=True):
    k = KB()
    di = lambda n, s: k.dram(n, s, "ExternalInput")
    qx = di("qx", [NTOK, 2048]); kh = di("kh", [1280, 512]); vh = di("vh", [1280, 512])
    kc = di("kc", [256, 512]); vc = di("vc", [256, 512])
    rqc = di("rqc", [1024, 2048]); rqs = di("rqs", [1024, 2048]); rkc = di("rkc", [1280, 512]); rks = di("rks", [1280, 512])
    masks = di("masks", [4 * 128, 512]); sink = di("sink", [16])
    ug = di("ug", [NTOK, 2048]); cng = di("cng", [CW]); wsT = di("wsT", [128, 1024]); bsT = di("bsT", [128, 8])
    rkvp = di("rkvp", [1026 + 130, 3072]); lora = di("lora", [NTOK, 512]); convw = di("convw", [3, 3072])
    w2a2 = di("w2a2", [128, 4096]); w0a0 = di("w0a0", [1, 4096]); kkp = di("kkp", [RW]); kap = di("kap", [RW])
    identd = di("identd", [128, 128])
    attn = k.dram("attn", [NTOK, 2048], "ExternalOutput")
    cmlp = k.dram("cmlp", [NTOK, CW], "ExternalOutput")
    scin = k.dram("scin", [NTOK, 10 * RW], "ExternalOutput")

    ident = k.sb([128, 128], "ident")
    k.dma("sp", ident[:], identd.t.ap(), dst=ident)
    WK = [k.sb([128, 3072], "wk%d" % i) for i in range(6)]
    pst = [k.ps([128, 512], "pst%d" % i) for i in range(2)]
    psA = [k.ps([128, 512], "psA%d" % i) for i in range(3)]
    psB = [k.ps([128, 512], "psB%d" % i) for i in range(2)]
    scale = float(HD) ** -0.5

    if do_attn:
        kT = k.sb([128, NKV * 1280], "kT")
        va = k.sb([128, 10 * NKV * 129], "va")
        kcT = k.sb([128, NKV * 256], "kcT")
        vca = k.sb([128, 2 * NKV * 129], "vca")
        mk = k.sb([128, 4 * 512], "mk")
        esk = k.sb([128, 16], "esk")
        st = k.sb([128, 8], "stA")
        for m in range(4):
            k.dma("sp", mk[:, m * 512:(m + 1) * 512], masks.t.ap()[m * 128:(m + 1) * 128, :], dst=mk)
        k.dma("sp", esk[:], sink.t.ap().partition_broadcast(128), dst=esk)
        k.op("act", lambda e: e.activation(out=esk[:], in_=esk[:], func=AF.Exp), reads=[esk], writes=[esk])
        k.op("dve", lambda e: e.memset(va[:], 1.0), writes=[va])
        k.op("dve", lambda e: e.memset(vca[:], 1.0), writes=[vca])
        va4 = lambda blk, h: va[:, (blk * NKV + h) * 129:(blk * NKV + h) * 129 + 129]
        vca4 = lambda blk, h: vca[:, (blk * NKV + h) * 129:(blk * NKV + h) * 129 + 129]
        kt_, vt_, c_, s_, t_ = WK[0], WK[1], WK[2], WK[3], WK[4]
        for blk in range(12):
            isctx = blk >= 10
            src_k = kc.t.ap()[(blk - 10) * 128:(blk - 9) * 128, :] if isctx else kh.t.ap()[blk * 128:(blk + 1) * 128, :]
            src_v = vc.t.ap()[(blk - 10) * 128:(blk - 9) * 128, :] if isctx else vh.t.ap()[blk * 128:(blk + 1) * 128, :]
            k.dma("sp", kt_[:, :512], src_k, dst=kt_)
            k.dma("sp", vt_[:, :512], src_v, dst=vt_)
            if not isctx:
                k.dma("sp", c_[:, :512], rkc.t.ap()[blk * 128:(blk + 1) * 128, :], dst=c_)
                k.dma("sp", s_[:, :512], rks.t.ap()[blk * 128:(blk + 1) * 128, :], dst=s_)
                rope(k, kt_, c_, s_, t_, 512)
                transpose_tile(k, kt_, ident, pst, lambda c, blk=blk: kT[:, c * 1280 + blk * 128:c * 1280 + (blk + 1) * 128],
                               kT, nch=NKV)
            else:
                transpose_tile(k, kt_, ident, pst,
                               lambda c, blk=blk: kcT[:, c * 256 + (blk - 10) * 128:c * 256 + (blk - 9) * 128], kcT, nch=NKV)
            for h in range(NKV):
                dst = vca4(blk - 10, h) if isctx else va4(blk, h)
                k.op("dve", lambda e, dst=dst, h=h: e.tensor_copy(out=dst[:, 0:128], in_=vt_[:, h * 128:(h + 1) * 128]),
                     reads=[vt_], writes=[vca if isctx else va])
        qt, qT, PT, ao = WK[0], WK[1], WK[2], WK[3]
        c_, s_, t_ = WK[4], WK[5], WK[2]
        for ti in range(NT):
            isctx = ti == 8
            k.dma("sp", qt[:, :2048], qx.t.ap()[ti * 128:(ti + 1) * 128, :], dst=qt)
            if not isctx:
                k.dma("sp", c_[:, :2048], rqc.t.ap()[ti * 128:(ti + 1) * 128, :], dst=c_)
                k.dma("sp", s_[:, :2048], rqs.t.ap()[ti * 128:(ti + 1) * 128, :], dst=s_)
                rope(k, qt, c_, s_, t_, 2048)
            transpose_tile(k, qt, ident, pst, lambda c: qT[:, c * 128:(c + 1) * 128], qT, nch=NH)
            chunks = []
            if not isctx:
                for d_ in range(3):
                    blk = ti + d_
                    m = None
                    if d_ == 0:
                        m = 0 if ti == 0 else 1
                    if d_ == 2:
                        m = 3 if ti == 7 else 2
                    chunks.append((lambda h, blk=blk: kT[:, h * 1280 + blk * 128:h * 1280 + (blk + 1) * 128],
                                   lambda h, blk=blk: va4(blk, h), va, kT, m))
            for cb in range(2):
                chunks.append((lambda h, cb=cb: kcT[:, h * 256 + cb * 128:h * 256 + (cb + 1) * 128],
                               lambda h, cb=cb: vca4(cb, h), vca, kcT, None))
            for h in range(NKV):
                for ci, (kf, vf, vbuf, kbuf, m) in enumerate(chunks):
                    p = psA[ci % 3]
                    k.op("pe", lambda e, kf=kf, p=p: e.matmul(p[:], lhsT=kf(h), rhs=qT[:, h * 512:(h + 1) * 512],
                                                              start=True, stop=True), reads=[kbuf, qT], writes=[p])
                    k.op("act", lambda e, ci=ci, p=p: e.activation(out=PT[:, ci * 512:(ci + 1) * 512], in_=p[:], func=AF.Exp,
                                                                   scale=scale), reads=[p], writes=[PT])
                    if m is not None:
                        k.op("dve", lambda e, ci=ci, m=m: e.tensor_tensor(out=PT[:, ci * 512:(ci + 1) * 512],
                                                                          in0=PT[:, ci * 512:(ci + 1) * 512],
                                                                          in1=mk[:, m * 512:(m + 1) * 512], op=ALU.mult),
                             reads=[PT, mk], writes=[PT])
                for g in range(4):
                    hq = h * 4 + g
                    p = psB[g % 2]
                    for ci, (kf, vf, vbuf, kbuf, m) in enumerate(chunks):
                        k.op("pe", lambda e, ci=ci, vf=vf, p=p: e.matmul(p[:, 0:129],
                                                                         lhsT=PT[:, ci * 512 + g * 128:ci * 512 + (g + 1) * 128],
                                                                         rhs=vf(h), start=(ci == 0), stop=(ci == len(chunks) - 1)),
                             reads=[PT, vbuf], writes=[p])
                    k.op("dve", lambda e, p=p, hq=hq: e.tensor_tensor(out=st[:, 0:1], in0=p[:, 128:129], in1=esk[:, hq:hq + 1],
                                                                      op=ALU.add), reads=[p, esk], writes=[st])
                    k.op("dve", lambda e: e.reciprocal(out=st[:, 1:2], in_=st[:, 0:1]), reads=[st], writes=[st])
                    k.op("dve", lambda e, p=p, hq=hq: e.tensor_scalar(out=ao[:, hq * 128:(hq + 1) * 128], in0=p[:, 0:128],
                                                                      scalar1=st[:, 1:2], scalar2=None, op0=ALU.mult),
                         reads=[p, st], writes=[ao])
            k.dma("pool", attn.t.ap()[ti * 128:(ti + 1) * 128, :], ao[:, :2048], src=ao, final=True)

    if do_cmlp:
        ng = k.sb([128, CW], "ng"); wst = k.sb([128, 1024], "wst"); bst = k.sb([128, 8], "bst")
        stc = k.sb([128, 4], "stC")
        k.dma("sp", ng[:], cng.t.ap().partition_broadcast(128), dst=ng)
        k.dma("sp", wst[:], wsT.t.ap(), dst=wst)
        k.dma("sp", bst[:], bsT.t.ap(), dst=bst)
        ut, t1, sq, cm = WK[0], WK[1], WK[2], WK[3]
        for ti in range(NT):
            k.dma("sp", ut[:, :2048], ug.t.ap()[ti * 128:(ti + 1) * 128, :], dst=ut)
            gelu_tanh(k, ut, t1, 2048)
            gvv = Buf("gvview", None)
            k.op("dve", lambda e: e.tensor_tensor(out=sq[:, :CW], in0=ut[:, CW:2 * CW], in1=ut[:, CW:2 * CW], op=ALU.mult),
                 reads=[ut], writes=[sq])
            k.op("dve", lambda e: e.tensor_reduce(out=stc[:, 0:1], in_=sq[:, :CW], axis=AX.X, op=ALU.add), reads=[sq], writes=[stc])
            k.op("dve", lambda e: e.tensor_scalar(out=stc[:, 1:2], in0=stc[:, 0:1], scalar1=1.0 / CW, scalar2=1e-6,
                                                  op0=ALU.mult, op1=ALU.add), reads=[stc], writes=[stc])
            k.op("act", lambda e: e.activation(out=stc[:, 2:3], in_=stc[:, 1:2], func=AF.Sqrt), reads=[stc], writes=[stc])
            k.op("dve", lambda e: e.reciprocal(out=stc[:, 3:4], in_=stc[:, 2:3]), reads=[stc], writes=[stc])
            k.op("dve", lambda e: e.scalar_tensor_tensor(out=sq[:, :CW], in0=ut[:, CW:2 * CW], scalar=stc[:, 3:4], in1=ng[:],
                                                         op0=ALU.mult, op1=ALU.mult), reads=[ut, stc, ng], writes=[sq])
            for g in range(8):
                p = psA[(g // 4) % 3]
                k.op("pe", lambda e, g=g, p=p: e.matmul(p[:, (g % 4) * 128:(g % 4 + 1) * 128], lhsT=wst[:, g * 128:(g + 1) * 128],
                                                        rhs=sq[:, g * 128:(g + 1) * 128], start=True, stop=True),
                     reads=[wst, sq], writes=[p])
            for g in range(8):
                p = psA[(g // 4) % 3]
                k.op("dve", lambda e, g=g, p=p: e.scalar_tensor_tensor(out=cm[:, g * 128:(g + 1) * 128],
                                                                       in0=p[:, (g % 4) * 128:(g % 4 + 1) * 128],
                                                                       scalar=bst[:, g:g + 1], in1=ut[:, g * 128:(g + 1) * 128],
                                                                       op0=ALU.add, op1=ALU.mult), reads=[p, bst, ut], writes=[cm])
            k.dma("pool", cmlp.t.ap()[ti * 128:(ti + 1) * 128, :], cm[:, :CW], src=cm, final=True)

    if do_rwkv:
        w2t = k.sb([128, 4096], "w2t"); w0t = k.sb([1, 4096], "w0t"); ones = k.sb([1, 128], "ones1")
        kkpt = k.sb([128, RW], "kkpt"); kapt = k.sb([128, RW], "kapt")
        lt = k.sb([128, 512], "lt"); lT = k.sb([128, 512], "lT")
        ss = k.sb([128, 64], "ss")
        k.dma("sp", w2t[:], w2a2.t.ap(), dst=w2t)
        k.dma("sp", w0t[:], w0a0.t.ap(), dst=w0t)
        k.dma("sp", kkpt[:], kkp.t.ap().partition_broadcast(128), dst=kkpt)
        k.dma("sp", kapt[:], kap.t.ap().partition_broadcast(128), dst=kapt)
        k.op("dve", lambda e: e.memset(ones[:], 1.0), writes=[ones])
        xs, cw, o1, kf, kk, o2 = WK
        for ti in range(NT):
            r0 = ti * 128 if ti < 8 else 1026
            outs = {}
            for part in range(3):
                for j in range(3):
                    k.dma("sp", xs[:, j * 1024:(j + 1) * 1024], rkvp.t.ap()[r0 + j:r0 + j + 128, part * 1024:(part + 1) * 1024], dst=xs)
                    k.dma("sp", cw[:, j * 1024:(j + 1) * 1024],
                          convw.t.ap()[j, part * 1024:(part + 1) * 1024].partition_broadcast(128), dst=cw)
                dstb = kf if part == 1 else o1
                k.op("dve", lambda e: e.tensor_tensor(out=xs[:], in0=xs[:], in1=cw[:], op=ALU.mult), reads=[xs, cw], writes=[xs])
                k.op("dve", lambda e, dstb=dstb: e.tensor_tensor(out=dstb[:, :RW], in0=xs[:, 0:RW], in1=xs[:, RW:2 * RW], op=ALU.add),
                     reads=[xs], writes=[dstb])
                k.op("dve", lambda e, dstb=dstb: e.tensor_tensor(out=dstb[:, :RW], in0=dstb[:, :RW], in1=xs[:, 2 * RW:3 * RW], op=ALU.add),
                     reads=[xs, dstb], writes=[dstb])
                qi = (0, 9, 2)[part]
                k.dma("pool", scin.t.ap()[ti * 128:(ti + 1) * 128, qi * RW:(qi + 1) * RW], dstb[:, :RW], src=dstb, final=True)
            k.op("dve", lambda e: e.tensor_tensor(out=kk[:, :RW], in0=kf[:, :RW], in1=kkpt[:], op=ALU.mult), reads=[kf, kkpt], writes=[kk])
            k.op("dve", lambda e: e.tensor_tensor(out=o2[:, :RW], in0=kk[:, :RW], in1=kk[:, :RW], op=ALU.mult), reads=[kk], writes=[o2])
            k.op("dve", lambda e: e.tensor_reduce(out=ss[:, 0:16], in_=o2[:, :RW].rearrange("p (h n) -> p h n", n=64), axis=AX.X,
                                                  op=ALU.add), reads=[o2], writes=[ss])
            k.op("dve", lambda e: e.tensor_scalar(out=ss[:, 0:16], in0=ss[:, 0:16], scalar1=1e-12, scalar2=None, op0=ALU.add),
                 reads=[ss], writes=[ss])
            k.op("act", lambda e: e.activation(out=ss[:, 16:32], in_=ss[:, 0:16], func=AF.Sqrt), reads=[ss], writes=[ss])
            k.op("dve", lambda e: e.reciprocal(out=ss[:, 32:48], in_=ss[:, 16:32]), reads=[ss], writes=[ss])
            for h in range(16):
                k.op("dve", lambda e, h=h: e.tensor_scalar(out=kk[:, h * 64:(h + 1) * 64], in0=kk[:, h * 64:(h + 1) * 64],
                                                           scalar1=ss[:, 32 + h:33 + h], scalar2=None, op0=ALU.mult),
                     reads=[kk, ss], writes=[kk])
            k.op("dve", lambda e: e.tensor_scalar(out=o2[:, :RW], in0=kk[:, :RW], scalar1=-1.0, scalar2=None, op0=ALU.mult),
                 reads=[kk], writes=[o2])
            k.dma("pool", scin.t.ap()[ti * 128:(ti + 1) * 128, 1 * RW:2 * RW], o2[:, :RW], src=o2, final=True)
            k.dma("sp", lt[:], lora.t.ap()[ti * 128:(ti + 1) * 128, :], dst=lt)
            k.op("act", lambda e: e.activation(out=lt[:, 0:256], in_=lt[:, 0:256], func=AF.Tanh), reads=[lt], writes=[lt])
            transpose_tile(k, lt, ident, pst, lambda c: lT[:, c * 128:(c + 1) * 128], lT, nch=4)
            for z in range(2):
                for half in range(2):
                    p = psA[half]
                    cs = slice(z * 1024 + half * 512, z * 1024 + half * 512 + 512)
                    k.op("pe", lambda e, p=p, cs=cs: e.matmul(p[:], lhsT=lT[:, z * 128:(z + 1) * 128], rhs=w2t[:, cs], start=True,
                                                              stop=False), reads=[lT, w2t], writes=[p])
                    k.op("pe", lambda e, p=p, cs=cs: e.matmul(p[:], lhsT=ones[:], rhs=w0t[:, cs], start=False, stop=True),
                         reads=[ones, w0t], writes=[p])
                    k.op("act", lambda e, p=p, half=half: e.activation(out=o1[:, half * 512:(half + 1) * 512], in_=p[:],
                                                                       func=AF.Sigmoid), reads=[p], writes=[o1])
                k.op("act", lambda e: e.activation(out=o1[:, :RW], in_=o1[:, :RW], func=AF.Exp, scale=-float(np.exp(-0.5))),
                     reads=[o1], writes=[o1])
                k.dma("pool", scin.t.ap()[ti * 128:(ti + 1) * 128, (3 + 3 * z) * RW:(4 + 3 * z) * RW], o1[:, :RW], src=o1, final=True)
                for half in range(2):
                    p = psA[half]
                    cs = slice(2048 + z * 1024 + half * 512, 2048 + z * 1024 + half * 512 + 512)
                    k.op("pe", lambda e, p=p, cs=cs: e.matmul(p[:], lhsT=lT[:, (2 + z) * 128:(3 + z) * 128], rhs=w2t[:, cs], start=True,
                                                              stop=False), reads=[lT, w2t], writes=[p])
                    k.op("pe", lambda e, p=p, cs=cs: e.matmul(p[:], lhsT=ones[:], rhs=w0t[:, cs], start=False, stop=True),
                         reads=[ones, w0t], writes=[p])
                    k.op("act", lambda e, p=p, half=half: e.activation(out=xs[:, half * 512:(half + 1) * 512], in_=p[:],
                                                                       func=AF.Sigmoid), reads=[p], writes=[xs])
                k.op("dve", lambda e: e.tensor_tensor(out=cw[:, :RW], in0=kk[:, :RW], in1=xs[:, :RW], op=ALU.mult),
                     reads=[kk, xs], writes=[cw])
                k.dma("pool", scin.t.ap()[ti * 128:(ti + 1) * 128, (4 + 3 * z) * RW:(5 + 3 * z) * RW], cw[:, :RW], src=cw, final=True)
                k.op("dve", lambda e: e.scalar_tensor_tensor(out=xs[:, RW:2 * RW], in0=xs[:, :RW], scalar=-1.0, in1=kapt[:],
                                                             op0=ALU.add, op1=ALU.mult), reads=[xs, kapt], writes=[xs])
                k.op("dve", lambda e: e.scalar_tensor_tensor(out=xs[:, 2 * RW:3 * RW], in0=xs[:, RW:2 * RW], scalar=1.0, in1=kf[:, :RW],
                                                             op0=ALU.add, op1=ALU.mult), reads=[xs, kf], writes=[xs])
                k.dma("pool", scin.t.ap()[ti * 128:(ti + 1) * 128, (5 + 3 * z) * RW:(6 + 3 * z) * RW], xs[:, 2 * RW:3 * RW], src=xs,
                      final=True)
    k.finish()
    return k.nc


def rope_tables():
    half = 32
    freqs = 10000.0 ** (-np.arange(half, dtype=np.float32) / half)
    t = np.arange(T)
    row = (t // 64).astype(np.float32)[:, None] * freqs[None, :]
    col = (t % 64).astype(np.float32)[:, None] * freqs[None, :]
    cr, sr, cc, sc = np.cos(row), np.sin(row), np.cos(col), np.sin(col)
    C = np.concatenate([cr, cr, cc, cc], 1).astype(np.float32)
    S = np.concatenate([-sr, sr, -sc, sc], 1).astype(np.float32)
    return C, S


def l2_inmaps(px, ph, P):
    C, S = rope_tables()
    ident = np.eye(128, dtype=np.float32)
    jj = np.arange(128)[:, None]
    ii = np.arange(128)[None, :]
    mprev = np.tile((jj >= ii).astype(np.float32), (1, 4))
    mnext = np.tile((jj <= ii).astype(np.float32), (1, 4))
    zero = np.zeros_like(mprev)
    wsT = np.ascontiguousarray(P["cmlp_ws"].transpose(2, 0, 1).reshape(128, 1024))
    bsT = np.ascontiguousarray(P["cmlp_b"].T)
    w2a2 = np.ascontiguousarray(np.concatenate([P["rwkv_w2"][0], P["rwkv_w2"][1], P["rwkv_a2"][0], P["rwkv_a2"][1]], 1))
    w0a0 = np.ascontiguousarray(np.concatenate([P["rwkv_w0"][0], P["rwkv_w0"][1], P["rwkv_a0"][0], P["rwkv_a0"][1]])[None])
    in_maps = []
    for i in range(NCORES):
        b, t0, cj = tok_rows(i)
        cat = lambda a, c: np.ascontiguousarray(np.concatenate([a, c], 0))
        ctile = ph[b, cj * 128:(cj + 1) * 128]
        lat = px[b, t0:t0 + 1024]
        def halo(cols, n, src, s0, s1):
            o = np.zeros((s1 - s0 + 2 * n, cols.stop - cols.start), np.float32)
            lo, hi = max(s0 - n, 0), min(s1 + n, src.shape[0])
            o[lo - (s0 - n):hi - (s0 - n)] = src[lo:hi, cols]
            return o
        Ck = np.zeros((1280, 128), np.float32); Sk = np.zeros((1280, 128), np.float32)
        lo, hi = max(t0 - 128, 0), min(t0 + 1152, T)
        Ck[lo - (t0 - 128):hi - (t0 - 128)] = C[lo:hi]; Sk[lo - (t0 - 128):hi - (t0 - 128)] = S[lo:hi]
        m = np.concatenate([zero if t0 == 0 else mprev, mprev, mnext, zero if t0 + 1024 == T else mnext], 0)
        in_maps.append({
            "qx": cat(lat[:, 0:2048], ctile[:, 0:2048]),
            "kh": halo(slice(2048, 2560), 128, px[b], t0, t0 + 1024), "vh": halo(slice(2560, 3072), 128, px[b], t0, t0 + 1024),
            "kc": np.ascontiguousarray(ph[b][:, 2048:2560]), "vc": np.ascontiguousarray(ph[b][:, 2560:3072]),
            "rqc": np.ascontiguousarray(np.tile(C[t0:t0 + 1024], (1, 16))), "rqs": np.ascontiguousarray(np.tile(S[t0:t0 + 1024], (1, 16))),
            "rkc": np.ascontiguousarray(np.tile(Ck, (1, 4))), "rks": np.ascontiguousarray(np.tile(Sk, (1, 4))),
            "masks": np.ascontiguousarray(m), "sink": np.ascontiguousarray(P["attn_sink"]),
            "ug": cat(lat[:, 3072:5120], ctile[:, 3072:5120]), "cng": np.ascontiguousarray(P["cmlp_norm_g"]),
            "wsT": wsT, "bsT": bsT,
            "rkvp": cat(halo(slice(5120, 8192), 1, px[b], t0, t0 + 1024), halo(slice(5120, 8192), 1, ph[b], cj * 128, cj * 128 + 128)),
            "lora": cat(lat[:, 9216:9728], ctile[:, 9216:9728]), "convw": np.ascontiguousarray(P["rwkv_conv"]),
            "w2a2": w2a2, "w0a0": w0a0, "kkp": np.ascontiguousarray(P["rwkv_kk"]), "kap": np.ascontiguousarray(P["rwkv_ka"]),
            "identd": ident,
        })
    return in_maps


def l2_gather(res, cores=None):
    cores = list(range(NCORES)) if cores is None else cores
    attn_x = np.zeros((2, T, 2048), np.float32); attn_c = np.zeros((2, LCTX, 2048), np.float32)
    cm_x = np.zeros((2, T, CW), np.float32); cm_c = np.zeros((2, LCTX, CW), np.float32)
    sc_x = np.zeros((2, T, 10 * RW), np.float32); sc_c = np.zeros((2, LCTX, 10 * RW), np.float32)
    for r, i in zip(res.results, cores):
        b, t0, cj = tok_rows(i)
        for key, X, Cc in (("attn", attn_x, attn_c), ("cmlp", cm_x, cm_c), ("scin", sc_x, sc_c)):
            X[b, t0:t0 + 1024] = r[key][:1024]
            if (i % 4) < 2:
                Cc[b, cj * 128:(cj + 1) * 128] = r[key][1024:]
    return attn_x, attn_c, cm_x, cm_c, sc_x, sc_c


NS = LCTX + T
TC = 8


def build_l3(ns=NS, tc=TC, stage=9):
    k = KB()
    nchk = ns // tc
    RR = k.dram("RR", [nchk * 2, 4 * tc * 256], "ExternalInput")
    V2 = k.dram("V2", [nchk * 2, 4 * tc * 64], "ExternalInput")
    LL = k.dram("LL", [nchk * 128, 4 * tc * 4], "ExternalInput")
    WC = k.dram("WC", [nchk * 128, 4 * tc], "ExternalInput")
    Y = k.dram("Y", [nchk * 4 * 4, tc * 64], "ExternalOutput")
    rr = [k.sb([2, 4 * tc * 256], "rr%d" % i) for i in range(2)]
    v2 = [k.sb([2, 4 * tc * 64], "v2%d" % i) for i in range(2)]
    ll = [k.sb([128, 4 * tc * 4], "ll%d" % i) for i in range(2)]
    wc = [k.sb([128, 4 * tc], "wc%d" % i) for i in range(2)]
    y4 = [[k.sb([4, tc * 64], "y4_%d_%d" % (i, p)) for p in range(4)] for i in range(2)]
    ST = [k.sb([128, 64], "ST%d" % p) for p in range(4)]
    U = [[k.ps([128, 64], "U%d" % p)] * 2 for p in range(4)]
    P1 = [[k.ps([4, 64], "P%d" % p)] * 2 for p in range(4)]
    for p in range(4):
        k.op("dve", lambda e, p=p: e.memset(ST[p][:], 0.0), writes=[ST[p]])

    def load(c):
        s = c % 2
        k.dma("sp", rr[s][:], RR.t.ap()[c * 2:(c + 1) * 2, :], dst=rr[s])
        k.dma("sp", v2[s][:], V2.t.ap()[c * 2:(c + 1) * 2, :], dst=v2[s])
        k.dma("sp", ll[s][:], LL.t.ap()[c * 128:(c + 1) * 128, :], dst=ll[s])
        k.dma("sp", wc[s][:], WC.t.ap()[c * 128:(c + 1) * 128, :], dst=wc[s])

    load(0)
    for c in range(nchk):
        if c + 1 < nchk:
            load(c + 1)
        s = c % 2
        for tt in range(tc):
            t = c * tc + tt
            for p in range(4):
                u = U[p][t % 2]
                o = (p * tc + tt) * 256
                ov = (p * tc + tt) * 64
                if stage < 1:
                    continue
                k.op("pe", lambda e, u=u, o=o, ov=ov: e.matmul(u[:], lhsT=rr[s][0:2, o:o + 128], rhs=v2[s][0:2, ov:ov + 64],
                                                               start=True, stop=(t == 0)), reads=[rr[s], v2[s]], writes=[u])
                if t > 0 and stage >= 2:
                    yp, tp = (y4[s][p], tt - 1) if tt > 0 else (y4[1 - s][p], tc - 1)
                    k.op("pe", lambda e, u=u, o=o, yp=yp, tp=tp: e.matmul(u[:], lhsT=rr[s][0:2, o + 128:o + 256],
                                                                          rhs=yp[0:2, tp * 64:(tp + 1) * 64], start=False, stop=True),
                         reads=[rr[s], yp], writes=[u])
            for p in range(4):
                u = U[p][t % 2]
                if stage < 3:
                    continue
                k.op("dve", lambda e, p=p, u=u: e.scalar_tensor_tensor(out=ST[p][:], in0=ST[p][:],
                                                                       scalar=wc[s][:, p * tc + tt:p * tc + tt + 1], in1=u[:],
                                                                       op0=ALU.mult, op1=ALU.add), reads=[ST[p], wc[s], u], writes=[ST[p]])
            for p in range(4):
                pp = P1[p][t % 2]
                ol = (p * tc + tt) * 4
                if stage < 4:
                    if tt == 0:
                        k.op("dve", lambda e, p=p: e.memset(y4[s][p][:], 1.0), writes=[y4[s][p]])
                    continue
                k.op("pe", lambda e, p=p, pp=pp, ol=ol: e.matmul(pp[:], lhsT=ll[s][:, ol:ol + 4], rhs=ST[p][:], start=True, stop=True),
                     reads=[ll[s], ST[p]], writes=[pp])
                k.op("act", lambda e, p=p, pp=pp: e.copy(out=y4[s][p][:, tt * 64:(tt + 1) * 64], in_=pp[:]), reads=[pp], writes=[y4[s][p]])
        for p in range(4):
            k.dma("pool", Y.t.ap()[(c * 4 + p) * 4:(c * 4 + p + 1) * 4, :], y4[s][p][:], src=y4[s][p], final=True)
    k.finish()
    return k.nc


def scan_core(i):
    return i // 4, (i // 2) % 2, (i % 2) * 512


def l3_inmaps(sc_x, sc_c, ns=NS, tc=TC):
    nchk = ns // tc
    in_maps = []
    for i in range(NCORES):
        z, b, c0 = scan_core(i)
        seq = np.concatenate([sc_c[b][::-1] if z else sc_c[b], sc_x[b][::-1] if z else sc_x[b]], 0)[:ns]
        q = lambda qi: seq[:, qi * RW + c0:qi * RW + c0 + 512].reshape(ns, 4, 2, 64)
        r, nkk, v, w, bb, kr = q(0), q(1), q(2), q(3 + 3 * z), q(4 + 3 * z), q(5 + 3 * z)
        RRa = np.zeros((nchk, 2, 4, tc, 256), np.float32)
        V2a = np.zeros((nchk, 2, 4, tc, 64), np.float32)
        LLa = np.zeros((nchk, 2, 64, 4, tc, 4), np.float32)
        WCa = np.zeros((nchk, 2, 64, 4, tc), np.float32)
        c5 = lambda a: a.reshape(nchk, tc, 4, 2, 64)
        nkk_next = np.concatenate([nkk[1:], np.zeros_like(nkk[:1])], 0)
        for h in range(2):
            RRa[:, h, :, :, h * 64:(h + 1) * 64] = c5(kr)[:, :, :, h, :].transpose(0, 2, 1, 3)
            RRa[:, h, :, :, 128 + h * 64:128 + (h + 1) * 64] = c5(bb)[:, :, :, h, :].transpose(0, 2, 1, 3)
            V2a[:, h] = c5(v)[:, :, :, h, :].transpose(0, 2, 1, 3)
            LLa[:, h, :, :, :, h] = c5(nkk_next)[:, :, :, h, :].transpose(0, 3, 2, 1)
            LLa[:, h, :, :, :, 2 + h] = c5(r)[:, :, :, h, :].transpose(0, 3, 2, 1)
            WCa[:, h] = c5(w)[:, :, :, h, :].transpose(0, 3, 2, 1)
        in_maps.append({"RR": RRa.reshape(nchk * 2, -1), "V2": V2a.reshape(nchk * 2, -1),
                        "LL": LLa.reshape(nchk * 128, -1), "WC": WCa.reshape(nchk * 128, -1)})
    return in_maps


def l3_gather(res, ns=NS, tc=TC, cores=None):
    cores = list(range(NCORES)) if cores is None else cores
    nchk = ns // tc
    y = np.zeros((2, 2, ns, RW), np.float32)
    for rs, i in zip(res.results, cores):
        z, b, c0 = scan_core(i)
        Ya = rs["Y"].reshape(nchk, 4, 4, tc, 64)
        yy = Ya[:, :, 2:4].transpose(0, 3, 1, 2, 4).reshape(ns, 512)
        if z:
            yy = np.concatenate([yy[:LCTX][::-1], yy[LCTX:][::-1]], 0) if ns == NS else yy[::-1]
        y[z, b, :, c0:c0 + 512] = yy
    return y


GROUPS2 = [(0, 1), (2, 3), (4, 5), (6, 7), (8,)]


def build_l4a():
    k = KB()
    di = lambda n, s: k.dram(n, s, "ExternalInput")
    y01 = di("y01", [NTOK, 2 * RW]); rkv = di("rkv", [NTOK, 3 * RW]); gt = di("gt", [NTOK, RW])
    attn = di("attn", [NTOK, 2048]); cmlp = di("cmlp", [NTOK, CW]); xin = di("xin", [NTOK, D])
    prm = di("prm", [3, RW]); g1row = di("g1row", [2, D]); Wo = di("Wo", [D, D]); identd = di("identd", [128, 128])
    x1 = k.dram("x1", [NTOK, D], "ExternalOutput")
    ident = k.sb([128, 128], "ident")
    k.dma("sp", ident[:], identd.t.ap(), dst=ident)
    pt = k.sb([128, 3 * RW], "pt")
    for j in range(3):
        k.dma("sp", pt[:, j * RW:(j + 1) * RW], prm.t.ap()[j, :].partition_broadcast(128), dst=pt)
    mix = k.sb([128, D], "mix")
    mixT = k.sb([128, 2 * NCH * 128], "mixT", mdt())
    BW = 256
    wb = [k.sb([128, NCH, BW], "wb%d" % i, mdt()) for i in range(2)]
    LQ, SQ = ("pool", "sp") if MM_R[0] else ("sp", "pool")
    yt = k.sb([128, 2 * RW], "yt"); rt = k.sb([128, 3 * RW], "rt"); gg = k.sb([128, RW], "gg")
    s1 = k.sb([128, RW], "s1"); s2 = k.sb([128, RW], "s2")
    st = k.sb([128, 128], "st")
    gb = [k.sb([128, BW], "gb%d" % i) for i in range(2)]
    xb = [k.sb([128, BW], "xb%d" % i) for i in range(3)]
    pst = [k.ps([128, 512], "pst%d" % i) for i in range(2)]
    pso = [k.ps([128, BW], "pso%d" % i) for i in range(3)]
    Wv = Wo.t.ap().rearrange("(c p) n -> p c n", p=128)
    it = 0
    for grp in GROUPS2:
        ng = len(grp)
        for j, ti in enumerate(grp):
            rows = slice(ti * 128, (ti + 1) * 128)
            k.dma("sp", mix[:, 0:2048], attn.t.ap()[rows, :], dst=mix)
            k.dma("sp", mix[:, 2048:3072], cmlp.t.ap()[rows, :], dst=mix)
            k.dma("sp", yt[:], y01.t.ap()[rows, :], dst=yt)
            k.dma("sp", rt[:], rkv.t.ap()[rows, :], dst=rt)
            k.dma("sp", gg[:], gt.t.ap()[rows, :], dst=gg)
            k.op("dve", lambda e: e.tensor_tensor(out=s1[:], in0=yt[:, 0:RW], in1=yt[:, RW:2 * RW], op=ALU.add), reads=[yt], writes=[s1])
            k.op("dve", lambda e: e.tensor_reduce(out=st[:, 0:16], in_=s1[:].rearrange("p (h n) -> p h n", n=64), axis=AX.X, op=ALU.add),
                 reads=[s1], writes=[st])
            k.op("dve", lambda e: e.tensor_tensor(out=s2[:], in0=s1[:], in1=s1[:], op=ALU.mult), reads=[s1], writes=[s2])
            k.op("dve", lambda e: e.tensor_reduce(out=st[:, 16:32], in_=s2[:].rearrange("p (h n) -> p h n", n=64), axis=AX.X, op=ALU.add),
                 reads=[s2], writes=[st])
            k.op("dve", lambda e: e.tensor_scalar(out=st[:, 0:32], in0=st[:, 0:32], scalar1=1.0 / 64, scalar2=None, op0=ALU.mult),
                 reads=[st], writes=[st])
            k.op("dve", lambda e: e.tensor_tensor(out=st[:, 32:48], in0=st[:, 0:16], in1=st[:, 0:16], op=ALU.mult), reads=[st], writes=[st])
            k.op("dve", lambda e: e.tensor_tensor(out=st[:, 48:64], in0=st[:, 16:32], in1=st[:, 32:48], op=ALU.subtract), reads=[st], writes=[st])
            k.op("dve", lambda e: e.tensor_scalar(out=st[:, 48:64], in0=st[:, 48:64], scalar1=64e-5, scalar2=None, op0=ALU.add),
                 reads=[st], writes=[st])
            k.op("act", lambda e: e.activation(out=st[:, 64:80], in_=st[:, 48:64], func=AF.Sqrt), reads=[st], writes=[st])
            k.op("dve", lambda e: e.reciprocal(out=st[:, 80:96], in_=st[:, 64:80]), reads=[st], writes=[st])
            for h in range(16):
                k.op("dve", lambda e, h=h: e.tensor_scalar(out=s1[:, h * 64:(h + 1) * 64], in0=s1[:, h * 64:(h + 1) * 64],
                                                           scalar1=st[:, h:h + 1], scalar2=st[:, 80 + h:81 + h], op0=ALU.subtract,
                                                           op1=ALU.mult), reads=[s1, st], writes=[s1])
            k.op("dve", lambda e: e.tensor_tensor(out=s1[:], in0=s1[:], in1=pt[:, RW:2 * RW], op=ALU.mult), reads=[s1, pt], writes=[s1])
            k.op("dve", lambda e: e.tensor_tensor(out=s1[:], in0=s1[:], in1=pt[:, 2 * RW:3 * RW], op=ALU.add), reads=[s1, pt], writes=[s1])
            k.op("dve", lambda e: e.tensor_tensor(out=s2[:], in0=rt[:, 0:RW], in1=rt[:, RW:2 * RW], op=ALU.mult), reads=[rt], writes=[s2])
            k.op("dve", lambda e: e.tensor_tensor(out=s2[:], in0=s2[:], in1=pt[:, 0:RW], op=ALU.mult), reads=[s2, pt], writes=[s2])
            k.op("dve", lambda e: e.tensor_reduce(out=st[:, 96:112], in_=s2[:].rearrange("p (h n) -> p h n", n=64), axis=AX.X, op=ALU.add),
                 reads=[s2], writes=[st])
            for h in range(16):
                k.op("dve", lambda e, h=h: e.scalar_tensor_tensor(out=s1[:, h * 64:(h + 1) * 64], in0=rt[:, 2 * RW + h * 64:2 * RW + (h + 1) * 64],
                                                                  scalar=st[:, 96 + h:97 + h], in1=s1[:, h * 64:(h + 1) * 64],
                                                                  op0=ALU.mult, op1=ALU.add), reads=[rt, st, s1], writes=[s1])
            k.op("act", lambda e: e.activation(out=gg[:], in_=gg[:], func=AF.Sigmoid), reads=[gg], writes=[gg])
            k.op("dve", lambda e: e.tensor_tensor(out=mix[:, 3072:4096], in0=s1[:], in1=gg[:], op=ALU.mult), reads=[s1, gg], writes=[mix])
            transpose_tile(k, mix, ident, pst, lambda c, j=j: mixT[:, (c * 2 + j) * 128:(c * 2 + j + 1) * 128], mixT)
        for blk in range(D // BW):
            w = wb[blk % 2]
            cs = slice(blk * BW, (blk + 1) * BW)
            for q4 in range(4):
                k.dma(LQ, w[:, q4 * 8:(q4 + 1) * 8, :], Wv[:, q4 * 8:(q4 + 1) * 8, cs], dst=w)
            sets = sorted(set(0 if ti < 8 else 1 for ti in grp))
            for s_ in sets:
                k.dma("sp", gb[s_][:], g1row.t.ap()[s_, cs].partition_broadcast(128), dst=gb[s_])
            for j, ti in enumerate(grp):
                p = pso[it % 3]
                xx = xb[it % 3]
                it += 1
                k.dma("sp", xx[:], xin.t.ap()[ti * 128:(ti + 1) * 128, cs], dst=xx)
                for c in range(NCH):
                    k.op("pe", lambda e, c=c, j=j, p=p: mmr(e, p[:], mixT[:, (c * 2 + j) * 128:(c * 2 + j + 1) * 128], w[:, c, :],
                                                            (c == 0), (c == NCH - 1)), reads=[mixT, w], writes=[p])
                g_ = gb[0 if ti < 8 else 1]
                k.op("dve", lambda e, p=p, g_=g_: e.tensor_tensor(out=p[:], in0=p[:], in1=g_[:], op=ALU.mult), reads=[p, g_], writes=[p])
                k.op("dve", lambda e, p=p, xx=xx: e.tensor_tensor(out=xx[:], in0=xx[:], in1=p[:], op=ALU.add), reads=[p, xx], writes=[xx])
                k.dma(SQ, x1.t.ap()[ti * 128:(ti + 1) * 128, cs], xx[:], src=xx, final=True)
    k.finish()
    return k.nc


def l4a_inmaps(y, sc_x, sc_c, px, ph, attn_x, attn_c, cm_x, cm_c, x, h, mod_l, P):
    ident = np.eye(128, dtype=np.float32)
    prm = np.ascontiguousarray(np.stack([P["rwkv_rk"], P["rwkv_ln_w"], P["rwkv_ln_b"]]))
    in_maps = []
    for i in range(NCORES):
        b, t0, cj = tok_rows(i)
        cat = lambda a, c: np.ascontiguousarray(np.concatenate([a, c], 0))
        lat = slice(t0, t0 + 1024); ct = slice(cj * 128, (cj + 1) * 128)
        yl = np.concatenate([y[0, b, LCTX:][lat], y[1, b, LCTX:][lat]], 1)
        yc = np.concatenate([y[0, b, :LCTX][ct], y[1, b, :LCTX][ct]], 1)
        sel = lambda a: np.concatenate([a[:, 0:RW], a[:, 9 * RW:10 * RW], a[:, 2 * RW:3 * RW]], 1)
        in_maps.append({
            "y01": cat(yl, yc), "rkv": cat(sel(sc_x[b][lat]), sel(sc_c[b][ct])),
            "gt": cat(px[b][lat, 8192:9216], ph[b][ct, 8192:9216]),
            "attn": cat(attn_x[b][lat], attn_c[b][ct]), "cmlp": cat(cm_x[b][lat], cm_c[b][ct]),
            "xin": cat(x[b][lat], h[b][ct]), "prm": prm,
            "g1row": np.ascontiguousarray(np.stack([mod_l[b, 2 * D:3 * D], mod_l[2, 2 * D:3 * D]])),
            "Wo": P["w_out"], "identd": ident})
    return in_maps


def tok_gather(res, key, width, cores=None):
    cores = list(range(NCORES)) if cores is None else cores
    X = np.zeros((2, T, width), np.float32); Hc = np.zeros((2, LCTX, width), np.float32)
    for r, i in zip(res.results, cores):
        b, t0, cj = tok_rows(i)
        X[b, t0:t0 + 1024] = r[key][:1024]
        if (i % 4) < 2:
            Hc[b, cj * 128:(cj + 1) * 128] = r[key][1024:]
    return X, Hc


NE = 16
FF = 1024


def build_l4b(final=False, n_exp=NE):
    k = KB()
    di = lambda n, s: k.dram(n, s, "ExternalInput")
    x1 = di("x1", [NTOK, D]); modcol = di("modcol", [128, 5 * NCH]); g2row = di("g2row", [2, D])
    rwc = di("rwc", [128, NCH * NE]); rb = di("rb", [NE]); fg = di("fg", [D]); identd = di("identd", [128, 128])
    W1 = di("W1", [NE * D, FF]); W3 = di("W3", [NE * D, FF]); W2 = di("W2", [NE * FF, D])
    x2 = k.dram("x2", [NTOK, D], "ExternalOutput")
    xf = k.dram("xf", [NTOK, D], "ExternalOutput") if final else None
    ident = k.sb([128, 128], "ident")
    k.dma("sp", ident[:], identd.t.ap(), dst=ident)
    mc = k.sb([128, 5 * NCH], "mc"); gs = k.sb([128, 4 * NCH], "gs")
    k.dma("sp", mc[:], modcol.t.ap(), dst=mc)
    g = mc[:, 0:NCH]
    for s in range(2):
        sc = mc[:, (1 + 2 * s) * NCH:(2 + 2 * s) * NCH]
        sh = mc[:, (2 + 2 * s) * NCH:(3 + 2 * s) * NCH]
        k.op("dve", lambda e, s=s, sc=sc: e.tensor_tensor(out=gs[:, 2 * s * NCH:(2 * s + 1) * NCH], in0=g, in1=sc, op=ALU.mult),
             reads=[mc], writes=[gs])
        k.op("dve", lambda e, s=s: e.tensor_tensor(out=gs[:, 2 * s * NCH:(2 * s + 1) * NCH], in0=gs[:, 2 * s * NCH:(2 * s + 1) * NCH],
                                                   in1=g, op=ALU.add), reads=[mc, gs], writes=[gs])
        k.op("dve", lambda e, s=s, sh=sh: e.tensor_copy(out=gs[:, (2 * s + 1) * NCH:(2 * s + 2) * NCH], in_=sh), reads=[mc], writes=[gs])
    LQ, SQ = ("pool", "sp") if MM_R[0] else ("sp", "pool")
    rw = k.sb([128, NCH * NE], "rw", mdt()); rbt = k.sb([128, NE], "rbt")
    k.dma(LQ, rw[:], rwc.t.ap(), dst=rw)
    k.dma("sp", rbt[:], rb.t.ap().partition_broadcast(128), dst=rbt)
    xt = k.sb([128, D], "xt"); sq = k.sb([128, D], "sq"); st = k.sb([128, 4], "st")
    znT = k.sb([128, NCH * 256], "znT", mdt())
    acc = [k.sb([128, D], "acc%d" % j) for j in range(2)]
    wh = [k.sb([128, 16, 128], "wh%d" % i, mdt()) for i in range(4)]
    w2s = [k.sb([128, 8, 256], "w2s%d" % i, mdt()) for i in range(2)]
    hidT = k.sb([128, 8 * 256], "hidT", mdt()); sl = k.sb([128, 256], "sl")
    R = [k.sb([128, 160], "R%d" % j) for j in range(2)]
    pst = [k.ps([128, 512], "pst%d" % i) for i in range(2)]
    pH = [k.ps([128, 256], "pH%d" % i) for i in range(2)]
    po = [k.ps([128, 256], "po%d" % i) for i in range(2)]
    pr = k.ps([128, NE], "pr")

    def route(r, j):
        o = lambda fn, rd=(), wr=(): k.op("dve", fn, reads=[r] + list(rd), writes=[r] + list(wr))
        k.op("act", lambda e: e.activation(out=r[:, 0:16], in_=pr[:], func=AF.Sigmoid), reads=[pr], writes=[r])
        o(lambda e: e.tensor_tensor(out=r[:, 16:32], in0=r[:, 0:16], in1=rbt[:], op=ALU.add), rd=[rbt])
        sel3 = r[:, 16:32].rearrange("p (g e) -> p g e", e=4)
        ps6 = r[:, 32:56].rearrange("p (g s) -> p g s", s=6)
        idx = 0
        for a in range(4):
            for b in range(a + 1, 4):
                o(lambda e, a=a, b=b, idx=idx: e.tensor_tensor(out=ps6[:, :, idx], in0=sel3[:, :, a], in1=sel3[:, :, b], op=ALU.add))
                idx += 1
        o(lambda e: e.tensor_reduce(out=r[:, 56:60], in_=ps6, axis=AX.X, op=ALU.max))
        o(lambda e: e.tensor_reduce(out=r[:, 60:61], in_=r[:, 56:60], axis=AX.X, op=ALU.max))
        o(lambda e: e.tensor_scalar(out=r[:, 61:65], in0=r[:, 56:60], scalar1=r[:, 60:61], scalar2=None, op0=ALU.is_equal))
        o(lambda e: e.tensor_reduce(out=r[:, 65:69], in_=sel3, axis=AX.X, op=ALU.max))
        for gi in range(4):
            o(lambda e, gi=gi: e.tensor_scalar(out=r[:, 69 + gi * 4:73 + gi * 4], in0=r[:, 16 + gi * 4:20 + gi * 4],
                                               scalar1=r[:, 65 + gi:66 + gi], scalar2=None, op0=ALU.is_equal))
        o(lambda e: e.scalar_tensor_tensor(out=r[:, 85:101], in0=r[:, 69:85], scalar=-1e30, in1=r[:, 16:32], op0=ALU.mult, op1=ALU.add))
        o(lambda e: e.tensor_reduce(out=r[:, 101:105], in_=r[:, 85:101].rearrange("p (g e) -> p g e", e=4), axis=AX.X, op=ALU.max))
        for gi in range(4):
            o(lambda e, gi=gi: e.tensor_scalar(out=r[:, 105 + gi * 4:109 + gi * 4], in0=r[:, 85 + gi * 4:89 + gi * 4],
                                               scalar1=r[:, 101 + gi:102 + gi], scalar2=None, op0=ALU.is_equal))
        o(lambda e: e.tensor_tensor(out=r[:, 105:121], in0=r[:, 105:121], in1=r[:, 69:85], op=ALU.add))
        for gi in range(4):
            o(lambda e, gi=gi: e.tensor_scalar(out=r[:, 105 + gi * 4:109 + gi * 4], in0=r[:, 105 + gi * 4:109 + gi * 4],
                                               scalar1=r[:, 61 + gi:62 + gi], scalar2=None, op0=ALU.mult))
        o(lambda e: e.tensor_tensor(out=r[:, 121:137], in0=r[:, 105:121], in1=r[:, 0:16], op=ALU.mult))
        o(lambda e: e.tensor_reduce(out=r[:, 137:138], in_=r[:, 121:137], axis=AX.X, op=ALU.add))
        o(lambda e: e.reciprocal(out=r[:, 138:139], in_=r[:, 137:138]))
        o(lambda e: e.tensor_scalar(out=r[:, 139:155], in0=r[:, 121:137], scalar1=r[:, 138:139], scalar2=None, op0=ALU.mult))

    for grp in GROUPS2:
        ng = len(grp)
        ntok = ng * 128
        for j, ti in enumerate(grp):
            k.dma("sp", xt[:], x1.t.ap()[ti * 128:(ti + 1) * 128, :], dst=xt)
            rms_scale(k, xt, sq, st, D)
            s = 0 if ti < 8 else 1
            transpose_tile(k, xt, ident, pst, lambda c, j=j: znT[:, (c * 2 + j) * 128:(c * 2 + j + 1) * 128], znT,
                           gcol=gs[:, 2 * s * NCH:(2 * s + 1) * NCH], scol=gs[:, (2 * s + 1) * NCH:(2 * s + 2) * NCH], mbuf=gs)
            for c in range(NCH):
                k.op("pe", lambda e, c=c, j=j: e.matmul(pr[:], lhsT=znT[:, (c * 2 + j) * 128:(c * 2 + j + 1) * 128],
                                                        rhs=rw[:, c * NE:(c + 1) * NE], start=(c == 0), stop=(c == NCH - 1)),
                     reads=[znT, rw], writes=[pr])
            route(R[j], j)
            k.op("dve", lambda e, j=j: e.memset(acc[j][:], 0.0), writes=[acc[j]])
        for ex in range(n_exp):
            for fc in range(8):
                for wi, Wd in ((0, W1), (2, W3)):
                    for hf in range(2):
                        wbuf = wh[wi + hf]
                        src = Wd.t.ap()[ex * D + hf * 2048:ex * D + (hf + 1) * 2048, :].rearrange("(c p) n -> p c n", p=128)
                        for q2 in range(2):
                            k.dma(LQ, wbuf[:, q2 * 8:(q2 + 1) * 8, :], src[:, q2 * 8:(q2 + 1) * 8, fc * 128:(fc + 1) * 128], dst=wbuf)
                    p = pH[wi // 2]
                    for c in range(NCH):
                        k.op("pe", lambda e, c=c, p=p, wi=wi: mmr(e, p[:, 0:ntok], wh[wi + c // 16][:, c % 16, :],
                                                                  znT[:, c * 256:c * 256 + ntok], (c == 0), (c == NCH - 1)),
                             reads=[wh[wi + c // 16], znT], writes=[p])
                k.op("act", lambda e: e.activation(out=sl[:, 0:ntok], in_=pH[0][:, 0:ntok], func=AF.Silu), reads=[pH[0]], writes=[sl])
                k.op("dve", lambda e, fc=fc: e.tensor_tensor(out=hidT[:, fc * 256:fc * 256 + ntok], in0=sl[:, 0:ntok], in1=pH[1][:, 0:ntok],
                                                             op=ALU.mult), reads=[sl, pH[1]], writes=[hidT])
            for dblk in range(D // 256):
                w2 = w2s[dblk % 2]
                src = W2.t.ap()[ex * FF:(ex + 1) * FF, :].rearrange("(f p) n -> p f n", p=128)
                k.dma(LQ, w2[:], src[:, :, dblk * 256:(dblk + 1) * 256], dst=w2)
                for j in range(ng):
                    p = po[j]
                    for fc in range(8):
                        k.op("pe", lambda e, fc=fc, p=p, j=j: mmr(e, p[:], hidT[:, fc * 256 + j * 128:fc * 256 + (j + 1) * 128],
                                                                  w2[:, fc, :], (fc == 0), (fc == 7)),
                             reads=[hidT, w2], writes=[p])
                    k.op("dve", lambda e, p=p, j=j, dblk=dblk: e.scalar_tensor_tensor(
                        out=acc[j][:, dblk * 256:(dblk + 1) * 256], in0=p[:], scalar=R[j][:, 139 + ex:140 + ex],
                        in1=acc[j][:, dblk * 256:(dblk + 1) * 256], op0=ALU.mult, op1=ALU.add), reads=[p, R[j], acc[j]], writes=[acc[j]])
        for j, ti in enumerate(grp):
            rows = slice(ti * 128, (ti + 1) * 128)
            k.dma("sp", xt[:], x1.t.ap()[rows, :], dst=xt)
            k.dma("sp", sq[:], g2row.t.ap()[0 if ti < 8 else 1, :].partition_broadcast(128), dst=sq)
            k.op("dve", lambda e, j=j: e.tensor_tensor(out=acc[j][:], in0=acc[j][:], in1=sq[:], op=ALU.mult), reads=[acc[j], sq], writes=[acc[j]])
            k.op("dve", lambda e, j=j: e.tensor_tensor(out=acc[j][:], in0=acc[j][:], in1=xt[:], op=ALU.add), reads=[acc[j], xt], writes=[acc[j]])
            k.dma(SQ, x2.t.ap()[rows, :], acc[j][:], src=acc[j], final=True)
            if final:
                k.dma("sp", xt[:], fg.t.ap().partition_broadcast(128), dst=xt)
                rms_scale(k, acc[j], sq, st, D)
                k.op("dve", lambda e, j=j: e.tensor_tensor(out=acc[j][:], in0=acc[j][:], in1=xt[:], op=ALU.mult), reads=[acc[j], xt],
                     writes=[acc[j]])
                k.dma(SQ, xf.t.ap()[rows, :], acc[j][:], src=acc[j], final=True)
    k.finish()
    return k.nc


def l4b_inmaps(x1x, x1c, mod_l, norm2_g_l, router_w, router_b, w1, w3, w2, final_g):
    ident = np.eye(128, dtype=np.float32)
    rwc = np.ascontiguousarray(router_w.reshape(NCH, 128, NE).transpose(1, 0, 2).reshape(128, NCH * NE))
    W1 = w1.reshape(NE * D, FF); W3 = w3.reshape(NE * D, FF); W2 = w2.reshape(NE * FF, D)
    in_maps = []
    for i in range(NCORES):
        b, t0, cj = tok_rows(i)
        mc = np.concatenate([col_layout(norm2_g_l), col_layout(mod_l[b, 4 * D:5 * D]), col_layout(mod_l[b, 3 * D:4 * D]),
                             col_layout(mod_l[2, 4 * D:5 * D]), col_layout(mod_l[2, 3 * D:4 * D])], 1)
        in_maps.append({"x1": np.ascontiguousarray(np.concatenate([x1x[b, t0:t0 + 1024], x1c[b, cj * 128:(cj + 1) * 128]], 0)),
                        "modcol": np.ascontiguousarray(mc),
                        "g2row": np.ascontiguousarray(np.stack([mod_l[b, 5 * D:6 * D], mod_l[2, 5 * D:6 * D]])),
                        "rwc": rwc, "rb": np.ascontiguousarray(router_b), "fg": np.ascontiguousarray(final_g), "identd": ident,
                        "W1": W1, "W3": W3, "W2": W2})
    return in_maps


_ALL = list(range(NCORES))


def _run(nc, in_maps):
    return run_bass_kernel_spmd(nc, in_maps, core_ids=_ALL)


def kernel(x, c, ctx, c_ctx, ada_w, ada_b, norm1_g, w_in, rwkv_conv, attn_sink, cmlp_norm_g, cmlp_ws, cmlp_b,
           rwkv_w0, rwkv_w1, rwkv_w2, rwkv_a0, rwkv_a1, rwkv_a2, rwkv_kk, rwkv_ka, rwkv_rk, rwkv_ln_w, rwkv_ln_b,
           w_out, norm2_g, router_w, router_b, moe_w1, moe_w3, moe_w2, final_g):
    f = lambda a: np.ascontiguousarray(np.asarray(a, dtype=np.float32))
    x, h = f(x), f(ctx)
    mod = run_ada(f(c), f(c_ctx), np.asarray(ada_w), np.asarray(ada_b))
    xf = None
    for l in range(2):
        P = dict(attn_sink=f(attn_sink[l]), cmlp_norm_g=f(cmlp_norm_g[l]), cmlp_ws=f(cmlp_ws[l]), cmlp_b=f(cmlp_b[l]),
                 rwkv_conv=f(rwkv_conv[l]), rwkv_w0=f(rwkv_w0[l]), rwkv_w2=f(rwkv_w2[l]), rwkv_a0=f(rwkv_a0[l]),
                 rwkv_a2=f(rwkv_a2[l]), rwkv_kk=f(rwkv_kk[l]), rwkv_ka=f(rwkv_ka[l]), rwkv_rk=f(rwkv_rk[l]),
                 rwkv_ln_w=f(rwkv_ln_w[l]), rwkv_ln_b=f(rwkv_ln_b[l]), w_out=f(w_out[l]))
        Wcat = np.ascontiguousarray(np.concatenate([w_in[l], rwkv_w1[l][0], rwkv_w1[l][1], rwkv_a1[l][0], rwkv_a1[l][1]], 1))
        px, ph = run_l1(x, h, mod[l], f(norm1_g[l]), Wcat)
        del Wcat
        attn_x, attn_c, cm_x, cm_c, sc_x, sc_c = l2_gather(_run(build_l2(), l2_inmaps(px, ph, P)))
        y = l3_gather(_run(build_l3(), l3_inmaps(sc_x, sc_c)))
        x1x, x1c = tok_gather(_run(build_l4a(), l4a_inmaps(y, sc_x, sc_c, px, ph, attn_x, attn_c, cm_x, cm_c, x, h, mod[l], P)),
                              "x1", D)
        del px, ph, attn_x, attn_c, cm_x, cm_c, sc_x, sc_c, y
        last = l == 1
        res = _run(build_l4b(final=last), l4b_inmaps(x1x, x1c, mod[l], f(norm2_g[l]), f(router_w), f(router_b),
                                                     f(moe_w1[l]), f(moe_w3[l]), f(moe_w2[l]), f(final_g)))
        x, h = tok_gather(res, "x2", D)
        if last:
            xf, _ = tok_gather(res, "xf", D)
    return xf
```

```python
import numpy as np
import concourse.bass as bass
import concourse.mybir as mybir
from concourse.bass_utils import run_bass_kernel_spmd

F32 = mybir.dt.float32
AF = mybir.ActivationFunctionType
ALU = mybir.AluOpType
AX = mybir.AxisListType

NCORES = 8
F32R = mybir.dt.float32r
MM_R = [1]


def mmr(e, out, lhsT, rhs, start, stop):
    return e.matmul(out, lhsT=lhsT, rhs=rhs, start=start, stop=stop)


BF16 = mybir.dt.bfloat16


def mdt():
    return {False: F32, True: F32R, 1: F32R, 2: BF16}[MM_R[0]]
D = 4096
T = 4096
LCTX = 256
DPROJ = 9216
NCH = D // 128


class Buf:
    __slots__ = ("name", "t", "w", "r", "dsem", "dcnt")

    def __init__(self, name, t):
        self.name = name
        self.t = t
        self.w = None
        self.r = {}
        self.dsem = None
        self.dcnt = 0

    def __getitem__(self, idx):
        return self.t[idx]


class KB:
    def __init__(self):
        self.nc = bass.Bass("TRN2", target_bir_lowering=False)
        nc = self.nc
        self.E = {"pe": nc.tensor, "dve": nc.vector, "act": nc.scalar, "pool": nc.gpsimd, "sp": nc.sync}
        self.sem = {e: nc.alloc_semaphore("c_" + e) for e in self.E}
        self.cnt = {e: 0 for e in self.E}
        self.seen = {e: {} for e in self.E}
        self.nb = 0
        self.final = {}

    def sb(self, shape, name=None, dtype=F32):
        self.nb += 1
        name = name or "sb%d" % self.nb
        return Buf(name, self.nc.alloc_sbuf_tensor(name, list(shape), dtype))

    def ps(self, shape, name=None, dtype=F32):
        self.nb += 1
        name = name or "ps%d" % self.nb
        return Buf(name, self.nc.alloc_psum_tensor(name, list(shape), dtype))

    def dram(self, name, shape, kind, dtype=F32):
        return Buf(name, self.nc.dram_tensor(name, list(shape), dtype, kind=kind))

    def _wait(self, eng, deps):
        for k, v in deps.items():
            if self.seen[eng].get(k, 0) < v:
                self.E[eng].wait_ge(self.sem[k], v)
                self.seen[eng][k] = v

    @staticmethod
    def _add(deps, d):
        if d is not None:
            k, v = d
            if deps.get(k, 0) < v:
                deps[k] = v

    def op(self, eng, fn, reads=(), writes=()):
        deps = {}
        for b in reads:
            self._add(deps, b.w)
        for b in writes:
            self._add(deps, b.w)
            for k, v in b.r.items():
                self._add(deps, (k, v))
        if eng == "pe":
            deps.pop("pe", None)
        self._wait(eng, deps)
        inst = fn(self.E[eng])
        self.cnt[eng] += 1
        c = self.cnt[eng]
        inst.then_inc(self.sem[eng], 1)
        for b in reads:
            b.r[eng] = c
        for b in writes:
            b.w = (eng, c)
            b.r = {}
        return inst

    def dma(self, q, out, in_, src=None, dst=None, owner=None, final=False, **kw):
        owner = owner or (dst if dst is not None else src)
        if owner.dsem is None:
            owner.dsem = "d_" + owner.name
            self.sem[owner.dsem] = self.nc.alloc_semaphore(owner.dsem)
        deps = {}
        if src is not None:
            self._add(deps, src.w)
        if dst is not None:
            self._add(deps, dst.w)
            for k, v in dst.r.items():
                self._add(deps, (k, v))
        self._wait(q, deps)
        inst = self.E[q].dma_start(out=out, in_=in_, **kw)
        owner.dcnt += 16
        inst.then_inc(self.sem[owner.dsem], 16)
        if src is not None:
            src.r[owner.dsem] = owner.dcnt
        if dst is not None:
            dst.w = (owner.dsem, owner.dcnt)
            dst.r = {}
        if final:
            self.final[owner.dsem] = owner.dcnt
        return inst

    def finish(self):
        self._wait("sp", dict(self.final))


ADA_COLS = 2 * 6 * D // NCORES


def build_ada():
    k = KB()
    ccT = k.dram("ccT", [128, NCH * 3], "ExternalInput")
    W = k.dram("W", [D, ADA_COLS], "ExternalInput")
    bias = k.dram("bias", [ADA_COLS], "ExternalInput")
    out = k.dram("out", [3, ADA_COLS], "ExternalOutput")
    cc = k.sb([128, NCH * 3], "cc")
    bt = k.sb([3, ADA_COLS], "bt")
    ot = k.sb([3, ADA_COLS], "ot")
    wb = [k.sb([128, NCH, 512], "wb%d" % i) for i in range(2)]
    pb = [k.ps([3, 512], "pb%d" % i) for i in range(2)]
    k.dma("sp", cc[:], ccT.t.ap(), dst=cc)
    k.dma("sp", bt[:], bias.t.ap().partition_broadcast(3), dst=bt)
    k.op("act", lambda e: e.activation(out=cc[:], in_=cc[:], func=AF.Silu), reads=[cc], writes=[cc])
    Wv = W.t.ap().rearrange("(c p) n -> p c n", p=128)
    ncb = ADA_COLS // 512
    for cb in range(ncb):
        w = wb[cb % 2]
        for q4 in range(4):
            k.dma("sp", w[:, q4 * 8:(q4 + 1) * 8, :], Wv[:, q4 * 8:(q4 + 1) * 8, cb * 512:(cb + 1) * 512], dst=w)
        p = pb[cb % 2]
        for c in range(NCH):
            k.op("pe", lambda e, c=c: e.matmul(p[:], lhsT=cc[:, c * 3:(c + 1) * 3], rhs=w[:, c, :],
                                                start=(c == 0), stop=(c == NCH - 1)),
                 reads=[cc, w], writes=[p])
        k.op("dve", lambda e: e.tensor_tensor(out=ot[:, cb * 512:(cb + 1) * 512], in0=p[:],
                                              in1=bt[:, cb * 512:(cb + 1) * 512], op=ALU.add),
             reads=[p, bt], writes=[ot])
    k.dma("sp", out.t.ap(), ot[:], src=ot, final=True)
    k.finish()
    return k.nc


def run_ada(c, c_ctx, ada_w, ada_b):
    cc = np.concatenate([c, c_ctx[None]], 0)
    ccT = np.ascontiguousarray(cc.T.reshape(NCH, 128, 3).transpose(1, 0, 2).reshape(128, NCH * 3))
    per = 6 * D // NCORES
    in_maps = []
    for i in range(NCORES):
        Wc = np.ascontiguousarray(np.concatenate([ada_w[l][:, i * per:(i + 1) * per] for l in range(2)], 1))
        bc = np.ascontiguousarray(np.concatenate([ada_b[l][i * per:(i + 1) * per] for l in range(2)], 0))
        in_maps.append({"ccT": ccT, "W": Wc, "bias": bc})
    res = run_bass_kernel_spmd(build_ada(), in_maps, core_ids=list(range(NCORES)))
    mod = np.zeros((2, 3, 6 * D), np.float32)
    for i in range(NCORES):
        o = res.results[i]["out"]
        for l in range(2):
            mod[l][:, i * per:(i + 1) * per] = o[:, l * per:(l + 1) * per]
    return mod


NT = 9
NTOK = NT * 128


def rms_scale(k, xt, sq, st, width):
    k.op("dve", lambda e: e.tensor_tensor(out=sq[:, :width], in0=xt[:, :width], in1=xt[:, :width], op=ALU.mult),
         reads=[xt], writes=[sq])
    k.op("dve", lambda e: e.tensor_reduce(out=st[:, 0:1], in_=sq[:, :width], axis=AX.X, op=ALU.add),
         reads=[sq], writes=[st])
    k.op("dve", lambda e: e.tensor_scalar(out=st[:, 1:2], in0=st[:, 0:1], scalar1=1.0 / width, scalar2=1e-6,
                                          op0=ALU.mult, op1=ALU.add), reads=[st], writes=[st])
    k.op("act", lambda e: e.activation(out=st[:, 2:3], in_=st[:, 1:2], func=AF.Sqrt), reads=[st], writes=[st])
    k.op("dve", lambda e: e.reciprocal(out=st[:, 3:4], in_=st[:, 2:3]), reads=[st], writes=[st])
    k.op("dve", lambda e: e.tensor_scalar(out=xt[:, :width], in0=xt[:, :width], scalar1=st[:, 3:4], scalar2=None,
                                          op0=ALU.mult), reads=[xt, st], writes=[xt])


def transpose_tile(k, xt, ident, pst, xT_dst, dst_buf, gcol=None, scol=None, mbuf=None, nch=NCH):
    for c4 in range(0, nch, 4):
        p = pst[(c4 // 4) % len(pst)]
        n4 = min(4, nch - c4)
        for j in range(n4):
            c = c4 + j
            k.op("pe", lambda e, c=c, j=j: e.transpose(out=p[:, j * 128:(j + 1) * 128],
                                                       in_=xt[:, c * 128:(c + 1) * 128], identity=ident[:]),
                 reads=[xt, ident], writes=[p])
        for j in range(n4):
            c = c4 + j
            if gcol is not None:
                k.op("dve", lambda e, c=c, j=j: e.tensor_scalar(out=xT_dst(c), in0=p[:, j * 128:(j + 1) * 128],
                                                                scalar1=gcol[:, c:c + 1], scalar2=scol[:, c:c + 1],
                                                                op0=ALU.mult, op1=ALU.add),
                     reads=[p, mbuf], writes=[dst_buf])
            else:
                k.op("act", lambda e, c=c, j=j: e.copy(out=xT_dst(c), in_=p[:, j * 128:(j + 1) * 128]),
                     reads=[p], writes=[dst_buf])


NC1 = DPROJ + 512
GT = 3


def build_l1():
    k = KB()
    xin = k.dram("xin", [NTOK, D], "ExternalInput")
    modcol = k.dram("modcol", [128, 5 * NCH], "ExternalInput")
    identd = k.dram("identd", [128, 128], "ExternalInput")
    W = k.dram("W", [D, NC1], "ExternalInput")
    proj = k.dram("proj", [NTOK, NC1], "ExternalOutput")
    ident = k.sb([128, 128], "ident")
    mc = k.sb([128, 5 * NCH], "mc")
    gs = k.sb([128, 4 * NCH], "gs")
    xt = [k.sb([128, D], "xt%d" % i) for i in range(2)]
    sq = k.sb([128, D], "sq")
    st = k.sb([128, 4], "st")
    xT = k.sb([128, GT * NCH * 128], "xT", mdt())
    BW = 256
    wb = [k.sb([128, NCH, BW], "wb%d" % i, mdt()) for i in range(2)]
    og = [k.sb([128, BW], "og%d" % i) for i in range(3)]
    pst = [k.ps([128, 512], "pst%d" % i) for i in range(2)]
    pso = [k.ps([128, BW], "pso%d" % i) for i in range(3)]
    k.dma("sp", ident[:], identd.t.ap(), dst=ident)
    k.dma("sp", mc[:], modcol.t.ap(), dst=mc)
    g = mc[:, 0:NCH]
    for s in range(2):
        sc = mc[:, (1 + 2 * s) * NCH:(2 + 2 * s) * NCH]
        sh = mc[:, (2 + 2 * s) * NCH:(3 + 2 * s) * NCH]
        k.op("dve", lambda e, s=s, sc=sc: e.tensor_tensor(out=gs[:, 2 * s * NCH:(2 * s + 1) * NCH], in0=g, in1=sc,
                                                          op=ALU.mult), reads=[mc], writes=[gs])
        k.op("dve", lambda e, s=s: e.tensor_tensor(out=gs[:, 2 * s * NCH:(2 * s + 1) * NCH],
                                                   in0=gs[:, 2 * s * NCH:(2 * s + 1) * NCH], in1=g, op=ALU.add),
             reads=[mc, gs], writes=[gs])
        k.op("dve", lambda e, s=s, sh=sh: e.tensor_copy(out=gs[:, (2 * s + 1) * NCH:(2 * s + 2) * NCH], in_=sh),
             reads=[mc], writes=[gs])
    Wv = W.t.ap().rearrange("(c p) n -> p c n", p=128)
    nblk = NC1 // BW
    it = 0
    for grp in range(NT // GT):
        for j in range(GT):
            ti = grp * GT + j
            x = xt[ti % 2]
            k.dma("sp", x[:], xin.t.ap()[ti * 128:(ti + 1) * 128, :], dst=x)
            rms_scale(k, x, sq, st, D)
            s = 0 if ti < 8 else 1
            transpose_tile(k, x, ident, pst,
                           lambda c, j=j: xT[:, (j * NCH + c) * 128:(j * NCH + c + 1) * 128], xT,
                           gcol=gs[:, 2 * s * NCH:(2 * s + 1) * NCH], scol=gs[:, (2 * s + 1) * NCH:(2 * s + 2) * NCH],
                           mbuf=gs)
        for blk in range(nblk):
            w = wb[blk % 2]
            for q4 in range(4):
                k.dma("pool" if MM_R[0] else "sp", w[:, q4 * 8:(q4 + 1) * 8, :], Wv[:, q4 * 8:(q4 + 1) * 8, blk * BW:(blk + 1) * BW], dst=w)
            for j in range(GT):
                ti = grp * GT + j
                p = pso[it % 3]
                o = og[it % 3]
                it += 1
                for c in range(NCH):
                    k.op("pe", lambda e, c=c, j=j: mmr(e, p[:], xT[:, (j * NCH + c) * 128:(j * NCH + c + 1) * 128],
                                                       w[:, c, :], (c == 0), (c == NCH - 1)),
                         reads=[xT, w], writes=[p])
                k.op("act", lambda e: e.copy(out=o[:], in_=p[:]), reads=[p], writes=[o])
                k.dma("sp" if MM_R[0] else "pool", proj.t.ap()[ti * 128:(ti + 1) * 128, blk * BW:(blk + 1) * BW], o[:], src=o, final=True)
    k.finish()
    return k.nc


def tok_rows(i):
    return i // 4, (i % 4) * 1024, (i % 4) % 2


def col_layout(v):
    return np.ascontiguousarray(v.reshape(NCH, 128).T)


def run_l1(x, h, mod_l, norm1_g_l, Wcat):
    ident = np.eye(128, dtype=np.float32)
    in_maps = []
    for i in range(NCORES):
        b, t0, cj = tok_rows(i)
        xin = np.ascontiguousarray(np.concatenate([x[b, t0:t0 + 1024], h[b, cj * 128:(cj + 1) * 128]], 0))
        mc = np.concatenate([col_layout(norm1_g_l), col_layout(mod_l[b, D:2 * D]), col_layout(mod_l[b, 0:D]),
                             col_layout(mod_l[2, D:2 * D]), col_layout(mod_l[2, 0:D])], 1)
        in_maps.append({"xin": xin, "modcol": np.ascontiguousarray(mc), "identd": ident, "W": Wcat})
    res = run_bass_kernel_spmd(build_l1(), in_maps, core_ids=list(range(NCORES)))
    px = np.zeros((2, T, NC1), np.float32)
    ph = np.zeros((2, LCTX, NC1), np.float32)
    for i in range(NCORES):
        b, t0, cj = tok_rows(i)
        o = res.results[i]["proj"]
        px[b, t0:t0 + 1024] = o[:1024]
        if (i % 4) < 2:
            ph[b, cj * 128:(cj + 1) * 128] = o[1024:]
    return px, ph


HD = 128
NH = 16
NKV = 4
CW = 1024
RW = 1024
GELU_C = 1.5957691216057308


def gelu_tanh(k, x, t1, w):
    k.op("dve", lambda e: e.tensor_tensor(out=t1[:, :w], in0=x[:, :w], in1=x[:, :w], op=ALU.mult), reads=[x], writes=[t1])
    k.op("dve", lambda e: e.tensor_scalar(out=t1[:, :w], in0=t1[:, :w], scalar1=0.044715, scalar2=1.0, op0=ALU.mult,
                                          op1=ALU.add), reads=[t1], writes=[t1])
    k.op("dve", lambda e: e.tensor_tensor(out=t1[:, :w], in0=t1[:, :w], in1=x[:, :w], op=ALU.mult), reads=[t1, x], writes=[t1])
    k.op("act", lambda e: e.activation(out=t1[:, :w], in_=t1[:, :w], func=AF.Sigmoid, scale=GELU_C), reads=[t1], writes=[t1])
    k.op("dve", lambda e: e.tensor_tensor(out=x[:, :w], in0=x[:, :w], in1=t1[:, :w], op=ALU.mult), reads=[t1, x], writes=[x])


def rope(k, x, c, s, t1, w):
    def v(b, e):
        return b[:, :w].rearrange("p (a two s) -> p a two s", two=2, s=32)[:, :, e, :]
    for e_ in range(2):
        k.op("dve", lambda e, e_=e_: e.tensor_tensor(out=v(t1, e_), in0=v(x, 1 - e_), in1=v(s, e_), op=ALU.mult),
             reads=[x, s], writes=[t1])
    k.op("dve", lambda e: e.tensor_tensor(out=x[:, :w], in0=x[:, :w], in1=c[:, :w], op=ALU.mult), reads=[x, c], writes=[x])
    k.op("dve", lambda e: e.tensor_tensor(out=x[:, :w], in0=x[:, :w], in1=t1[:, :w], op=ALU.add), reads=[x, t1], writes=[x])


def build_l2(do_attn=True, do_cmlp=True, do_rwkv=True):
    k = KB()
    di = lambda n, s: k.dram(n, s, "ExternalInput")
    qx = di("qx", [NTOK, 2048]); kh = di("kh", [1280, 512]); vh = di("vh", [1280, 512])
    kc = di("kc", [256, 512]); vc = di("vc", [256, 512])
    rqc = di("rqc", [1024, 2048]); rqs = di("rqs", [1024, 2048]); rkc = di("rkc", [1280, 512]); rks = di("rks", [1280, 512])
    masks = di("masks", [4 * 128, 512]); sink = di("sink", [16])
    ug = di("ug", [NTOK, 2048]); cng = di("cng", [CW]); wsT = di("wsT", [128, 1024]); bsT = di("bsT", [128, 8])
    rkvp = di("rkvp", [1026 + 130, 3072]); lora = di("lora", [NTOK, 512]); convw = di("convw", [3, 3072])
    w2a2 = di("w2a2", [128, 4096]); w0a0 = di("w0a0", [1, 4096]); kkp = di("kkp", [RW]); kap = di("kap", [RW])
    identd = di("identd", [128, 128])
    attn = k.dram("attn", [NTOK, 2048], "ExternalOutput")
    cmlp = k.dram("cmlp", [NTOK, CW], "ExternalOutput")
    scin = k.dram("scin", [NTOK, 10 * RW], "ExternalOutput")

    ident = k.sb([128, 128], "ident")
    k.dma("sp", ident[:], identd.t.ap(), dst=ident)
    WK = [k.sb([128, 3072], "wk%d" % i) for i in range(6)]
    pst = [k.ps([128, 512], "pst%d" % i) for i in range(2)]
    psA = [k.ps([128, 512], "psA%d" % i) for i in range(3)]
    psB = [k.ps([128, 512], "psB%d" % i) for i in range(2)]
    scale = float(HD) ** -0.5

    if do_attn:
        kT = k.sb([128, NKV * 1280], "kT")
        va = k.sb([128, 10 * NKV * 129], "va")
        kcT = k.sb([128, NKV * 256], "kcT")
        vca = k.sb([128, 2 * NKV * 129], "vca")
        mk = k.sb([128, 4 * 512], "mk")
        esk = k.sb([128, 16], "esk")
        st = k.sb([128, 8], "stA")
        for m in range(4):
            k.dma("sp", mk[:, m * 512:(m + 1) * 512], masks.t.ap()[m * 128:(m + 1) * 128, :], dst=mk)
        k.dma("sp", esk[:], sink.t.ap().partition_broadcast(128), dst=esk)
        k.op("act", lambda e: e.activation(out=esk[:], in_=esk[:], func=AF.Exp), reads=[esk], writes=[esk])
        k.op("dve", lambda e: e.memset(va[:], 1.0), writes=[va])
        k.op("dve", lambda e: e.memset(vca[:], 1.0), writes=[vca])
        va4 = lambda blk, h: va[:, (blk * NKV + h) * 129:(blk * NKV + h) * 129 + 129]
        vca4 = lambda blk, h: vca[:, (blk * NKV + h) * 129:(blk * NKV + h) * 129 + 129]
        kt_, vt_, c_, s_, t_ = WK[0], WK[1], WK[2], WK[3], WK[4]
        for blk in range(12):
            isctx = blk >= 10
            src_k = kc.t.ap()[(blk - 10) * 128:(blk - 9) * 128, :] if isctx else kh.t.ap()[blk * 128:(blk + 1) * 128, :]
            src_v = vc.t.ap()[(blk - 10) * 128:(blk - 9) * 128, :] if isctx else vh.t.ap()[blk * 128:(blk + 1) * 128, :]
            k.dma("sp", kt_[:, :512], src_k, dst=kt_)
            k.dma("sp", vt_[:, :512], src_v, dst=vt_)
            if not isctx:
                k.dma("sp", c_[:, :512], rkc.t.ap()[blk * 128:(blk + 1) * 128, :], dst=c_)
                k.dma("sp", s_[:, :512], rks.t.ap()[blk * 128:(blk + 1) * 128, :], dst=s_)
                rope(k, kt_, c_, s_, t_, 512)
                transpose_tile(k, kt_, ident, pst, lambda c, blk=blk: kT[:, c * 1280 + blk * 128:c * 1280 + (blk + 1) * 128],
                               kT, nch=NKV)
            else:
                transpose_tile(k, kt_, ident, pst,
                               lambda c, blk=blk: kcT[:, c * 256 + (blk - 10) * 128:c * 256 + (blk - 9) * 128], kcT, nch=NKV)
            for h in range(NKV):
                dst = vca4(blk - 10, h) if isctx else va4(blk, h)
                k.op("dve", lambda e, dst=dst, h=h: e.tensor_copy(out=dst[:, 0:128], in_=vt_[:, h * 128:(h + 1) * 128]),
                     reads=[vt_], writes=[vca if isctx else va])
        qt, qT, PT, ao = WK[0], WK[1], WK[2], WK[3]
        c_, s_, t_ = WK[4], WK[5], WK[2]
        for ti in range(NT):
            isctx = ti == 8
            k.dma("sp", qt[:, :2048], qx.t.ap()[ti * 128:(ti + 1) * 128, :], dst=qt)
            if not isctx:
                k.dma("sp", c_[:, :2048], rqc.t.ap()[ti * 128:(ti + 1) * 128, :], dst=c_)
                k.dma("sp", s_[:, :2048], rqs.t.ap()[ti * 128:(ti + 1) * 128, :], dst=s_)
                rope(k, qt, c_, s_, t_, 2048)
            transpose_tile(k, qt, ident, pst, lambda c: qT[:, c * 128:(c + 1) * 128], qT, nch=NH)
            chunks = []
            if not isctx:
                for d_ in range(3):
                    blk = ti + d_
                    m = None
                    if d_ == 0:
                        m = 0 if ti == 0 else 1
                    if d_ == 2:
                        m = 3 if ti == 7 else 2
                    chunks.append((lambda h, blk=blk: kT[:, h * 1280 + blk * 128:h * 1280 + (blk + 1) * 128],
                                   lambda h, blk=blk: va4(blk, h), va, kT, m))
            for cb in range(2):
                chunks.append((lambda h, cb=cb: kcT[:, h * 256 + cb * 128:h * 256 + (cb + 1) * 128],
                               lambda h, cb=cb: vca4(cb, h), vca, kcT, None))
            for h in range(NKV):
                for ci, (kf, vf, vbuf, kbuf, m) in enumerate(chunks):
                    p = psA[ci % 3]
                    k.op("pe", lambda e, kf=kf, p=p: e.matmul(p[:], lhsT=kf(h), rhs=qT[:, h * 512:(h + 1) * 512],
                                                              start=True, stop=True), reads=[kbuf, qT], writes=[p])
                    k.op("act", lambda e, ci=ci, p=p: e.activation(out=PT[:, ci * 512:(ci + 1) * 512], in_=p[:], func=AF.Exp,
                                                                   scale=scale), reads=[p], writes=[PT])
                    if m is not None:
                        k.op("dve", lambda e, ci=ci, m=m: e.tensor_tensor(out=PT[:, ci * 512:(ci + 1) * 512],
                                                                          in0=PT[:, ci * 512:(ci + 1) * 512],
                                                                          in1=mk[:, m * 512:(m + 1) * 512], op=ALU.mult),
                             reads=[PT, mk], writes=[PT])
                for g in range(4):
                    hq = h * 4 + g
                    p = psB[g % 2]
                    for ci, (kf, vf, vbuf, kbuf, m) in enumerate(chunks):
                        k.op("pe", lambda e, ci=ci, vf=vf, p=p: e.matmul(p[:, 0:129],
                                                                         lhsT=PT[:, ci * 512 + g * 128:ci * 512 + (g + 1) * 128],
                                                                         rhs=vf(h), start=(ci == 0), stop=(ci == len(chunks) - 1)),
                             reads=[PT, vbuf], writes=[p])
                    k.op("dve", lambda e, p=p, hq=hq: e.tensor_tensor(out=st[:, 0:1], in0=p[:, 128:129], in1=esk[:, hq:hq + 1],
                                                                      op=ALU.add), reads=[p, esk], writes=[st])
                    k.op("dve", lambda e: e.reciprocal(out=st[:, 1:2], in_=st[:, 0:1]), reads=[st], writes=[st])
                    k.op("dve", lambda e, p=p, hq=hq: e.tensor_scalar(out=ao[:, hq * 128:(hq + 1) * 128], in0=p[:, 0:128],
                                                                      scalar1=st[:, 1:2], scalar2=None, op0=ALU.mult),
                         reads=[p, st], writes=[ao])
            k.dma("pool", attn.t.ap()[ti * 128:(ti + 1) * 128, :], ao[:, :2048], src=ao, final=True)

    if do_cmlp:
        ng = k.sb([128, CW], "ng"); wst = k.sb([128, 1024], "wst"); bst = k.sb([128, 8], "bst")
        stc = k.sb([128, 4], "stC")
        k.dma("sp", ng[:], cng.t.ap().partition_broadcast(128), dst=ng)
        k.dma("sp", wst[:], wsT.t.ap(), dst=wst)
        k.dma("sp", bst[:], bsT.t.ap(), dst=bst)
        ut, t1, sq, cm = WK[0], WK[1], WK[2], WK[3]
        for ti in range(NT):
            k.dma("sp", ut[:, :2048], ug.t.ap()[ti * 128:(ti + 1) * 128, :], dst=ut)
            gelu_tanh(k, ut, t1, 2048)
            gvv = Buf("gvview", None)
            k.op("dve", lambda e: e.tensor_tensor(out=sq[:, :CW], in0=ut[:, CW:2 * CW], in1=ut[:, CW:2 * CW], op=ALU.mult),
                 reads=[ut], writes=[sq])
            k.op("dve", lambda e: e.tensor_reduce(out=stc[:, 0:1], in_=sq[:, :CW], axis=AX.X, op=ALU.add), reads=[sq], writes=[stc])
            k.op("dve", lambda e: e.tensor_scalar(out=stc[:, 1:2], in0=stc[:, 0:1], scalar1=1.0 / CW, scalar2=1e-6,
                                                  op0=ALU.mult, op1=ALU.add), reads=[stc], writes=[stc])
            k.op("act", lambda e: e.activation(out=stc[:, 2:3], in_=stc[:, 1:2], func=AF.Sqrt), reads=[stc], writes=[stc])
            k.op("dve", lambda e: e.reciprocal(out=stc[:, 3:4], in_=stc[:, 2:3]), reads=[stc], writes=[stc])
            k.op("dve", lambda e: e.scalar_tensor_tensor(out=sq[:, :CW], in0=ut[:, CW:2 * CW], scalar=stc[:, 3:4], in1=ng[:],
                                                         op0=ALU.mult, op1=ALU.mult), reads=[ut, stc, ng], writes=[sq])
            for g in range(8):
                p = psA[(g // 4) % 3]
                k.op("pe", lambda e, g=g, p=p: e.matmul(p[:, (g % 4) * 128:(g % 4 + 1) * 128], lhsT=wst[:, g * 128:(g + 1) * 128],
                                                        rhs=sq[:, g * 128:(g + 1) * 128], start=True, stop=True),
                     reads=[wst, sq], writes=[p])
            for g in range(8):
                p = psA[(g // 4) % 3]
                k.op("dve", lambda e, g=g, p=p: e.scalar_tensor_tensor(out=cm[:, g * 128:(g + 1) * 128],
                                                                       in0=p[:, (g % 4) * 128:(g % 4 + 1) * 128],
                                                                       scalar=bst[:, g:g + 1], in1=ut[:, g * 128:(g + 1) * 128],
                                                                       op0=ALU.add, op1=ALU.mult), reads=[p, bst, ut], writes=[cm])
            k.dma("pool", cmlp.t.ap()[ti * 128:(ti + 1) * 128, :], cm[:, :CW], src=cm, final=True)

    if do_rwkv:
        w2t = k.sb([128, 4096], "w2t"); w0t = k.sb([1, 4096], "w0t"); ones = k.sb([1, 128], "ones1")
        kkpt = k.sb([128, RW], "kkpt"); kapt = k.sb([128, RW], "kapt")
        lt = k.sb([128, 512], "lt"); lT = k.sb([128, 512], "lT")
        ss = k.sb([128, 64], "ss")
        k.dma("sp", w2t[:], w2a2.t.ap(), dst=w2t)
        k.dma("sp", w0t[:], w0a0.t.ap(), dst=w0t)
        k.dma("sp", kkpt[:], kkp.t.ap().partition_broadcast(128), dst=kkpt)
        k.dma("sp", kapt[:], kap.t.ap().partition_broadcast(128), dst=kapt)
        k.op("dve", lambda e: e.memset(ones[:], 1.0), writes=[ones])
        xs, cw, o1, kf, kk, o2 = WK
        for ti in range(NT):
            r0 = ti * 128 if ti < 8 else 1026
            outs = {}
            for part in range(3):
                for j in range(3):
                    k.dma("sp", xs[:, j * 1024:(j + 1) * 1024], rkvp.t.ap()[r0 + j:r0 + j + 128, part * 1024:(part + 1) * 1024], dst=xs)
                    k.dma("sp", cw[:, j * 1024:(j + 1) * 1024],
                          convw.t.ap()[j, part * 1024:(part + 1) * 1024].partition_broadcast(128), dst=cw)
                dstb = kf if part == 1 else o1
                k.op("dve", lambda e: e.tensor_tensor(out=xs[:], in0=xs[:], in1=cw[:], op=ALU.mult), reads=[xs, cw], writes=[xs])
                k.op("dve", lambda e, dstb=dstb: e.tensor_tensor(out=dstb[:, :RW], in0=xs[:, 0:RW], in1=xs[:, RW:2 * RW], op=ALU.add),
                     reads=[xs], writes=[dstb])
                k.op("dve", lambda e, dstb=dstb: e.tensor_tensor(out=dstb[:, :RW], in0=dstb[:, :RW], in1=xs[:, 2 * RW:3 * RW], op=ALU.add),
                     reads=[xs, dstb], writes=[dstb])
                qi = (0, 9, 2)[part]
                k.dma("pool", scin.t.ap()[ti * 128:(ti + 1) * 128, qi * RW:(qi + 1) * RW], dstb[:, :RW], src=dstb, final=True)
            k.op("dve", lambda e: e.tensor_tensor(out=kk[:, :RW], in0=kf[:, :RW], in1=kkpt[:], op=ALU.mult), reads=[kf, kkpt], writes=[kk])
            k.op("dve", lambda e: e.tensor_tensor(out=o2[:, :RW], in0=kk[:, :RW], in1=kk[:, :RW], op=ALU.mult), reads=[kk], writes=[o2])
            k.op("dve", lambda e: e.tensor_reduce(out=ss[:, 0:16], in_=o2[:, :RW].rearrange("p (h n) -> p h n", n=64), axis=AX.X,
                                                  op=ALU.add), reads=[o2], writes=[ss])
            k.op("dve", lambda e: e.tensor_scalar(out=ss[:, 0:16], in0=ss[:, 0:16], scalar1=1e-12, scalar2=None, op0=ALU.add),
                 reads=[ss], writes=[ss])
            k.op("act", lambda e: e.activation(out=ss[:, 16:32], in_=ss[:, 0:16], func=AF.Sqrt), reads=[ss], writes=[ss])
            k.op("dve", lambda e: e.reciprocal(out=ss[:, 32:48], in_=ss[:, 16:32]), reads=[ss], writes=[ss])
            for h in range(16):
                k.op("dve", lambda e, h=h: e.tensor_scalar(out=kk[:, h * 64:(h + 1) * 64], in0=kk[:, h * 64:(h + 1) * 64],
                                                           scalar1=ss[:, 32 + h:33 + h], scalar2=None, op0=ALU.mult),
                     reads=[kk, ss], writes=[kk])
            k.op("dve", lambda e: e.tensor_scalar(out=o2[:, :RW], in0=kk[:, :RW], scalar1=-1.0, scalar2=None, op0=ALU.mult),
                 reads=[kk], writes=[o2])
            k.dma("pool", scin.t.ap()[ti * 128:(ti + 1) * 128, 1 * RW:2 * RW], o2[:, :RW], src=o2, final=True)
            k.dma("sp", lt[:], lora.t.ap()[ti * 128:(ti + 1) * 128, :], dst=lt)
            k.op("act", lambda e: e.activation(out=lt[:, 0:256], in_=lt[:, 0:256], func=AF.Tanh), reads=[lt], writes=[lt])
            transpose_tile(k, lt, ident, pst, lambda c: lT[:, c * 128:(c + 1) * 128], lT, nch=4)
            for z in range(2):
                for half in range(2):
                    p = psA[half]
                    cs = slice(z * 1024 + half * 512, z * 1024 + half * 512 + 512)
                    k.op("pe", lambda e, p=p, cs=cs: e.matmul(p[:], lhsT=lT[:, z * 128:(z + 1) * 128], rhs=w2t[:, cs], start=True,
                                                              stop=False), reads=[lT, w2t], writes=[p])
                    k.op("pe", lambda e, p=p, cs=cs: e.matmul(p[:], lhsT=ones[:], rhs=w0t[:, cs], start=False, stop=True),
                         reads=[ones, w0t], writes=[p])
                    k.op("act", lambda e, p=p, half=half: e.activation(out=o1[:, half * 512:(half + 1) * 512], in_=p[:],
                                                                       func=AF.Sigmoid), reads=[p], writes=[o1])
                k.op("act", lambda e: e.activation(out=o1[:, :RW], in_=o1[:, :RW], func=AF.Exp, scale=-float(np.exp(-0.5))),
                     reads=[o1], writes=[o1])
                k.dma("pool", scin.t.ap()[ti * 128:(ti + 1) * 128, (3 + 3 * z) * RW:(4 + 3 * z) * RW], o1[:, :RW], src=o1, final=True)
                for half in range(2):
                    p = psA[half]
                    cs = slice(2048 + z * 1024 + half * 512, 2048 + z * 1024 + half * 512 + 512)
                    k.op("pe", lambda e, p=p, cs=cs: e.matmul(p[:], lhsT=lT[:, (2 + z) * 128:(3 + z) * 128], rhs=w2t[:, cs], start=True,
                                                              stop=False), reads=[lT, w2t], writes=[p])
                    k.op("pe", lambda e, p=p, cs=cs: e.matmul(p[:], lhsT=ones[:], rhs=w0t[:, cs], start=False, stop=True),
                         reads=[ones, w0t], writes=[p])
                    k.op("act", lambda e, p=p, half=half: e.activation(out=xs[:, half * 512:(half + 1) * 512], in_=p[:],
                                                                       func=AF.Sigmoid), reads=[p], writes=[xs])
                k.op("dve", lambda e: e.tensor_tensor(out=cw[:, :RW], in0=kk[:, :RW], in1=xs[:, :RW], op=ALU.mult),
                     reads=[kk, xs], writes=[cw])
                k.dma("pool", scin.t.ap()[ti * 128:(ti + 1) * 128, (4 + 3 * z) * RW:(5 + 3 * z) * RW], cw[:, :RW], src=cw, final=True)
                k.op("dve", lambda e: e.scalar_tensor_tensor(out=xs[:, RW:2 * RW], in0=xs[:, :RW], scalar=-1.0, in1=kapt[:],
                                                             op0=ALU.add, op1=ALU.mult), reads=[xs, kapt], writes=[xs])
                k.op("dve", lambda e: e.scalar_tensor_tensor(out=xs[:, 2 * RW:3 * RW], in0=xs[:, RW:2 * RW], scalar=1.0, in1=kf[:, :RW],
                                                             op0=ALU.add, op1=ALU.mult), reads=[xs, kf], writes=[xs])
                k.dma("pool", scin.t.ap()[ti * 128:(ti + 1) * 128, (5 + 3 * z) * RW:(6 + 3 * z) * RW], xs[:, 2 * RW:3 * RW], src=xs,
                      final=True)
    k.finish()
    return k.nc


def rope_tables():
    half = 32
    freqs = 10000.0 ** (-np.arange(half, dtype=np.float32) / half)
    t = np.arange(T)
    row = (t // 64).astype(np.float32)[:, None] * freqs[None, :]
    col = (t % 64).astype(np.float32)[:, None] * freqs[None, :]
    cr, sr, cc, sc = np.cos(row), np.sin(row), np.cos(col), np.sin(col)
    C = np.concatenate([cr, cr, cc, cc], 1).astype(np.float32)
    S = np.concatenate([-sr, sr, -sc, sc], 1).astype(np.float32)
    return C, S


def l2_inmaps(px, ph, P):
    C, S = rope_tables()
    ident = np.eye(128, dtype=np.float32)
    jj = np.arange(128)[:, None]
    ii = np.arange(128)[None, :]
    mprev = np.tile((jj >= ii).astype(np.float32), (1, 4))
    mnext = np.tile((jj <= ii).astype(np.float32), (1, 4))
    zero = np.zeros_like(mprev)
    wsT = np.ascontiguousarray(P["cmlp_ws"].transpose(2, 0, 1).reshape(128, 1024))
    bsT = np.ascontiguousarray(P["cmlp_b"].T)
    w2a2 = np.ascontiguousarray(np.concatenate([P["rwkv_w2"][0], P["rwkv_w2"][1], P["rwkv_a2"][0], P["rwkv_a2"][1]], 1))
    w0a0 = np.ascontiguousarray(np.concatenate([P["rwkv_w0"][0], P["rwkv_w0"][1], P["rwkv_a0"][0], P["rwkv_a0"][1]])[None])
    in_maps = []
    for i in range(NCORES):
        b, t0, cj = tok_rows(i)
        cat = lambda a, c: np.ascontiguousarray(np.concatenate([a, c], 0))
        ctile = ph[b, cj * 128:(cj + 1) * 128]
        lat = px[b, t0:t0 + 1024]
        def halo(cols, n, src, s0, s1):
            o = np.zeros((s1 - s0 + 2 * n, cols.stop - cols.start), np.float32)
            lo, hi = max(s0 - n, 0), min(s1 + n, src.shape[0])
            o[lo - (s0 - n):hi - (s0 - n)] = src[lo:hi, cols]
            return o
        Ck = np.zeros((1280, 128), np.float32); Sk = np.zeros((1280, 128), np.float32)
        lo, hi = max(t0 - 128, 0), min(t0 + 1152, T)
        Ck[lo - (t0 - 128):hi - (t0 - 128)] = C[lo:hi]; Sk[lo - (t0 - 128):hi - (t0 - 128)] = S[lo:hi]
        m = np.concatenate([zero if t0 == 0 else mprev, mprev, mnext, zero if t0 + 1024 == T else mnext], 0)
        in_maps.append({
            "qx": cat(lat[:, 0:2048], ctile[:, 0:2048]),
            "kh": halo(slice(2048, 2560), 128, px[b], t0, t0 + 1024), "vh": halo(slice(2560, 3072), 128, px[b], t0, t0 + 1024),
            "kc": np.ascontiguousarray(ph[b][:, 2048:2560]), "vc": np.ascontiguousarray(ph[b][:, 2560:3072]),
            "rqc": np.ascontiguousarray(np.tile(C[t0:t0 + 1024], (1, 16))), "rqs": np.ascontiguousarray(np.tile(S[t0:t0 + 1024], (1, 16))),
            "rkc": np.ascontiguousarray(np.tile(Ck, (1, 4))), "rks": np.ascontiguousarray(np.tile(Sk, (1, 4))),
            "masks": np.ascontiguousarray(m), "sink": np.ascontiguousarray(P["attn_sink"]),
            "ug": cat(lat[:, 3072:5120], ctile[:, 3072:5120]), "cng": np.ascontiguousarray(P["cmlp_norm_g"]),
            "wsT": wsT, "bsT": bsT,
            "rkvp": cat(halo(slice(5120, 8192), 1, px[b], t0, t0 + 1024), halo(slice(5120, 8192), 1, ph[b], cj * 128, cj * 128 + 128)),
            "lora": cat(lat[:, 9216:9728], ctile[:, 9216:9728]), "convw": np.ascontiguousarray(P["rwkv_conv"]),
            "w2a2": w2a2, "w0a0": w0a0, "kkp": np.ascontiguousarray(P["rwkv_kk"]), "kap": np.ascontiguousarray(P["rwkv_ka"]),
            "identd": ident,
        })
    return in_maps


def l2_gather(res, cores=None):
    cores = list(range(NCORES)) if cores is None else cores
    attn_x = np.zeros((2, T, 2048), np.float32); attn_c = np.zeros((2, LCTX, 2048), np.float32)
    cm_x = np.zeros((2, T, CW), np.float32); cm_c = np.zeros((2, LCTX, CW), np.float32)
    sc_x = np.zeros((2, T, 10 * RW), np.float32); sc_c = np.zeros((2, LCTX, 10 * RW), np.float32)
    for r, i in zip(res.results, cores):
        b, t0, cj = tok_rows(i)
        for key, X, Cc in (("attn", attn_x, attn_c), ("cmlp", cm_x, cm_c), ("scin", sc_x, sc_c)):
            X[b, t0:t0 + 1024] = r[key][:1024]
            if (i % 4) < 2:
                Cc[b, cj * 128:(cj + 1) * 128] = r[key][1024:]
    return attn_x, attn_c, cm_x, cm_c, sc_x, sc_c


NS = LCTX + T
TC = 8


def build_l3(ns=NS, tc=TC, stage=9):
    k = KB()
    nchk = ns // tc
    RR = k.dram("RR", [nchk * 2, 4 * tc * 256], "ExternalInput")
    V2 = k.dram("V2", [nchk * 2, 4 * tc * 64], "ExternalInput")
    LL = k.dram("LL", [nchk * 128, 4 * tc * 4], "ExternalInput")
    WC = k.dram("WC", [nchk * 128, 4 * tc], "ExternalInput")
    Y = k.dram("Y", [nchk * 4 * 4, tc * 64], "ExternalOutput")
    rr = [k.sb([2, 4 * tc * 256], "rr%d" % i) for i in range(2)]
    v2 = [k.sb([2, 4 * tc * 64], "v2%d" % i) for i in range(2)]
    ll = [k.sb([128, 4 * tc * 4], "ll%d" % i) for i in range(2)]
    wc = [k.sb([128, 4 * tc], "wc%d" % i) for i in range(2)]
    y4 = [[k.sb([4, tc * 64], "y4_%d_%d" % (i, p)) for p in range(4)] for i in range(2)]
    ST = [k.sb([128, 64], "ST%d" % p) for p in range(4)]
    U = [[k.ps([128, 64], "U%d" % p)] * 2 for p in range(4)]
    P1 = [[k.ps([4, 64], "P%d" % p)] * 2 for p in range(4)]
    for p in range(4):
        k.op("dve", lambda e, p=p: e.memset(ST[p][:], 0.0), writes=[ST[p]])

    def load(c):
        s = c % 2
        k.dma("sp", rr[s][:], RR.t.ap()[c * 2:(c + 1) * 2, :], dst=rr[s])
        k.dma("sp", v2[s][:], V2.t.ap()[c * 2:(c + 1) * 2, :], dst=v2[s])
        k.dma("sp", ll[s][:], LL.t.ap()[c * 128:(c + 1) * 128, :], dst=ll[s])
        k.dma("sp", wc[s][:], WC.t.ap()[c * 128:(c + 1) * 128, :], dst=wc[s])

    load(0)
    for c in range(nchk):
        if c + 1 < nchk:
            load(c + 1)
        s = c % 2
        for tt in range(tc):
            t = c * tc + tt
            for p in range(4):
                u = U[p][t % 2]
                o = (p * tc + tt) * 256
                ov = (p * tc + tt) * 64
                if stage < 1:
                    continue
                k.op("pe", lambda e, u=u, o=o, ov=ov: e.matmul(u[:], lhsT=rr[s][0:2, o:o + 128], rhs=v2[s][0:2, ov:ov + 64],
                                                               start=True, stop=(t == 0)), reads=[rr[s], v2[s]], writes=[u])
                if t > 0 and stage >= 2:
                    yp, tp = (y4[s][p], tt - 1) if tt > 0 else (y4[1 - s][p], tc - 1)
                    k.op("pe", lambda e, u=u, o=o, yp=yp, tp=tp: e.matmul(u[:], lhsT=rr[s][0:2, o + 128:o + 256],
                                                                          rhs=yp[0:2, tp * 64:(tp + 1) * 64], start=False, stop=True),
                         reads=[rr[s], yp], writes=[u])
            for p in range(4):
                u = U[p][t % 2]
                if stage < 3:
                    continue
                k.op("dve", lambda e, p=p, u=u: e.scalar_tensor_tensor(out=ST[p][:], in0=ST[p][:],
                                                                       scalar=wc[s][:, p * tc + tt:p * tc + tt + 1], in1=u[:],
                                                                       op0=ALU.mult, op1=ALU.add), reads=[ST[p], wc[s], u], writes=[ST[p]])
            for p in range(4):
                pp = P1[p][t % 2]
                ol = (p * tc + tt) * 4
                if stage < 4:
                    if tt == 0:
                        k.op("dve", lambda e, p=p: e.memset(y4[s][p][:], 1.0), writes=[y4[s][p]])
                    continue
                k.op("pe", lambda e, p=p, pp=pp, ol=ol: e.matmul(pp[:], lhsT=ll[s][:, ol:ol + 4], rhs=ST[p][:], start=True, stop=True),
                     reads=[ll[s], ST[p]], writes=[pp])
                k.op("act", lambda e, p=p, pp=pp: e.copy(out=y4[s][p][:, tt * 64:(tt + 1) * 64], in_=pp[:]), reads=[pp], writes=[y4[s][p]])
        for p in range(4):
            k.dma("pool", Y.t.ap()[(c * 4 + p) * 4:(c * 4 + p + 1) * 4, :], y4[s][p][:], src=y4[s][p], final=True)
    k.finish()
    return k.nc


def scan_core(i):
    return i // 4, (i // 2) % 2, (i % 2) * 512


def l3_inmaps(sc_x, sc_c, ns=NS, tc=TC):
    nchk = ns // tc
    in_maps = []
    for i in range(NCORES):
        z, b, c0 = scan_core(i)
        seq = np.concatenate([sc_c[b][::-1] if z else sc_c[b], sc_x[b][::-1] if z else sc_x[b]], 0)[:ns]
        q = lambda qi: seq[:, qi * RW + c0:qi * RW + c0 + 512].reshape(ns, 4, 2, 64)
        r, nkk, v, w, bb, kr = q(0), q(1), q(2), q(3 + 3 * z), q(4 + 3 * z), q(5 + 3 * z)
        RRa = np.zeros((nchk, 2, 4, tc, 256), np.float32)
        V2a = np.zeros((nchk, 2, 4, tc, 64), np.float32)
        LLa = np.zeros((nchk, 2, 64, 4, tc, 4), np.float32)
        WCa = np.zeros((nchk, 2, 64, 4, tc), np.float32)
        c5 = lambda a: a.reshape(nchk, tc, 4, 2, 64)
        nkk_next = np.concatenate([nkk[1:], np.zeros_like(nkk[:1])], 0)
        for h in range(2):
            RRa[:, h, :, :, h * 64:(h + 1) * 64] = c5(kr)[:, :, :, h, :].transpose(0, 2, 1, 3)
            RRa[:, h, :, :, 128 + h * 64:128 + (h + 1) * 64] = c5(bb)[:, :, :, h, :].transpose(0, 2, 1, 3)
            V2a[:, h] = c5(v)[:, :, :, h, :].transpose(0, 2, 1, 3)
            LLa[:, h, :, :, :, h] = c5(nkk_next)[:, :, :, h, :].transpose(0, 3, 2, 1)
            LLa[:, h, :, :, :, 2 + h] = c5(r)[:, :, :, h, :].transpose(0, 3, 2, 1)
            WCa[:, h] = c5(w)[:, :, :, h, :].transpose(0, 3, 2, 1)
        in_maps.append({"RR": RRa.reshape(nchk * 2, -1), "V2": V2a.reshape(nchk * 2, -1),
                        "LL": LLa.reshape(nchk * 128, -1), "WC": WCa.reshape(nchk * 128, -1)})
    return in_maps


def l3_gather(res, ns=NS, tc=TC, cores=None):
    cores = list(range(NCORES)) if cores is None else cores
    nchk = ns // tc
    y = np.zeros((2, 2, ns, RW), np.float32)
    for rs, i in zip(res.results, cores):
        z, b, c0 = scan_core(i)
        Ya = rs["Y"].reshape(nchk, 4, 4, tc, 64)
        yy = Ya[:, :, 2:4].transpose(0, 3, 1, 2, 4).reshape(ns, 512)
        if z:
            yy = np.concatenate([yy[:LCTX][::-1], yy[LCTX:][::-1]], 0) if ns == NS else yy[::-1]
        y[z, b, :, c0:c0 + 512] = yy
    return y


GROUPS2 = [(0, 1), (2, 3), (4, 5), (6, 7), (8,)]


def build_l4a():
    k = KB()
    di = lambda n, s: k.dram(n, s, "ExternalInput")
    y01 = di("y01", [NTOK, 2 * RW]); rkv = di("rkv", [NTOK, 3 * RW]); gt = di("gt", [NTOK, RW])
    attn = di("attn", [NTOK, 2048]); cmlp = di("cmlp", [NTOK, CW]); xin = di("xin", [NTOK, D])
    prm = di("prm", [3, RW]); g1row = di("g1row", [2, D]); Wo = di("Wo", [D, D]); identd = di("identd", [128, 128])
    x1 = k.dram("x1", [NTOK, D], "ExternalOutput")
    ident = k.sb([128, 128], "ident")
    k.dma("sp", ident[:], identd.t.ap(), dst=ident)
    pt = k.sb([128, 3 * RW], "pt")
    for j in range(3):
        k.dma("sp", pt[:, j * RW:(j + 1) * RW], prm.t.ap()[j, :].partition_broadcast(128), dst=pt)
    mix = k.sb([128, D], "mix")
    mixT = k.sb([128, 2 * NCH * 128], "mixT", mdt())
    BW = 256
    wb = [k.sb([128, NCH, BW], "wb%d" % i, mdt()) for i in range(2)]
    LQ, SQ = ("pool", "sp") if MM_R[0] else ("sp", "pool")
    yt = k.sb([128, 2 * RW], "yt"); rt = k.sb([128, 3 * RW], "rt"); gg = k.sb([128, RW], "gg")
    s1 = k.sb([128, RW], "s1"); s2 = k.sb([128, RW], "s2")
    st = k.sb([128, 128], "st")
    gb = [k.sb([128, BW], "gb%d" % i) for i in range(2)]
    xb = [k.sb([128, BW], "xb%d" % i) for i in range(3)]
    pst = [k.ps([128, 512], "pst%d" % i) for i in range(2)]
    pso = [k.ps([128, BW], "pso%d" % i) for i in range(3)]
    Wv = Wo.t.ap().rearrange("(c p) n -> p c n", p=128)
    it = 0
    for grp in GROUPS2:
        ng = len(grp)
        for j, ti in enumerate(grp):
            rows = slice(ti * 128, (ti + 1) * 128)
            k.dma("sp", mix[:, 0:2048], attn.t.ap()[rows, :], dst=mix)
            k.dma("sp", mix[:, 2048:3072], cmlp.t.ap()[rows, :], dst=mix)
            k.dma("sp", yt[:], y01.t.ap()[rows, :], dst=yt)
            k.dma("sp", rt[:], rkv.t.ap()[rows, :], dst=rt)
            k.dma("sp", gg[:], gt.t.ap()[rows, :], dst=gg)
            k.op("dve", lambda e: e.tensor_tensor(out=s1[:], in0=yt[:, 0:RW], in1=yt[:, RW:2 * RW], op=ALU.add), reads=[yt], writes=[s1])
            k.op("dve", lambda e: e.tensor_reduce(out=st[:, 0:16], in_=s1[:].rearrange("p (h n) -> p h n", n=64), axis=AX.X, op=ALU.add),
                 reads=[s1], writes=[st])
            k.op("dve", lambda e: e.tensor_tensor(out=s2[:], in0=s1[:], in1=s1[:], op=ALU.mult), reads=[s1], writes=[s2])
            k.op("dve", lambda e: e.tensor_reduce(out=st[:, 16:32], in_=s2[:].rearrange("p (h n) -> p h n", n=64), axis=AX.X, op=ALU.add),
                 reads=[s2], writes=[st])
            k.op("dve", lambda e: e.tensor_scalar(out=st[:, 0:32], in0=st[:, 0:32], scalar1=1.0 / 64, scalar2=None, op0=ALU.mult),
                 reads=[st], writes=[st])
            k.op("dve", lambda e: e.tensor_tensor(out=st[:, 32:48], in0=st[:, 0:16], in1=st[:, 0:16], op=ALU.mult), reads=[st], writes=[st])
            k.op("dve", lambda e: e.tensor_tensor(out=st[:, 48:64], in0=st[:, 16:32], in1=st[:, 32:48], op=ALU.subtract), reads=[st], writes=[st])
            k.op("dve", lambda e: e.tensor_scalar(out=st[:, 48:64], in0=st[:, 48:64], scalar1=64e-5, scalar2=None, op0=ALU.add),
                 reads=[st], writes=[st])
            k.op("act", lambda e: e.activation(out=st[:, 64:80], in_=st[:, 48:64], func=AF.Sqrt), reads=[st], writes=[st])
            k.op("dve", lambda e: e.reciprocal(out=st[:, 80:96], in_=st[:, 64:80]), reads=[st], writes=[st])
            for h in range(16):
                k.op("dve", lambda e, h=h: e.tensor_scalar(out=s1[:, h * 64:(h + 1) * 64], in0=s1[:, h * 64:(h + 1) * 64],
                                                           scalar1=st[:, h:h + 1], scalar2=st[:, 80 + h:81 + h], op0=ALU.subtract,
                                                           op1=ALU.mult), reads=[s1, st], writes=[s1])
            k.op("dve", lambda e: e.tensor_tensor(out=s1[:], in0=s1[:], in1=pt[:, RW:2 * RW], op=ALU.mult), reads=[s1, pt], writes=[s1])
            k.op("dve", lambda e: e.tensor_tensor(out=s1[:], in0=s1[:], in1=pt[:, 2 * RW:3 * RW], op=ALU.add), reads=[s1, pt], writes=[s1])
            k.op("dve", lambda e: e.tensor_tensor(out=s2[:], in0=rt[:, 0:RW], in1=rt[:, RW:2 * RW], op=ALU.mult), reads=[rt], writes=[s2])
            k.op("dve", lambda e: e.tensor_tensor(out=s2[:], in0=s2[:], in1=pt[:, 0:RW], op=ALU.mult), reads=[s2, pt], writes=[s2])
            k.op("dve", lambda e: e.tensor_reduce(out=st[:, 96:112], in_=s2[:].rearrange("p (h n) -> p h n", n=64), axis=AX.X, op=ALU.add),
                 reads=[s2], writes=[st])
            for h in range(16):
                k.op("dve", lambda e, h=h: e.scalar_tensor_tensor(out=s1[:, h * 64:(h + 1) * 64], in0=rt[:, 2 * RW + h * 64:2 * RW + (h + 1) * 64],
                                                                  scalar=st[:, 96 + h:97 + h], in1=s1[:, h * 64:(h + 1) * 64],
                                                                  op0=ALU.mult, op1=ALU.add), reads=[rt, st, s1], writes=[s1])
            k.op("act", lambda e: e.activation(out=gg[:], in_=gg[:], func=AF.Sigmoid), reads=[gg], writes=[gg])
            k.op("dve", lambda e: e.tensor_tensor(out=mix[:, 3072:4096], in0=s1[:], in1=gg[:], op=ALU.mult), reads=[s1, gg], writes=[mix])
            transpose_tile(k, mix, ident, pst, lambda c, j=j: mixT[:, (c * 2 + j) * 128:(c * 2 + j + 1) * 128], mixT)
        for blk in range(D // BW):
            w = wb[blk % 2]
            cs = slice(blk * BW, (blk + 1) * BW)
            for q4 in range(4):
                k.dma(LQ, w[:, q4 * 8:(q4 + 1) * 8, :], Wv[:, q4 * 8:(q4 + 1) * 8, cs], dst=w)
            sets = sorted(set(0 if ti < 8 else 1 for ti in grp))
            for s_ in sets:
                k.dma("sp", gb[s_][:], g1row.t.ap()[s_, cs].partition_broadcast(128), dst=gb[s_])
            for j, ti in enumerate(grp):
                p = pso[it % 3]
                xx = xb[it % 3]
                it += 1
                k.dma("sp", xx[:], xin.t.ap()[ti * 128:(ti + 1) * 128, cs], dst=xx)
                for c in range(NCH):
                    k.op("pe", lambda e, c=c, j=j, p=p: mmr(e, p[:], mixT[:, (c * 2 + j) * 128:(c * 2 + j + 1) * 128], w[:, c, :],
                                                            (c == 0), (c == NCH - 1)), reads=[mixT, w], writes=[p])
                g_ = gb[0 if ti < 8 else 1]
                k.op("dve", lambda e, p=p, g_=g_: e.tensor_tensor(out=p[:], in0=p[:], in1=g_[:], op=ALU.mult), reads=[p, g_], writes=[p])
                k.op("dve", lambda e, p=p, xx=xx: e.tensor_tensor(out=xx[:], in0=xx[:], in1=p[:], op=ALU.add), reads=[p, xx], writes=[xx])
                k.dma(SQ, x1.t.ap()[ti * 128:(ti + 1) * 128, cs], xx[:], src=xx, final=True)
    k.finish()
    return k.nc


def l4a_inmaps(y, sc_x, sc_c, px, ph, attn_x, attn_c, cm_x, cm_c, x, h, mod_l, P):
    ident = np.eye(128, dtype=np.float32)
    prm = np.ascontiguousarray(np.stack([P["rwkv_rk"], P["rwkv_ln_w"], P["rwkv_ln_b"]]))
    in_maps = []
    for i in range(NCORES):
        b, t0, cj = tok_rows(i)
        cat = lambda a, c: np.ascontiguousarray(np.concatenate([a, c], 0))
        lat = slice(t0, t0 + 1024); ct = slice(cj * 128, (cj + 1) * 128)
        yl = np.concatenate([y[0, b, LCTX:][lat], y[1, b, LCTX:][lat]], 1)
        yc = np.concatenate([y[0, b, :LCTX][ct], y[1, b, :LCTX][ct]], 1)
        sel = lambda a: np.concatenate([a[:, 0:RW], a[:, 9 * RW:10 * RW], a[:, 2 * RW:3 * RW]], 1)
        in_maps.append({
            "y01": cat(yl, yc), "rkv": cat(sel(sc_x[b][lat]), sel(sc_c[b][ct])),
            "gt": cat(px[b][lat, 8192:9216], ph[b][ct, 8192:9216]),
            "attn": cat(attn_x[b][lat], attn_c[b][ct]), "cmlp": cat(cm_x[b][lat], cm_c[b][ct]),
            "xin": cat(x[b][lat], h[b][ct]), "prm": prm,
            "g1row": np.ascontiguousarray(np.stack([mod_l[b, 2 * D:3 * D], mod_l[2, 2 * D:3 * D]])),
            "Wo": P["w_out"], "identd": ident})
    return in_maps


def tok_gather(res, key, width, cores=None):
    cores = list(range(NCORES)) if cores is None else cores
    X = np.zeros((2, T, width), np.float32); Hc = np.zeros((2, LCTX, width), np.float32)
    for r, i in zip(res.results, cores):
        b, t0, cj = tok_rows(i)
        X[b, t0:t0 + 1024] = r[key][:1024]
        if (i % 4) < 2:
            Hc[b, cj * 128:(cj + 1) * 128] = r[key][1024:]
    return X, Hc


NE = 16
FF = 1024


def build_l4b(final=False, n_exp=NE):
    k = KB()
    di = lambda n, s: k.dram(n, s, "ExternalInput")
    x1 = di("x1", [NTOK, D]); modcol = di("modcol", [128, 5 * NCH]); g2row = di("g2row", [2, D])
    rwc = di("rwc", [128, NCH * NE]); rb = di("rb", [NE]); fg = di("fg", [D]); identd = di("identd", [128, 128])
    W1 = di("W1", [NE * D, FF]); W3 = di("W3", [NE * D, FF]); W2 = di("W2", [NE * FF, D])
    x2 = k.dram("x2", [NTOK, D], "ExternalOutput")
    xf = k.dram("xf", [NTOK, D], "ExternalOutput") if final else None
    ident = k.sb([128, 128], "ident")
    k.dma("sp", ident[:], identd.t.ap(), dst=ident)
    mc = k.sb([128, 5 * NCH], "mc"); gs = k.sb([128, 4 * NCH], "gs")
    k.dma("sp", mc[:], modcol.t.ap(), dst=mc)
    g = mc[:, 0:NCH]
    for s in range(2):
        sc = mc[:, (1 + 2 * s) * NCH:(2 + 2 * s) * NCH]
        sh = mc[:, (2 + 2 * s) * NCH:(3 + 2 * s) * NCH]
        k.op("dve", lambda e, s=s, sc=sc: e.tensor_tensor(out=gs[:, 2 * s * NCH:(2 * s + 1) * NCH], in0=g, in1=sc, op=ALU.mult),
             reads=[mc], writes=[gs])
        k.op("dve", lambda e, s=s: e.tensor_tensor(out=gs[:, 2 * s * NCH:(2 * s + 1) * NCH], in0=gs[:, 2 * s * NCH:(2 * s + 1) * NCH],
                                                   in1=g, op=ALU.add), reads=[mc, gs], writes=[gs])
        k.op("dve", lambda e, s=s, sh=sh: e.tensor_copy(out=gs[:, (2 * s + 1) * NCH:(2 * s + 2) * NCH], in_=sh), reads=[mc], writes=[gs])
    LQ, SQ = ("pool", "sp") if MM_R[0] else ("sp", "pool")
    rw = k.sb([128, NCH * NE], "rw", mdt()); rbt = k.sb([128, NE], "rbt")
    k.dma(LQ, rw[:], rwc.t.ap(), dst=rw)
    k.dma("sp", rbt[:], rb.t.ap().partition_broadcast(128), dst=rbt)
    xt = k.sb([128, D], "xt"); sq = k.sb([128, D], "sq"); st = k.sb([128, 4], "st")
    znT = k.sb([128, NCH * 256], "znT", mdt())
    acc = [k.sb([128, D], "acc%d" % j) for j in range(2)]
    wh = [k.sb([128, 16, 128], "wh%d" % i, mdt()) for i in range(4)]
    w2s = [k.sb([128, 8, 256], "w2s%d" % i, mdt()) for i in range(2)]
    hidT = k.sb([128, 8 * 256], "hidT", mdt()); sl = k.sb([128, 256], "sl")
    R = [k.sb([128, 160], "R%d" % j) for j in range(2)]
    pst = [k.ps([128, 512], "pst%d" % i) for i in range(2)]
    pH = [k.ps([128, 256], "pH%d" % i) for i in range(2)]
    po = [k.ps([128, 256], "po%d" % i) for i in range(2)]
    pr = k.ps([128, NE], "pr")

    def route(r, j):
        o = lambda fn, rd=(), wr=(): k.op("dve", fn, reads=[r] + list(rd), writes=[r] + list(wr))
        k.op("act", lambda e: e.activation(out=r[:, 0:16], in_=pr[:], func=AF.Sigmoid), reads=[pr], writes=[r])
        o(lambda e: e.tensor_tensor(out=r[:, 16:32], in0=r[:, 0:16], in1=rbt[:], op=ALU.add), rd=[rbt])
        sel3 = r[:, 16:32].rearrange("p (g e) -> p g e", e=4)
        ps6 = r[:, 32:56].rearrange("p (g s) -> p g s", s=6)
        idx = 0
        for a in range(4):
            for b in range(a + 1, 4):
                o(lambda e, a=a, b=b, idx=idx: e.tensor_tensor(out=ps6[:, :, idx], in0=sel3[:, :, a], in1=sel3[:, :, b], op=ALU.add))
                idx += 1
        o(lambda e: e.tensor_reduce(out=r[:, 56:60], in_=ps6, axis=AX.X, op=ALU.max))
        o(lambda e: e.tensor_reduce(out=r[:, 60:61], in_=r[:, 56:60], axis=AX.X, op=ALU.max))
        o(lambda e: e.tensor_scalar(out=r[:, 61:65], in0=r[:, 56:60], scalar1=r[:, 60:61], scalar2=None, op0=ALU.is_equal))
        o(lambda e: e.tensor_reduce(out=r[:, 65:69], in_=sel3, axis=AX.X, op=ALU.max))
        for gi in range(4):
            o(lambda e, gi=gi: e.tensor_scalar(out=r[:, 69 + gi * 4:73 + gi * 4], in0=r[:, 16 + gi * 4:20 + gi * 4],
                                               scalar1=r[:, 65 + gi:66 + gi], scalar2=None, op0=ALU.is_equal))
        o(lambda e: e.scalar_tensor_tensor(out=r[:, 85:101], in0=r[:, 69:85], scalar=-1e30, in1=r[:, 16:32], op0=ALU.mult, op1=ALU.add))
        o(lambda e: e.tensor_reduce(out=r[:, 101:105], in_=r[:, 85:101].rearrange("p (g e) -> p g e", e=4), axis=AX.X, op=ALU.max))
        for gi in range(4):
            o(lambda e, gi=gi: e.tensor_scalar(out=r[:, 105 + gi * 4:109 + gi * 4], in0=r[:, 85 + gi * 4:89 + gi * 4],
                                               scalar1=r[:, 101 + gi:102 + gi], scalar2=None, op0=ALU.is_equal))
        o(lambda e: e.tensor_tensor(out=r[:, 105:121], in0=r[:, 105:121], in1=r[:, 69:85], op=ALU.add))
        for gi in range(4):
            o(lambda e, gi=gi: e.tensor_scalar(out=r[:, 105 + gi * 4:109 + gi * 4], in0=r[:, 105 + gi * 4:109 + gi * 4],
                                               scalar1=r[:, 61 + gi:62 + gi], scalar2=None, op0=ALU.mult))
        o(lambda e: e.tensor_tensor(out=r[:, 121:137], in0=r[:, 105:121], in1=r[:, 0:16], op=ALU.mult))
        o(lambda e: e.tensor_reduce(out=r[:, 137:138], in_=r[:, 121:137], axis=AX.X, op=ALU.add))
        o(lambda e: e.reciprocal(out=r[:, 138:139], in_=r[:, 137:138]))
        o(lambda e: e.tensor_scalar(out=r[:, 139:155], in0=r[:, 121:137], scalar1=r[:, 138:139], scalar2=None, op0=ALU.mult))

    for grp in GROUPS2:
        ng = len(grp)
        ntok = ng * 128
        for j, ti in enumerate(grp):
            k.dma("sp", xt[:], x1.t.ap()[ti * 128:(ti + 1) * 128, :], dst=xt)
            rms_scale(k, xt, sq, st, D)
            s = 0 if ti < 8 else 1
            transpose_tile(k, xt, ident, pst, lambda c, j=j: znT[:, (c * 2 + j) * 128:(c * 2 + j + 1) * 128], znT,
                           gcol=gs[:, 2 * s * NCH:(2 * s + 1) * NCH], scol=gs[:, (2 * s + 1) * NCH:(2 * s + 2) * NCH], mbuf=gs)
            for c in range(NCH):
                k.op("pe", lambda e, c=c, j=j: e.matmul(pr[:], lhsT=znT[:, (c * 2 + j) * 128:(c * 2 + j + 1) * 128],
                                                        rhs=rw[:, c * NE:(c + 1) * NE], start=(c == 0), stop=(c == NCH - 1)),
                     reads=[znT, rw], writes=[pr])
            route(R[j], j)
            k.op("dve", lambda e, j=j: e.memset(acc[j][:], 0.0), writes=[acc[j]])
        for ex in range(n_exp):
            for fc in range(8):
                for wi, Wd in ((0, W1), (2, W3)):
                    for hf in range(2):
                        wbuf = wh[wi + hf]
                        src = Wd.t.ap()[ex * D + hf * 2048:ex * D + (hf + 1) * 2048, :].rearrange("(c p) n -> p c n", p=128)
                        for q2 in range(2):
                            k.dma(LQ, wbuf[:, q2 * 8:(q2 + 1) * 8, :], src[:, q2 * 8:(q2 + 1) * 8, fc * 128:(fc + 1) * 128], dst=wbuf)
                    p = pH[wi // 2]
                    for c in range(NCH):
                        k.op("pe", lambda e, c=c, p=p, wi=wi: mmr(e, p[:, 0:ntok], wh[wi + c // 16][:, c % 16, :],
                                                                  znT[:, c * 256:c * 256 + ntok], (c == 0), (c == NCH - 1)),
                             reads=[wh[wi + c // 16], znT], writes=[p])
                k.op("act", lambda e: e.activation(out=sl[:, 0:ntok], in_=pH[0][:, 0:ntok], func=AF.Silu), reads=[pH[0]], writes=[sl])
                k.op("dve", lambda e, fc=fc: e.tensor_tensor(out=hidT[:, fc * 256:fc * 256 + ntok], in0=sl[:, 0:ntok], in1=pH[1][:, 0:ntok],
                                                             op=ALU.mult), reads=[sl, pH[1]], writes=[hidT])
            for dblk in range(D // 256):
                w2 = w2s[dblk % 2]
                src = W2.t.ap()[ex * FF:(ex + 1) * FF, :].rearrange("(f p) n -> p f n", p=128)
                k.dma(LQ, w2[:], src[:, :, dblk * 256:(dblk + 1) * 256], dst=w2)
                for j in range(ng):
                    p = po[j]
                    for fc in range(8):
                        k.op("pe", lambda e, fc=fc, p=p, j=j: mmr(e, p[:], hidT[:, fc * 256 + j * 128:fc * 256 + (j + 1) * 128],
                                                                  w2[:, fc, :], (fc == 0), (fc == 7)),
                             reads=[hidT, w2], writes=[p])
                    k.op("dve", lambda e, p=p, j=j, dblk=dblk: e.scalar_tensor_tensor(
                        out=acc[j][:, dblk * 256:(dblk + 1) * 256], in0=p[:], scalar=R[j][:, 139 + ex:140 + ex],
                        in1=acc[j][:, dblk * 256:(dblk + 1) * 256], op0=ALU.mult, op1=ALU.add), reads=[p, R[j], acc[j]], writes=[acc[j]])
        for j, ti in enumerate(grp):
            rows = slice(ti * 128, (ti + 1) * 128)
            k.dma("sp", xt[:], x1.t.ap()[rows, :], dst=xt)
            k.dma("sp", sq[:], g2row.t.ap()[0 if ti < 8 else 1, :].partition_broadcast(128), dst=sq)
            k.op("dve", lambda e, j=j: e.tensor_tensor(out=acc[j][:], in0=acc[j][:], in1=sq[:], op=ALU.mult), reads=[acc[j], sq], writes=[acc[j]])
            k.op("dve", lambda e, j=j: e.tensor_tensor(out=acc[j][:], in0=acc[j][:], in1=xt[:], op=ALU.add), reads=[acc[j], xt], writes=[acc[j]])
            k.dma(SQ, x2.t.ap()[rows, :], acc[j][:], src=acc[j], final=True)
            if final:
                k.dma("sp", xt[:], fg.t.ap().partition_broadcast(128), dst=xt)
                rms_scale(k, acc[j], sq, st, D)
                k.op("dve", lambda e, j=j: e.tensor_tensor(out=acc[j][:], in0=acc[j][:], in1=xt[:], op=ALU.mult), reads=[acc[j], xt],
                     writes=[acc[j]])
                k.dma(SQ, xf.t.ap()[rows, :], acc[j][:], src=acc[j], final=True)
    k.finish()
    return k.nc


def build_l4b3(final=False, n_exp=NE):
    k = KB()
    di = lambda n, s: k.dram(n, s, "ExternalInput")
    x1 = di("x1", [NTOK, D]); modcol = di("modcol", [128, 5 * NCH]); g2row = di("g2row", [2, D])
    rwc = di("rwc", [128, NCH * NE]); rb = di("rb", [NE]); fg = di("fg", [D]); identd = di("identd", [128, 128])
    W1 = di("W1", [NE * D, FF]); W3 = di("W3", [NE * D, FF]); W2 = di("W2", [NE * FF, D])
    x2 = k.dram("x2", [NTOK, D], "ExternalOutput")
    xf = k.dram("xf", [NTOK, D], "ExternalOutput") if final else None
    ident = k.sb([128, 128], "ident")
    k.dma("sp", ident[:], identd.t.ap(), dst=ident)
    mc = k.sb([128, 5 * NCH], "mc"); gs = k.sb([128, 4 * NCH], "gs")
    k.dma("sp", mc[:], modcol.t.ap(), dst=mc)
    g = mc[:, 0:NCH]
    for s in range(2):
        sc = mc[:, (1 + 2 * s) * NCH:(2 + 2 * s) * NCH]
        sh = mc[:, (2 + 2 * s) * NCH:(3 + 2 * s) * NCH]
        k.op("dve", lambda e, s=s, sc=sc: e.tensor_tensor(out=gs[:, 2 * s * NCH:(2 * s + 1) * NCH], in0=g, in1=sc, op=ALU.mult),
             reads=[mc], writes=[gs])
        k.op("dve", lambda e, s=s: e.tensor_tensor(out=gs[:, 2 * s * NCH:(2 * s + 1) * NCH], in0=gs[:, 2 * s * NCH:(2 * s + 1) * NCH],
                                                   in1=g, op=ALU.add), reads=[mc, gs], writes=[gs])
        k.op("dve", lambda e, s=s, sh=sh: e.tensor_copy(out=gs[:, (2 * s + 1) * NCH:(2 * s + 2) * NCH], in_=sh), reads=[mc], writes=[gs])
    LQ, SQ = ("pool", "sp") if MM_R[0] else ("sp", "pool")
    rw = k.sb([128, NCH * NE], "rw", mdt()); rbt = k.sb([128, NE], "rbt")
    k.dma(LQ, rw[:], rwc.t.ap(), dst=rw)
    k.dma("sp", rbt[:], rb.t.ap().partition_broadcast(128), dst=rbt)
    st = k.sb([128, 4], "st")
    G3 = 3
    big = k.sb([128, NCH * G3 * 128], "znT")
    zv = lambda a_, b_: big[:, a_:b_].bitcast(mdt()) if MM_R[0] else big[:, a_:b_]
    acc = [k.sb([128, D], "acc%d" % j) for j in range(G3)]
    xb_ = k.sb([128, 512], "xb_"); gb_ = k.sb([128, 512], "gb_"); sb_ = k.sb([128, 512], "sb_"); s8 = k.sb([128, 8], "s8")
    wh = [k.sb([128, 16, 128], "wh%d" % i, mdt()) for i in range(4)]
    w2s = [k.sb([128, 8, 256], "w2s%d" % i, mdt()) for i in range(2)]
    hidT = k.sb([128, 8 * 384], "hidT", mdt()); sl = k.sb([128, 384], "sl")
    R = [k.sb([128, 160], "R%d" % j) for j in range(G3)]
    pst = [k.ps([128, 512], "pst%d" % i) for i in range(2)]
    pH = [k.ps([128, 384], "pH%d" % i) for i in range(2)]
    po = [k.ps([128, 256], "po%d" % i) for i in range(G3)]
    pr = k.ps([128, NE], "pr")

    def route(r, j):
        o = lambda fn, rd=(), wr=(): k.op("dve", fn, reads=[r] + list(rd), writes=[r] + list(wr))
        k.op("act", lambda e: e.activation(out=r[:, 0:16], in_=pr[:], func=AF.Sigmoid), reads=[pr], writes=[r])
        o(lambda e: e.tensor_tensor(out=r[:, 16:32], in0=r[:, 0:16], in1=rbt[:], op=ALU.add), rd=[rbt])
        sel3 = r[:, 16:32].rearrange("p (g e) -> p g e", e=4)
        ps6 = r[:, 32:56].rearrange("p (g s) -> p g s", s=6)
        idx = 0
        for a in range(4):
            for b in range(a + 1, 4):
                o(lambda e, a=a, b=b, idx=idx: e.tensor_tensor(out=ps6[:, :, idx], in0=sel3[:, :, a], in1=sel3[:, :, b], op=ALU.add))
                idx += 1
        o(lambda e: e.tensor_reduce(out=r[:, 56:60], in_=ps6, axis=AX.X, op=ALU.max))
        o(lambda e: e.tensor_reduce(out=r[:, 60:61], in_=r[:, 56:60], axis=AX.X, op=ALU.max))
        o(lambda e: e.tensor_scalar(out=r[:, 61:65], in0=r[:, 56:60], scalar1=r[:, 60:61], scalar2=None, op0=ALU.is_equal))
        o(lambda e: e.tensor_reduce(out=r[:, 65:69], in_=sel3, axis=AX.X, op=ALU.max))
        for gi in range(4):
            o(lambda e, gi=gi: e.tensor_scalar(out=r[:, 69 + gi * 4:73 + gi * 4], in0=r[:, 16 + gi * 4:20 + gi * 4],
                                               scalar1=r[:, 65 + gi:66 + gi], scalar2=None, op0=ALU.is_equal))
        o(lambda e: e.scalar_tensor_tensor(out=r[:, 85:101], in0=r[:, 69:85], scalar=-1e30, in1=r[:, 16:32], op0=ALU.mult, op1=ALU.add))
        o(lambda e: e.tensor_reduce(out=r[:, 101:105], in_=r[:, 85:101].rearrange("p (g e) -> p g e", e=4), axis=AX.X, op=ALU.max))
        for gi in range(4):
            o(lambda e, gi=gi: e.tensor_scalar(out=r[:, 105 + gi * 4:109 + gi * 4], in0=r[:, 85 + gi * 4:89 + gi * 4],
                                               scalar1=r[:, 101 + gi:102 + gi], scalar2=None, op0=ALU.is_equal))
        o(lambda e: e.tensor_tensor(out=r[:, 105:121], in0=r[:, 105:121], in1=r[:, 69:85], op=ALU.add))
        for gi in range(4):
            o(lambda e, gi=gi: e.tensor_scalar(out=r[:, 105 + gi * 4:109 + gi * 4], in0=r[:, 105 + gi * 4:109 + gi * 4],
                                               scalar1=r[:, 61 + gi:62 + gi], scalar2=None, op0=ALU.mult))
        o(lambda e: e.tensor_tensor(out=r[:, 121:137], in0=r[:, 105:121], in1=r[:, 0:16], op=ALU.mult))
        o(lambda e: e.tensor_reduce(out=r[:, 137:138], in_=r[:, 121:137], axis=AX.X, op=ALU.add))
        o(lambda e: e.reciprocal(out=r[:, 138:139], in_=r[:, 137:138]))
        o(lambda e: e.tensor_scalar(out=r[:, 139:155], in0=r[:, 121:137], scalar1=r[:, 138:139], scalar2=None, op0=ALU.mult))

    for grp in [(0, 1, 2), (3, 4, 5), (6, 7, 8)]:
        ng = len(grp)
        ntok = ng * 128
        for j, ti in enumerate(grp):
            xt = acc[j]; sq = acc[(j + 1) % G3]
            k.dma("sp", xt[:], x1.t.ap()[ti * 128:(ti + 1) * 128, :], dst=xt)
            rms_scale(k, xt, sq, st, D)
            s = 0 if ti < 8 else 1
            transpose_tile(k, xt, ident, pst, lambda c, j=j: zv((c * G3 + j) * 128, (c * G3 + j + 1) * 128), big,
                           gcol=gs[:, 2 * s * NCH:(2 * s + 1) * NCH], scol=gs[:, (2 * s + 1) * NCH:(2 * s + 2) * NCH], mbuf=gs)
            for c in range(NCH):
                k.op("pe", lambda e, c=c, j=j: e.matmul(pr[:], lhsT=zv((c * G3 + j) * 128, (c * G3 + j + 1) * 128),
                                                        rhs=rw[:, c * NE:(c + 1) * NE], start=(c == 0), stop=(c == NCH - 1)),
                     reads=[big, rw], writes=[pr])
            route(R[j], j)
        for j in range(ng):
            k.op("dve", lambda e, j=j: e.memset(acc[j][:], 0.0), writes=[acc[j]])
        for ex in range(n_exp):
            for fc in range(8):
                for wi, Wd in ((0, W1), (2, W3)):
                    for hf in range(2):
                        wbuf = wh[wi + hf]
                        src = Wd.t.ap()[ex * D + hf * 2048:ex * D + (hf + 1) * 2048, :].rearrange("(c p) n -> p c n", p=128)
                        for q2 in range(2):
                            k.dma(LQ, wbuf[:, q2 * 8:(q2 + 1) * 8, :], src[:, q2 * 8:(q2 + 1) * 8, fc * 128:(fc + 1) * 128], dst=wbuf)
                    p = pH[wi // 2]
                    for c in range(NCH):
                        k.op("pe", lambda e, c=c, p=p, wi=wi: mmr(e, p[:, 0:ntok], wh[wi + c // 16][:, c % 16, :],
                                                                  zv(c * 384, c * 384 + ntok), (c == 0), (c == NCH - 1)),
                             reads=[wh[wi + c // 16], big], writes=[p])
                k.op("act", lambda e: e.activation(out=sl[:, 0:ntok], in_=pH[0][:, 0:ntok], func=AF.Silu), reads=[pH[0]], writes=[sl])
                k.op("dve", lambda e, fc=fc: e.tensor_tensor(out=hidT[:, fc * 384:fc * 384 + ntok], in0=sl[:, 0:ntok], in1=pH[1][:, 0:ntok],
                                                             op=ALU.mult), reads=[sl, pH[1]], writes=[hidT])
            for dblk in range(D // 256):
                w2 = w2s[dblk % 2]
                src = W2.t.ap()[ex * FF:(ex + 1) * FF, :].rearrange("(f p) n -> p f n", p=128)
                k.dma(LQ, w2[:], src[:, :, dblk * 256:(dblk + 1) * 256], dst=w2)
                for j in range(ng):
                    p = po[j]
                    for fc in range(8):
                        k.op("pe", lambda e, fc=fc, p=p, j=j: mmr(e, p[:], hidT[:, fc * 384 + j * 128:fc * 384 + (j + 1) * 128],
                                                                  w2[:, fc, :], (fc == 0), (fc == 7)),
                             reads=[hidT, w2], writes=[p])
                    k.op("dve", lambda e, p=p, j=j, dblk=dblk: e.scalar_tensor_tensor(
                        out=acc[j][:, dblk * 256:(dblk + 1) * 256], in0=p[:], scalar=R[j][:, 139 + ex:140 + ex],
                        in1=acc[j][:, dblk * 256:(dblk + 1) * 256], op0=ALU.mult, op1=ALU.add), reads=[p, R[j], acc[j]], writes=[acc[j]])
        for j, ti in enumerate(grp):
            rows = slice(ti * 128, (ti + 1) * 128)
            a_ = acc[j]
            for blk in range(8):
                cs = slice(blk * 512, (blk + 1) * 512)
                k.dma("sp", xb_[:], x1.t.ap()[rows, cs], dst=xb_)
                k.dma("sp", gb_[:], g2row.t.ap()[0 if ti < 8 else 1, cs].partition_broadcast(128), dst=gb_)
                k.op("dve", lambda e, a_=a_, cs=cs: e.tensor_tensor(out=a_[:, cs], in0=a_[:, cs], in1=gb_[:], op=ALU.mult),
                     reads=[a_, gb_], writes=[a_])
                k.op("dve", lambda e, a_=a_, cs=cs: e.tensor_tensor(out=a_[:, cs], in0=a_[:, cs], in1=xb_[:], op=ALU.add),
                     reads=[a_, xb_], writes=[a_])
                if final:
                    k.op("dve", lambda e, a_=a_, cs=cs: e.tensor_tensor(out=sb_[:], in0=a_[:, cs], in1=a_[:, cs], op=ALU.mult),
                         reads=[a_], writes=[sb_])
                    k.op("dve", lambda e, blk=blk: e.tensor_reduce(out=s8[:, blk:blk + 1], in_=sb_[:], axis=AX.X, op=ALU.add),
                         reads=[sb_], writes=[s8])
            k.dma(SQ, x2.t.ap()[rows, :], a_[:], src=a_, final=True)
            if final:
                k.op("dve", lambda e: e.tensor_reduce(out=st[:, 0:1], in_=s8[:, 0:8], axis=AX.X, op=ALU.add), reads=[s8], writes=[st])
                k.op("dve", lambda e: e.tensor_scalar(out=st[:, 1:2], in0=st[:, 0:1], scalar1=1.0 / D, scalar2=1e-6, op0=ALU.mult,
                                                      op1=ALU.add), reads=[st], writes=[st])
                k.op("act", lambda e: e.activation(out=st[:, 2:3], in_=st[:, 1:2], func=AF.Sqrt), reads=[st], writes=[st])
                k.op("dve", lambda e: e.reciprocal(out=st[:, 3:4], in_=st[:, 2:3]), reads=[st], writes=[st])
                for blk in range(8):
                    cs = slice(blk * 512, (blk + 1) * 512)
                    k.dma("sp", gb_[:], fg.t.ap()[cs].partition_broadcast(128), dst=gb_)
                    k.op("dve", lambda e, a_=a_, cs=cs: e.scalar_tensor_tensor(out=a_[:, cs], in0=a_[:, cs], scalar=st[:, 3:4], in1=gb_[:],
                                                                               op0=ALU.mult, op1=ALU.mult), reads=[a_, st, gb_], writes=[a_])
                k.dma(SQ, xf.t.ap()[rows, :], a_[:], src=a_, final=True)
    k.finish()
    return k.nc


def l4b_inmaps(x1x, x1c, mod_l, norm2_g_l, router_w, router_b, w1, w3, w2, final_g):
    ident = np.eye(128, dtype=np.float32)
    rwc = np.ascontiguousarray(router_w.reshape(NCH, 128, NE).transpose(1, 0, 2).reshape(128, NCH * NE))
    W1 = w1.reshape(NE * D, FF); W3 = w3.reshape(NE * D, FF); W2 = w2.reshape(NE * FF, D)
    in_maps = []
    for i in range(NCORES):
        b, t0, cj = tok_rows(i)
        mc = np.concatenate([col_layout(norm2_g_l), col_layout(mod_l[b, 4 * D:5 * D]), col_layout(mod_l[b, 3 * D:4 * D]),
                             col_layout(mod_l[2, 4 * D:5 * D]), col_layout(mod_l[2, 3 * D:4 * D])], 1)
        in_maps.append({"x1": np.ascontiguousarray(np.concatenate([x1x[b, t0:t0 + 1024], x1c[b, cj * 128:(cj + 1) * 128]], 0)),
                        "modcol": np.ascontiguousarray(mc),
                        "g2row": np.ascontiguousarray(np.stack([mod_l[b, 5 * D:6 * D], mod_l[2, 5 * D:6 * D]])),
                        "rwc": rwc, "rb": np.ascontiguousarray(router_b), "fg": np.ascontiguousarray(final_g), "identd": ident,
                        "W1": W1, "W3": W3, "W2": W2})
    return in_maps


_ALL = list(range(NCORES))


def _run(nc, in_maps):
    return run_bass_kernel_spmd(nc, in_maps, core_ids=_ALL)


def kernel(x, c, ctx, c_ctx, ada_w, ada_b, norm1_g, w_in, rwkv_conv, attn_sink, cmlp_norm_g, cmlp_ws, cmlp_b,
           rwkv_w0, rwkv_w1, rwkv_w2, rwkv_a0, rwkv_a1, rwkv_a2, rwkv_kk, rwkv_ka, rwkv_rk, rwkv_ln_w, rwkv_ln_b,
           w_out, norm2_g, router_w, router_b, moe_w1, moe_w3, moe_w2, final_g):
    f = lambda a: np.ascontiguousarray(np.asarray(a, dtype=np.float32))
    x, h = f(x), f(ctx)
    mod = run_ada(f(c), f(c_ctx), np.asarray(ada_w), np.asarray(ada_b))
    xf = None
    for l in range(2):
        P = dict(attn_sink=f(attn_sink[l]), cmlp_norm_g=f(cmlp_norm_g[l]), cmlp_ws=f(cmlp_ws[l]), cmlp_b=f(cmlp_b[l]),
                 rwkv_conv=f(rwkv_conv[l]), rwkv_w0=f(rwkv_w0[l]), rwkv_w2=f(rwkv_w2[l]), rwkv_a0=f(rwkv_a0[l]),
                 rwkv_a2=f(rwkv_a2[l]), rwkv_kk=f(rwkv_kk[l]), rwkv_ka=f(rwkv_ka[l]), rwkv_rk=f(rwkv_rk[l]),
                 rwkv_ln_w=f(rwkv_ln_w[l]), rwkv_ln_b=f(rwkv_ln_b[l]), w_out=f(w_out[l]))
        Wcat = np.ascontiguousarray(np.concatenate([w_in[l], rwkv_w1[l][0], rwkv_w1[l][1], rwkv_a1[l][0], rwkv_a1[l][1]], 1))
        px, ph = run_l1(x, h, mod[l], f(norm1_g[l]), Wcat)
        del Wcat
        attn_x, attn_c, cm_x, cm_c, sc_x, sc_c = l2_gather(_run(build_l2(), l2_inmaps(px, ph, P)))
        y = l3_gather(_run(build_l3(), l3_inmaps(sc_x, sc_c)))
        x1x, x1c = tok_gather(_run(build_l4a(), l4a_inmaps(y, sc_x, sc_c, px, ph, attn_x, attn_c, cm_x, cm_c, x, h, mod[l], P)),
                              "x1", D)
        del px, ph, attn_x, attn_c, cm_x, cm_c, sc_x, sc_c, y
        last = l == 1
        res = _run(build_l4b3(final=last), l4b_inmaps(x1x, x1c, mod[l], f(norm2_g[l]), f(router_w), f(router_b),
                                                     f(moe_w1[l]), f(moe_w3[l]), f(moe_w2[l]), f(final_g)))
        x, h = tok_gather(res, "x2", D)
        if last:
            xf, _ = tok_gather(res, "xf", D)
    return xf
```
